# Optimizing a Trainium2 kernel written in Bass

```python
import math
import jax
import jax.numpy as jnp
from jax import lax
import numpy as np


D_MODEL = 1024
BATCH = 8
SEQ = 2048
DEPTH = 1

MEM_TOKENS = 256
GRID_W = 64
Q_BLOCK = 128
ROPE_THETA = 10000.0

A_HEADS = 8
A_KV_HEADS = 2
A_GROUP = A_HEADS // A_KV_HEADS
A_HEAD_DIM = D_MODEL // 16
D_A = A_HEADS * A_HEAD_DIM

B_HEADS = 4
B_HEAD_DIM = D_MODEL // 16
B_VDIM = 2 * B_HEAD_DIM
D_B = B_HEADS * B_VDIM

D_MIX = D_A + D_B

A_Q_COLS = A_HEADS * A_HEAD_DIM
A_KV_COLS = A_KV_HEADS * A_HEAD_DIM
B_QK_COLS = B_HEADS * 2 * B_HEAD_DIM
B_V_COLS = B_HEADS * B_VDIM
D_IN_PROJ = A_Q_COLS + 2 * A_KV_COLS + 2 * B_QK_COLS + B_V_COLS
IN_SPLITS = (A_Q_COLS,
             A_Q_COLS + A_KV_COLS,
             A_Q_COLS + 2 * A_KV_COLS,
             A_Q_COLS + 2 * A_KV_COLS + B_QK_COLS,
             A_Q_COLS + 2 * A_KV_COLS + 2 * B_QK_COLS)

MEM_HEADS = 4
MEM_HEAD_DIM = D_MODEL // MEM_HEADS

N_EXPERTS = 32
TOP_K = 4
D_FF = D_MODEL
SWIGLU_LIMIT = 7.0
SWIGLU_ALPHA = 1.702

DEEPNORM_ALPHA = (2.0 * DEPTH) ** 0.25
DEEPNORM_BETA = (8.0 * DEPTH) ** -0.25
LN_EPS = 1e-5
RMS_EPS = 1e-6

kernel_name = 'hybrid_gqa_axial_diffattn_memxattn_moe_deepnorm'


def layer_norm(x, g, b):
    xf = x.astype(jnp.float32)
    mu = jnp.mean(xf, axis=-1, keepdims=True)
    xc = xf - mu
    var = jnp.mean(xc * xc, axis=-1, keepdims=True)
    y = xc * lax.rsqrt(var + LN_EPS) * g.astype(jnp.float32) + b.astype(jnp.float32)
    return y.astype(x.dtype)


def rms_norm(x, g):
    xf = x.astype(jnp.float32)
    y = xf * lax.rsqrt(jnp.mean(xf * xf, axis=-1, keepdims=True) + RMS_EPS) * g.astype(jnp.float32)
    return y.astype(x.dtype)


def rope_cos_sin(pos, dim):
    inv = ROPE_THETA ** (-jnp.arange(0, dim, 2, dtype=jnp.float32) / dim)
    ang = pos.astype(jnp.float32)[:, None] * inv[None, :]
    return jnp.cos(ang), jnp.sin(ang)


def apply_rope(x, cos, sin):
    xf = x.astype(jnp.float32)
    x1, x2 = jnp.split(xf, 2, axis=-1)
    return jnp.concatenate([x1 * cos - x2 * sin, x2 * cos + x1 * sin], axis=-1).astype(x.dtype)


def axial_rope(x, cs_row, cs_col):
    half = x.shape[-1] // 2
    return jnp.concatenate([apply_rope(x[..., :half], *cs_row),
                            apply_rope(x[..., half:], *cs_col)], axis=-1)


def sweep_query_blocks(block_fn, q):
    S = q.shape[-2]
    nblk = S // Q_BLOCK
    qb = q.reshape(q.shape[:-2] + (nblk, Q_BLOCK, q.shape[-1]))
    qb = jnp.moveaxis(qb, -3, 0)
    out = lax.map(block_fn, qb)
    out = jnp.moveaxis(out, 0, -3)
    return out.reshape(out.shape[:-3] + (S, out.shape[-1]))


def gqa_axial_attention(q, k, v, q_g, k_g, cs_row, cs_col):
    B, S = q.shape[0], q.shape[1]
    q = axial_rope(rms_norm(q, q_g).transpose(0, 2, 1, 3), cs_row, cs_col)
    k = axial_rope(rms_norm(k, k_g).transpose(0, 2, 1, 3), cs_row, cs_col)
    v = v.transpose(0, 2, 1, 3)
    q = q.reshape(B, A_KV_HEADS, A_GROUP, S, A_HEAD_DIM)
    scale = A_HEAD_DIM ** -0.5

    def block(qb):
        s = jnp.einsum('bkgqd,bksd->bkgqs', qb, k).astype(jnp.float32) * scale
        p = jax.nn.softmax(s, axis=-1).astype(v.dtype)
        return jnp.einsum('bkgqs,bksd->bkgqd', p, v)

    o = sweep_query_blocks(block, q)
    return o.reshape(B, A_HEADS, S, A_HEAD_DIM).transpose(0, 2, 1, 3).reshape(B, S, D_A)


def differential_attention(q, k, v, lq1, lk1, lq2, lk2, subln_g, cs, lambda_init):
    B, S = q.shape[0], q.shape[1]
    q = apply_rope(q.transpose(0, 2, 3, 1, 4), *cs)
    k = apply_rope(k.transpose(0, 2, 3, 1, 4), *cs)
    v = v.transpose(0, 2, 1, 3)
    lam = (jnp.exp(jnp.sum(lq1.astype(jnp.float32) * lk1.astype(jnp.float32)))
           - jnp.exp(jnp.sum(lq2.astype(jnp.float32) * lk2.astype(jnp.float32)))
           + lambda_init)
    scale = B_HEAD_DIM ** -0.5

    def block(qb):
        s = jnp.einsum('bhcqd,bhcsd->bhcqs', qb, k).astype(jnp.float32) * scale
        p = jax.nn.softmax(s, axis=-1)
        a = (p[:, :, 0] - lam * p[:, :, 1]).astype(v.dtype)
        return jnp.einsum('bhqs,bhsv->bhqv', a, v)

    o = sweep_query_blocks(block, q)
    o = rms_norm(o, subln_g) * (1.0 - lambda_init)
    return o.transpose(0, 2, 1, 3).reshape(B, S, D_B)


def memory_cross_attention(x, mem, w_q, w_kv, w_o):
    B, S, D = x.shape
    M = mem.shape[1]
    q = (x @ w_q).reshape(B, S, MEM_HEADS, MEM_HEAD_DIM)
    k, v = jnp.split(mem @ w_kv, 2, axis=-1)
    k = k.reshape(B, M, MEM_HEADS, MEM_HEAD_DIM)
    v = v.reshape(B, M, MEM_HEADS, MEM_HEAD_DIM)
    s = jnp.einsum('bqhd,bmhd->bhqm', q, k).astype(jnp.float32) * (MEM_HEAD_DIM ** -0.5)
    p = jax.nn.softmax(s, axis=-1).astype(v.dtype)
    o = jnp.einsum('bhqm,bmhd->bqhd', p, v).reshape(B, S, D)
    return o @ w_o


def clamped_swiglu(g, u):
    g = jnp.minimum(g, SWIGLU_LIMIT)
    u = jnp.clip(u, -SWIGLU_LIMIT, SWIGLU_LIMIT)
    return g * jax.nn.sigmoid(SWIGLU_ALPHA * g) * (u + 1.0)


def routed_experts(x, w_r, b_r, w_g, b_g, w_u, b_u, w_d, b_d):
    B, S, D = x.shape
    xt = x.reshape(B * S, D)
    logits = (xt @ w_r + b_r).astype(jnp.float32)
    top_v, top_i = lax.top_k(logits, TOP_K)
    gates = jax.nn.softmax(top_v, axis=-1)
    combine = jnp.einsum('nk,nke->ne', gates,
                         jax.nn.one_hot(top_i, N_EXPERTS, dtype=jnp.float32)).astype(x.dtype)
    y = jnp.zeros_like(xt)
    for e in range(N_EXPERTS):
        h = clamped_swiglu(xt @ w_g[e] + b_g[e], xt @ w_u[e] + b_u[e])
        y = y + combine[:, e:e + 1] * (h @ w_d[e] + b_d[e])
    return y.reshape(B, S, D)


def setup_inputs(seed: int = 0) -> dict:
    key = jax.random.key(seed)
    ks = jax.random.split(key, 32)
    L, D, E, F = DEPTH, D_MODEL, N_EXPERTS, D_FF

    def nrm(k, shape, std):
        return std * jax.random.normal(k, shape, jnp.float32)

    def gain(k, shape):
        return 1.0 + nrm(k, shape, 0.02)

    return {
        'x': nrm(ks[0], (BATCH, SEQ, D), 1.0),
        'mem': nrm(ks[1], (BATCH, MEM_TOKENS, D), 1.0),
        'w_in': nrm(ks[2], (L, D, D_IN_PROJ), D ** -0.5),
        'a_q_norm': gain(ks[3], (L, A_HEAD_DIM)),
        'a_k_norm': gain(ks[4], (L, A_HEAD_DIM)),
        'b_lambda_q1': nrm(ks[5], (L, B_HEAD_DIM), 0.1),
        'b_lambda_k1': nrm(ks[6], (L, B_HEAD_DIM), 0.1),
        'b_lambda_q2': nrm(ks[7], (L, B_HEAD_DIM), 0.1),
        'b_lambda_k2': nrm(ks[8], (L, B_HEAD_DIM), 0.1),
        'b_subln': gain(ks[9], (L, B_VDIM)),
        'w_mix_out': nrm(ks[10], (L, D_MIX, D), D_MIX ** -0.5 * DEEPNORM_BETA),
        'ln1_g': gain(ks[11], (L, D)),
        'ln1_b': nrm(ks[12], (L, D), 0.02),
        'w_mem_q': nrm(ks[13], (L, D, D), D ** -0.5),
        'w_mem_kv': nrm(ks[14], (L, D, 2 * D), D ** -0.5),
        'w_mem_out': nrm(ks[15], (L, D, D), D ** -0.5 * DEEPNORM_BETA),
        'ln2_g': gain(ks[16], (L, D)),
        'ln2_b': nrm(ks[17], (L, D), 0.02),
        'w_router': nrm(ks[18], (L, D, E), D ** -0.5),
        'b_router': nrm(ks[19], (L, E), 0.01),
        'w_e_gate': nrm(ks[20], (L, E, D, F), D ** -0.5),
        'b_e_gate': nrm(ks[21], (L, E, F), 0.02),
        'w_e_up': nrm(ks[22], (L, E, D, F), D ** -0.5),
        'b_e_up': nrm(ks[23], (L, E, F), 0.02),
        'w_e_down': nrm(ks[24], (L, E, F, D), F ** -0.5 * DEEPNORM_BETA),
        'b_e_down': nrm(ks[25], (L, E, D), 0.02),
        'ln3_g': gain(ks[26], (L, D)),
        'ln3_b': nrm(ks[27], (L, D), 0.02),
    }


def reference(x, mem, w_in, a_q_norm, a_k_norm, b_lambda_q1, b_lambda_k1, b_lambda_q2,
              b_lambda_k2, b_subln, w_mix_out, ln1_g, ln1_b, w_mem_q, w_mem_kv, w_mem_out,
              ln2_g, ln2_b, w_router, b_router, w_e_gate, b_e_gate, w_e_up, b_e_up,
              w_e_down, b_e_down, ln3_g, ln3_b):
    B, S, D = x.shape
    ROWS = S // GRID_W
    grid = jnp.stack(jnp.meshgrid(jnp.arange(ROWS, dtype=jnp.int32),
                                  jnp.arange(GRID_W, dtype=jnp.int32), indexing='ij'), axis=-1)
    grid = grid.reshape(S, 2)
    cs_row = rope_cos_sin(grid[:, 0], A_HEAD_DIM // 2)
    cs_col = rope_cos_sin(grid[:, 1], A_HEAD_DIM // 2)
    cs_seq = rope_cos_sin(jnp.arange(S, dtype=jnp.int32), B_HEAD_DIM)

    for layer in range(DEPTH):
        lambda_init = 0.8 - 0.6 * math.exp(-0.3 * layer)
        h = x @ w_in[layer]
        qa, ka, va, qb, kb, vb = jnp.split(h, IN_SPLITS, axis=-1)
        out_a = gqa_axial_attention(qa.reshape(B, S, A_HEADS, A_HEAD_DIM),
                                    ka.reshape(B, S, A_KV_HEADS, A_HEAD_DIM),
                                    va.reshape(B, S, A_KV_HEADS, A_HEAD_DIM),
                                    a_q_norm[layer], a_k_norm[layer], cs_row, cs_col)
        out_b = differential_attention(qb.reshape(B, S, B_HEADS, 2, B_HEAD_DIM),
                                       kb.reshape(B, S, B_HEADS, 2, B_HEAD_DIM),
                                       vb.reshape(B, S, B_HEADS, B_VDIM),
                                       b_lambda_q1[layer], b_lambda_k1[layer],
                                       b_lambda_q2[layer], b_lambda_k2[layer],
                                       b_subln[layer], cs_seq, lambda_init)
        mix = jnp.concatenate([out_a, out_b], axis=-1) @ w_mix_out[layer]
        x = layer_norm(DEEPNORM_ALPHA * x + mix, ln1_g[layer], ln1_b[layer])
        xa = memory_cross_attention(x, mem, w_mem_q[layer], w_mem_kv[layer], w_mem_out[layer])
        x = layer_norm(DEEPNORM_ALPHA * x + xa, ln2_g[layer], ln2_b[layer])
        ff = routed_experts(x, w_router[layer], b_router[layer], w_e_gate[layer], b_e_gate[layer],
                            w_e_up[layer], b_e_up[layer], w_e_down[layer], b_e_down[layer])
        x = layer_norm(DEEPNORM_ALPHA * x + ff, ln3_g[layer], ln3_b[layer])
    return x
```

```python
import contextlib
import numpy as np
import concourse.bass as bass
import concourse.mybir as mybir
from concourse.bass_utils import run_bass_kernel_spmd

F32 = mybir.dt.float32
BF16 = mybir.dt.bfloat16
I32 = mybir.dt.int32
U32 = mybir.dt.uint32
AF = mybir.ActivationFunctionType
ALU = mybir.AluOpType
AX = mybir.AxisListType

S = 2048
D = 1024
NT = 16
NE = 32
ALPHA = 2.0 ** 0.25
LAMBDA_INIT = 0.2
LN_EPS = 1e-5
RMS_EPS = 1e-6
DMA_RING = 16
ARENA_W = 52600
CAP = 640
NCH = CAP // 128
NSLOT = NE * CAP


class Prog:
    ENG = ("pe", "act", "dve", "pool", "sp")

    def __init__(self):
        self.stream = {e: [] for e in self.ENG}
        self.nops = {e: 0 for e in self.ENG}
        self.writer = {}
        self.readers = {}
        self.dma_n = {e: 0 for e in self.ENG}
        self.milestones = {e: set() for e in self.ENG}

    @staticmethod
    def _ident(tok):
        return (tok[0], tok[1])

    def _merge(self, d, tok):
        i = self._ident(tok)
        if i not in d or d[i][2] < tok[2]:
            d[i] = tok

    def alias(self, new_keys, old_keys):
        acc = {}
        for k in old_keys:
            w = self.writer.get(k)
            if w is not None:
                self._merge(acc, w)
            for t in self.readers.get(k, {}).values():
                self._merge(acc, t)
        for k in new_keys:
            r = self.readers.setdefault(k, {})
            for t in acc.values():
                self._merge(r, t)

    def op(self, eng, fn, reads=(), writes=(), dma=False):
        deps = {}
        for k in reads:
            w = self.writer.get(k)
            if w is not None:
                self._merge(deps, w)
        for k in writes:
            w = self.writer.get(k)
            if w is not None:
                self._merge(deps, w)
            for t in self.readers.get(k, {}).values():
                self._merge(deps, t)
        waits = []
        for t in deps.values():
            if t[0] == "c" and t[1] == eng and eng == "pe":
                continue
            waits.append(t)
            if t[0] == "c":
                self.milestones[t[1]].add(t[2])
        if dma:
            n = self.dma_n[eng]
            self.dma_n[eng] = n + 1
            sem = (eng, n % DMA_RING)
            if n >= DMA_RING:
                waits.append(("d", sem, 16 * (n // DMA_RING)))
            tok = ("d", sem, 16 * (n // DMA_RING + 1))
            self.stream[eng].append(("dma", fn, waits, tok))
        else:
            self.nops[eng] += 1
            tok = ("c", eng, self.nops[eng])
            self.stream[eng].append(("op", fn, waits, tok))
        for k in writes:
            self.writer[k] = tok
            self.readers[k] = {}
        for k in reads:
            if k in writes:
                continue
            self._merge(self.readers.setdefault(k, {}), tok)
        return tok

    def wait_tokens(self, eng, toks):
        for t in toks:
            if t[0] == "c":
                self.milestones[t[1]].add(t[2])
        self.stream[eng].append(("wait", None, list(toks), None))

    def emit(self, block, nc, esem, dsem):
        rank = {}
        for e in self.ENG:
            ms = sorted(self.milestones[e])
            rank[e] = {idx: i + 1 for i, idx in enumerate(ms)}

        def run(eng_name, eng):
            known = {}
            for kind, fn, waits, tok in self.stream[eng_name]:
                for t in waits:
                    if t[0] == "c":
                        sem = esem[t[1]]
                        val = rank[t[1]][t[2]]
                        key = ("c", t[1])
                    else:
                        sem = dsem[t[1]]
                        val = t[2]
                        key = ("d", t[1])
                    if known.get(key, 0) >= val:
                        continue
                    known[key] = val
                    eng.wait_ge(sem, val)
                if kind == "wait":
                    continue
                ins = fn(eng)
                if kind == "dma":
                    ins.then_inc(dsem[tok[1]], 16)
                elif tok[2] in rank[eng_name]:
                    ins.then_inc(esem[eng_name], 1)

        @block.tensor
        def _(e):
            run("pe", e)

        @block.scalar
        def _(e):
            run("act", e)

        @block.vector
        def _(e):
            run("dve", e)

        @block.gpsimd
        def _(e):
            run("pool", e)

        @block.sync
        def _(e):
            run("sp", e)


def build_program(stage="full"):
    nc = bass.Bass("TRN2", target_bir_lowering=False)

    def din(name, shape):
        return nc.dram_tensor(name, list(shape), F32, kind="ExternalInput").ap()

    x_d = din("x", [S, D])
    mem_d = din("mem", [256, D])
    w_in_d = din("w_in", [D, 2304])
    aq_d = din("a_q_norm", [1, 64])
    ak_d = din("a_k_norm", [1, 64])
    lam_d = din("b_lambda", [4, 64])
    subln_d = din("b_subln", [128, 1])
    wmix_d = din("w_mix_out", [D, D])
    ln_d = din("ln_gb", [6, D])
    wq_d = din("w_mem_q", [D, D])
    wkv_d = din("w_mem_kv", [D, 2 * D])
    wo_d = din("w_mem_out", [D, D])
    wr_d = din("w_router", [D, NE])
    br_d = din("b_router", [1, NE])
    weg_d = din("w_e_gate", [NE, D, D])
    beg_d = din("b_e_gate", [NE, D])
    weu_d = din("w_e_up", [NE, D, D])
    beu_d = din("b_e_up", [NE, D])
    wed_d = din("w_e_down", [NE, D, D])
    bed_d = din("b_e_down", [NE, D])
    ropeA_d = din("ropeA", [S, 2, 64])
    ropeB_d = din("ropeB", [S, 2, 64])
    zeros_d = din("zeros_rows", [2560, D])
    out_d = nc.dram_tensor("out", [S, D], F32, kind="ExternalOutput").ap()
    xrows_d = nc.dram_tensor("xrows", [S, D], BF16).ap()
    tok_d = nc.dram_tensor("tokslots", [NSLOT, 1], I32).ap()
    yg_d = nc.dram_tensor("ygrows", [NSLOT, D], F32).ap()
    dbg_d = None
    if stage != "full":
        dbg_d = nc.dram_tensor("dbg", [128, 40960], F32, kind="ExternalOutput").ap()

    P = Prog()
    st = contextlib.ExitStack()
    with st:
        arena = st.enter_context(nc.sbuf_tensor("arena", [128, ARENA_W], F32))
        ps = [st.enter_context(nc.psum_tensor(f"ps{i}", [128, 512], F32)) for i in range(8)]
        esem = {e: st.enter_context(nc.semaphore(f"s_{e}")) for e in Prog.ENG}
        dsem = {}
        for e in ("sp", "pool"):
            for i in range(DMA_RING):
                dsem[(e, i)] = st.enter_context(nc.semaphore(f"d_{e}{i}"))

        def f32v(off, n):
            return arena[:, off:off + n]

        def bfv(off, n_words):
            return arena[:, off:off + n_words].bitcast(BF16)

        R_OFF, XT_OFF, WR_OFF, HT_OFF, TMP_OFF, MISC_OFF = 0, 16384, 24576, 36864, 45056, 48128
        Rt = f32v(R_OFF, 16384).rearrange("p (c d) -> p c d", c=NT)
        XT = bfv(XT_OFF, 8192).rearrange("p (c t) -> p c t", c=8)

        mo = [MISC_OFF]

        def misc(nw):
            o = mo[0]
            mo[0] += nw
            assert mo[0] <= ARENA_W
            return o

        identF = f32v(misc(128), 128)
        identB = bfv(misc(64), 64)
        onesB = bfv(misc(64), 64)
        lnp = f32v(misc(2048), 2048).rearrange("p (a d) -> p a d", a=2)
        Cmb = f32v(misc(512), 512).rearrange("p (c e) -> p c e", c=NT)
        bgT = f32v(misc(256), 256).rearrange("p (e f) -> p e f", e=NE)
        buT = f32v(misc(256), 256).rearrange("p (e f) -> p e f", e=NE)
        wr_sb = f32v(misc(256), 256).rearrange("p (c e) -> p c e", c=8)
        br_sb = f32v(misc(32), 32)
        gq_sb = f32v(misc(64), 64)
        gk_sb = f32v(misc(64), 64)
        small = f32v(misc(64), 64)
        LNP_OFF = MISC_OFF + 128 + 64 + 64
        ropeT = f32v(LNP_OFF, 512).rearrange("p (s k a d) -> p s k a d", s=2, k=2, a=2)

        gidx_t = st.enter_context(nc.sbuf_tensor("gidx_t", [128, NE * NCH], I32))
        DEST_t = st.enter_context(nc.sbuf_tensor("DEST_t", [128, NT * 4], I32))
        DESTG_t = st.enter_context(nc.sbuf_tensor("DESTG_t", [128, NT * 4], I32))
        TOKID_t = st.enter_context(nc.sbuf_tensor("TOKID_t", [128, NT], I32))
        YIDX_t = st.enter_context(nc.sbuf_tensor("YIDX_t", [128, NE * NCH], I32))
        gidx_all = gidx_t[:, :].rearrange("p (e a) -> p e a", e=NE)
        DEST = DEST_t[:, :].rearrange("p (c k) -> p c k", c=NT)
        DESTG = DESTG_t[:, :].rearrange("p (c k) -> p c k", c=NT)
        Gk = f32v(misc(64), 64).rearrange("p (c k) -> p c k", c=NT)
        TOKID = TOKID_t[:, :]
        cnt = f32v(misc(32), 32)
        iota32 = f32v(misc(32), 32)
        Ltri = bfv(misc(64), 64)
        lam_col = small[:, 0:1]
        nlam_col = small[:, 1:2]
        gs_col = small[:, 2:3]
        lsum = small[:, 4:8]
        eps_col = small[:, 8:9]
        rmseps_col = small[:, 9:10]
        sig_bias_col = small[:, 10:11]

        psb = [p[:].bitcast(BF16) for p in ps]

        pool_regs = {}

        def mk_bc_reg(e):
            pool_regs["bc"] = e.alloc_register("bc")
            pool_regs["bx"] = e.alloc_register("bx")
            e.reg_mov(pool_regs["bx"], S - 1)
            return e.reg_mov(pool_regs["bc"], NSLOT - 1)

        P.op("pool", mk_bc_reg, writes=["bcreg"])
        P.op("pool", lambda e: e.memset(identF, 0.0), writes=["identF"])
        P.op("pool", lambda e: e.affine_select(out=identF, in_=identF, pattern=[[-1, 128]],
                                               compare_op=ALU.not_equal, fill=1.0, base=0,
                                               channel_multiplier=1), reads=["identF"], writes=["identF"])
        P.op("pool", lambda e: e.tensor_copy(out=identB, in_=identF), reads=["identF"], writes=["identB"])
        P.op("pool", lambda e: e.memset(onesB, 1.0), writes=["onesB"])
        P.op("pool", lambda e: e.memset(eps_col, LN_EPS), writes=["eps"])
        P.op("pool", lambda e: e.memset(rmseps_col, RMS_EPS), writes=["eps2"])
        P.op("pool", lambda e: e.memset(sig_bias_col, 1.702 * 7.0), writes=["sigb"])

        iota_i = small[:, 32:64].bitcast(I32)
        P.op("pool", lambda e: e.iota(iota_i, pattern=[[1, 32]], base=0, channel_multiplier=0), writes=["iota_i"])
        P.op("pool", lambda e: e.tensor_copy(out=iota32, in_=iota_i), reads=["iota_i"], writes=["iota32"])
        P.op("pool", lambda e: e.iota(TOKID, pattern=[[128, NT]], base=0, channel_multiplier=1), writes=["TOKID"])
        P.op("pool", lambda e: e.memset(cnt, 0.0), writes=["cnt"])
        P.op("pool", lambda e: e.memset(Ltri, 1.0), writes=["Ltri"])
        P.op("pool", lambda e: e.affine_select(out=Ltri, in_=Ltri, pattern=[[1, 128]], compare_op=ALU.is_gt, fill=0.0,
                                               base=0, channel_multiplier=-1), reads=["Ltri"], writes=["Ltri"])
        P.op("pool", lambda e: e.memset(gidx_t[:, :], 30000), writes=["gidx"])
        P.op("pool", lambda e: e.dma_start(out=tok_d.rearrange("(p a) o -> p (a o)", p=128),
                                           in_=gidx_t[:, :]), reads=["gidx"], writes=["TOKZ"], dma=True)
        P.op("sp", lambda e: e.dma_start(out=gq_sb, in_=aq_d.partition_broadcast(128)), writes=["gq"], dma=True)
        P.op("sp", lambda e: e.dma_start(out=gk_sb, in_=ak_d.partition_broadcast(128)), writes=["gk"], dma=True)

        xin = [f32v(TMP_OFF + i * 1024, 1024) for i in range(2)]
        Ering = [bfv(TMP_OFF + 2048 + i * 256, 256) for i in range(4)]

        def load_x(tc, slot):
            P.op("sp", lambda e: e.dma_start(out=xin[slot], in_=x_d[tc * 128:(tc + 1) * 128, :]),
                 writes=[("xin", slot)], dma=True)

        evac_flip = [0]

        def transpose_tile_to_XT(src, src_key, tc, banks):
            for half in range(2):
                b = banks[half]
                for j in range(4):
                    dc = half * 4 + j
                    P.op("pe", lambda e, b=b, j=j, dc=dc: e.transpose(ps[b][:, j * 128:(j + 1) * 128],
                                                                      src[:, dc * 128:(dc + 1) * 128], identF),
                         reads=[src_key, "identF"], writes=[("ps", b)])
                dst = XT[:, half * 4:(half + 1) * 4, tc * 128:(tc + 1) * 128]
                srcp = ps[b][:].rearrange("p (j t) -> p j t", j=4)
                eng = "act" if evac_flip[0] % 2 == 0 else "dve"
                evac_flip[0] += 1
                if eng == "act":
                    P.op("act", lambda e, dst=dst, srcp=srcp: e.activation(out=dst, in_=srcp, func=AF.Copy),
                         reads=[("ps", b)], writes=[("XT", tc, half)])
                else:
                    P.op("dve", lambda e, dst=dst, srcp=srcp: e.tensor_copy(out=dst, in_=srcp),
                         reads=[("ps", b)], writes=[("XT", tc, half)])

        def XTk(tcs):
            return [("XT", tc, h) for tc in tcs for h in range(2)]

        wi = bfv(WR_OFF, 9216).rearrange("p (c n) -> p c n", c=8)
        col_tiles = [(0, 512), (512, 768), (768, 1280), (1280, 1792), (1792, 2304)]
        w_in_v = w_in_d.rearrange("(c p) n -> p c n", p=128)
        for ci, (c0, c1) in enumerate(col_tiles):
            P.op("pool", lambda e, c0=c0, c1=c1: e.dma_start(out=wi[:, :, c0:c1], in_=w_in_v[:, :, c0:c1]),
                 writes=[("wi", ci)], dma=True)

        for tc in range(NT):
            load_x(tc, tc % 2)
            transpose_tile_to_XT(xin[tc % 2], ("xin", tc % 2), tc, (2 * (tc % 2), 2 * (tc % 2) + 1))

        QTA = bfv(R_OFF + 0, 4096).rearrange("p (j t) -> p j t", j=4)
        KTA = bfv(R_OFF + 4096, 1024)
        QKTB = bfv(R_OFF + 5120, 8192).rearrange("p (j t) -> p j t", j=8)
        VA = bfv(R_OFF + 13312, 3072).rearrange("p (c k n) -> p c k n", c=NT, k=2)
        VB = bfv(HT_OFF, 4096).rearrange("p (c n) -> p c n", c=NT)
        T1 = WR_OFF + 9216
        sqA = f32v(T1, 640)
        tA1 = f32v(T1 + 640, 640)
        tA2 = f32v(T1 + 1280, 640)
        tB1 = f32v(T1 + 1920, 512)
        tB2 = f32v(T1 + 2432, 512)
        ssA = f32v(T1 + 2944, 16)
        rstdA = f32v(T1 + 2960, 16)
        qkA_bf = bfv(TMP_OFF + 2560, 320)
        qkB_bf = bfv(T1 + 1920 + 0, 0) if False else None

        P.op("pool", lambda e: e.memset(VA[:, :, :, 0:64], 1.0), writes=["VAones0"])
        P.op("pool", lambda e: e.memset(VA[:, :, :, 128:192], 1.0), writes=["VAones1"])

        def load_rope(tc, slot):
            P.op("sp", lambda e: e.dma_start(out=ropeT[:, slot, 0], in_=ropeA_d[tc * 128:(tc + 1) * 128]),
                 writes=[("ropeA", slot)], dma=True)
            P.op("sp", lambda e: e.dma_start(out=ropeT[:, slot, 1], in_=ropeB_d[tc * 128:(tc + 1) * 128]),
                 writes=[("ropeB", slot)], dma=True)

        qkB_bf = bfv(TMP_OFF + 2048, 512)

        bank_rr = [0]

        def next_bank():
            b = bank_rr[0] % 8
            bank_rr[0] += 1
            return b

        for tc in range(NT):
            slot = tc % 2
            load_rope(tc, slot)
            banks = [next_bank() for _ in range(5)]
            for ci, (c0, c1) in enumerate(col_tiles):
                b = banks[ci]
                n = c1 - c0
                for dc in range(8):
                    P.op("pe", lambda e, b=b, n=n, dc=dc, c0=c0, c1=c1, tc=tc: e.matmul(
                        ps[b][:, 0:n], lhsT=XT[:, dc, tc * 128:(tc + 1) * 128], rhs=wi[:, dc, c0:c1],
                        start=(dc == 0), stop=(dc == 7)),
                        reads=XTk([tc]) + [("wi", ci)], writes=[("ps", b)])
            bA, bKV, bQ, bK, bV = banks
            P.op("act", lambda e, bA=bA: e.activation(out=sqA[:, 0:512], in_=ps[bA][:, 0:512], func=AF.Square),
                 reads=[("ps", bA)], writes=["sqA_q"])
            P.op("act", lambda e, bKV=bKV: e.activation(out=sqA[:, 512:640], in_=ps[bKV][:, 0:128], func=AF.Square),
                 reads=[("ps", bKV)], writes=["sqA_k"])
            P.op("dve", lambda e: e.reduce_sum(out=ssA[:, 0:10], in_=sqA.rearrange("p (h d) -> p h d", h=10), axis=AX.X),
                 reads=["sqA_q", "sqA_k"], writes=["ssA"])
            P.op("act", lambda e: e.activation(out=rstdA[:, 0:10], in_=ssA[:, 0:10], func=AF.Ln, scale=1.0 / 64.0, bias=rmseps_col),
                 reads=["ssA", "eps2"], writes=["rstdA"])
            P.op("act", lambda e: e.activation(out=rstdA[:, 0:10], in_=rstdA[:, 0:10], func=AF.Exp, scale=-0.5),
                 reads=["rstdA"], writes=["rstdA"])
            Ctab = ropeT[:, slot, 0, 0, :]
            Stab = ropeT[:, slot, 0, 1, :]
            gtab = f32v(T1 + 3040, 0) if False else None
            for (src_b, c_lo, nh, dst_lo, gsb, gkey) in ((bA, 0, 8, 0, gq_sb, "gq"), (bKV, 0, 2, 512, gk_sb, "gk")):
                xv = ps[src_b][:, c_lo:c_lo + nh * 64].rearrange("p (h d) -> p h d", h=nh)
                xg = tA1[:, dst_lo:dst_lo + nh * 64].rearrange("p (h d) -> p h d", h=nh)
                P.op("dve", lambda e, xv=xv, xg=xg, gsb=gsb, nh=nh: e.tensor_tensor(
                    out=xg, in0=xv, in1=gsb.unsqueeze(1).to_broadcast([128, nh, 64]), op=ALU.mult),
                    reads=[("ps", src_b), gkey], writes=[("tA1", dst_lo)])
                xg4 = tA1[:, dst_lo:dst_lo + nh * 64].rearrange("p (h a b d) -> p h a b d", h=nh, a=2, b=2)
                t24 = tA2[:, dst_lo:dst_lo + nh * 64].rearrange("p (h a b d) -> p h a b d", h=nh, a=2, b=2)
                S4 = Stab.rearrange("p (a b d) -> p a b d", a=2, b=2)
                for bb in range(2):
                    P.op("dve", lambda e, bb=bb, xg4=xg4, t24=t24, S4=S4, nh=nh: e.tensor_tensor(
                        out=t24[:, :, :, bb, :], in0=xg4[:, :, :, 1 - bb, :],
                        in1=S4[:, :, bb, :].unsqueeze(1).to_broadcast([128, nh, 2, 16]), op=ALU.mult),
                        reads=[("tA1", dst_lo), ("ropeA", slot)], writes=[("tA2", dst_lo, bb)])
                P.op("dve", lambda e, xg=xg, nh=nh, Ctab=Ctab: e.tensor_tensor(
                    out=xg, in0=xg, in1=Ctab.unsqueeze(1).to_broadcast([128, nh, 64]), op=ALU.mult),
                    reads=[("tA1", dst_lo), ("ropeA", slot), ("tA2", dst_lo, 0), ("tA2", dst_lo, 1)], writes=[("tA1", dst_lo)])
                t2v = tA2[:, dst_lo:dst_lo + nh * 64].rearrange("p (h d) -> p h d", h=nh)
                P.op("dve", lambda e, xg=xg, t2v=t2v: e.tensor_tensor(out=xg, in0=xg, in1=t2v, op=ALU.add),
                     reads=[("tA1", dst_lo), ("tA2", dst_lo, 0), ("tA2", dst_lo, 1)], writes=[("tA1", dst_lo)])
            qk3 = qkA_bf.rearrange("p (j g d) -> p j g d", j=5, g=2)
            for h in range(10):
                if h < 8:
                    j, g = h % 4, h // 4
                else:
                    j, g = 4, h - 8
                P.op("act", lambda e, h=h, j=j, g=g: e.activation(
                    out=qk3[:, j, g, :], in_=tA1[:, h * 64:(h + 1) * 64], func=AF.Copy, scale=rstdA[:, h:h + 1]),
                    reads=[("tA1", 0), ("tA1", 512), "rstdA"], writes=[("qkA", h)])
            bT = next_bank()
            for j in range(5):
                P.op("pe", lambda e, j=j, bT=bT: e.transpose(psb[bT][:, j * 128:(j + 1) * 128],
                                                             qkA_bf[:, j * 128:(j + 1) * 128], identB),
                     reads=[("qkA", h) for h in range(10)] + ["identB"], writes=[("ps", bT)])
            P.op("act", lambda e, bT=bT, tc=tc: e.activation(
                out=QTA[:, :, tc * 128:(tc + 1) * 128],
                in_=psb[bT][:, 0:512].rearrange("p (j t) -> p j t", j=4), func=AF.Copy),
                reads=[("ps", bT)], writes=[("QTA", tc)])
            P.op("act", lambda e, bT=bT, tc=tc: e.activation(
                out=KTA[:, tc * 128:(tc + 1) * 128], in_=psb[bT][:, 512:640], func=AF.Copy),
                reads=[("ps", bT)], writes=[("KTA", tc)])
            P.op("act", lambda e, bKV=bKV, tc=tc: e.activation(
                out=VA[:, tc, :, 64:128], in_=ps[bKV][:, 128:256].rearrange("p (k d) -> p k d", k=2), func=AF.Copy),
                reads=[("ps", bKV)], writes=[("VA", tc)])
            CB = ropeT[:, slot, 1, 0, :]
            SB = ropeT[:, slot, 1, 1, :]
            for qi, bsrc in enumerate((bQ, bK)):
                xv = ps[bsrc][:, 0:512].rearrange("p (h d) -> p h d", h=8)
                xv4 = ps[bsrc][:, 0:512].rearrange("p (h b d) -> p h b d", h=8, b=2)
                t1v = tB1.rearrange("p (h d) -> p h d", h=8)
                t24 = tB2.rearrange("p (h b d) -> p h b d", h=8, b=2)
                S3 = SB.rearrange("p (b d) -> p b d", b=2)
                P.op("dve", lambda e, xv=xv, t1v=t1v, CB=CB: e.tensor_tensor(
                    out=t1v, in0=xv, in1=CB.unsqueeze(1).to_broadcast([128, 8, 64]), op=ALU.mult),
                    reads=[("ps", bsrc), ("ropeB", slot)], writes=["tB1"])
                for bb in range(2):
                    P.op("dve", lambda e, bb=bb, xv4=xv4, t24=t24, S3=S3: e.tensor_tensor(
                        out=t24[:, :, bb, :], in0=xv4[:, :, 1 - bb, :],
                        in1=S3[:, bb, :].unsqueeze(1).to_broadcast([128, 8, 32]), op=ALU.mult),
                        reads=[("ps", bsrc), ("ropeB", slot)], writes=[("tB2", bb)])
                dstb = qkB_bf[:, qi * 512:(qi + 1) * 512]
                P.op("dve", lambda e, dstb=dstb: e.tensor_tensor(out=dstb, in0=tB1, in1=tB2, op=ALU.add),
                     reads=["tB1", ("tB2", 0), ("tB2", 1)], writes=[("qkB", qi)])
            bT2 = next_bank()
            for j in range(8):
                P.op("pe", lambda e, j=j, bT2=bT2: e.transpose(psb[bT2][:, j * 128:(j + 1) * 128],
                                                               qkB_bf[:, j * 128:(j + 1) * 128], identB),
                     reads=[("qkB", 0), ("qkB", 1), "identB"], writes=[("ps", bT2)])
            P.op("act", lambda e, bT2=bT2, tc=tc: e.activation(
                out=QKTB[:, :, tc * 128:(tc + 1) * 128],
                in_=psb[bT2][:].rearrange("p (j t) -> p j t", j=8), func=AF.Copy),
                reads=[("ps", bT2)], writes=[("QKTB", tc)])
            P.op("act", lambda e, bV=bV, tc=tc: e.activation(out=VB[:, tc, :], in_=ps[bV][:, 0:512], func=AF.Copy),
                 reads=[("ps", bV)], writes=[("VB", tc)])

        if stage == "p1b":
            def dump(view, off, n, keys):
                P.op("pool", lambda e: e.dma_start(out=dbg_d[:, off:off + n], in_=view), reads=keys, writes=[("dbgout", off)], dma=True)
            dump(bfv(R_OFF, 4096), 0, 8192, [("QTA", t) for t in range(NT)])
            dump(KTA, 8192, 2048, [("KTA", t) for t in range(NT)])
            dump(bfv(R_OFF + 5120, 8192), 10240, 16384, [("QKTB", t) for t in range(NT)])
            dump(bfv(R_OFF + 13312, 3072), 26624, 6144, [("VA", t) for t in range(NT)] + ["VAones0", "VAones1"])
            dump(bfv(HT_OFF, 4096), 32768, 8192, [("VB", t) for t in range(NT)])
            P.wait_tokens("sp", [P.writer[("dbgout", o)] for o in (0, 8192, 10240, 26624, 32768)])
            with nc.Block() as block:
                P.emit(block, nc, esem, dsem)
            return nc


        def finish(final_toks):
            P.wait_tokens("sp", final_toks)
            print("milestones", {e: len(P.milestones[e]) for e in P.ENG}, "ops", P.nops, "dma", P.dma_n)
            with nc.Block() as block:
                P.emit(block, nc, esem, dsem)
            return nc

        bst = f32v(T1, 2048).rearrange("p (a d) -> p a d", a=2)
        P.alias(["bst"], ["sqA_q", "sqA_k", "ssA", "rstdA", ("tA1", 0), ("tA1", 512), ("tA2", 0, 0), ("tA2", 0, 1),
                          ("tA2", 512, 0), ("tA2", 512, 1), "tB1", ("tB2", 0), ("tB2", 1)])
        P.op("sp", lambda e: e.dma_start(out=bst[0:32, 0, :], in_=beg_d), writes=["bst"], dma=True)
        P.op("sp", lambda e: e.dma_start(out=bst[0:32, 1, :], in_=beu_d), reads=["bst"], writes=["bst2"], dma=True)
        for a, (dstT, sc1, sc2) in enumerate(((bgT, -1.0, 7.0), (buT, 1.0, 7.0))):
            b = next_bank()
            for fc in range(8):
                P.op("pe", lambda e, b=b, fc=fc, a=a: e.transpose(ps[b][:, fc * 32:(fc + 1) * 32],
                                                                  bst[0:32, a, fc * 128:(fc + 1) * 128], identF[0:32, 0:32]),
                     reads=["bst", "bst2", "identF"], writes=[("ps", b)])
            P.op("dve", lambda e, b=b, dstT=dstT, sc1=sc1, sc2=sc2: e.tensor_scalar(
                out=dstT, in0=ps[b][:, 0:256].rearrange("p (f e) -> p e f", f=8), scalar1=sc1, scalar2=sc2,
                op0=ALU.mult, op1=ALU.add), reads=[("ps", b)], writes=[("bT", a)])
        wkv = bfv(XT_OFF, 8192).rearrange("p (c n) -> p c n", c=8)
        P.alias(["wkv"], XTk(range(NT)))
        P.op("pool", lambda e: e.dma_start(out=wkv, in_=wkv_d.rearrange("(c p) n -> p c n", p=128)), writes=["wkv"], dma=True)

        lamv = f32v(LNP_OFF + 512, 256).rearrange("p (a b d) -> p a b d", a=2, b=2)
        P.op("sp", lambda e: e.dma_start(out=lamv, in_=lam_d.rearrange("(a b) d -> a b d", a=2).partition_broadcast(128)),
             writes=["lamv"], dma=True)
        P.op("sp", lambda e: e.dma_start(out=gs_col, in_=subln_d), writes=["gs"], dma=True)
        P.op("dve", lambda e: e.tensor_tensor(out=lamv[:, :, 0, :], in0=lamv[:, :, 0, :], in1=lamv[:, :, 1, :], op=ALU.mult),
             reads=["lamv"], writes=["lamv"])
        P.op("dve", lambda e: e.reduce_sum(out=lsum[:, 0:2], in_=lamv[:, :, 0, :], axis=AX.X), reads=["lamv"], writes=["lsum"])
        P.op("act", lambda e: e.activation(out=lsum[:, 2:4], in_=lsum[:, 0:2], func=AF.Exp), reads=["lsum"], writes=["lsum2"])
        P.op("dve", lambda e: e.tensor_tensor(out=lam_col, in0=lsum[:, 2:3], in1=lsum[:, 3:4], op=ALU.subtract),
             reads=["lsum2"], writes=["lam"])
        P.op("dve", lambda e: e.tensor_scalar(out=nlam_col, in0=lam_col, scalar1=LAMBDA_INIT, scalar2=-1.0, op0=ALU.add, op1=ALU.mult),
             reads=["lam"], writes=["nlam"])
        P.op("dve", lambda e: e.tensor_scalar(out=gs_col, in0=gs_col, scalar1=1.0 - LAMBDA_INIT, scalar2=None, op0=ALU.mult),
             reads=["gs"], writes=["gs"])

        wmix = bfv(HT_OFF + 4096, 4096).rearrange("p (c n) -> p c n", c=8)
        P.op("pool", lambda e: e.dma_start(out=wmix, in_=wmix_d.rearrange("(c p) n -> p c n", p=128)), writes=["wmix"], dma=True)

        for g8 in range(NSLOT // 2560):
            P.op("sp", lambda e, g8=g8: e.dma_start(out=yg_d[g8 * 2560:(g8 + 1) * 2560, :], in_=zeros_d[:, :]),
                 writes=[("YGZ", g8)], dma=True)
        catT = bfv(WR_OFF, 8192).rearrange("p (c t) -> p c t", c=8)
        wi_keys = [("wi", i) for i in range(5)]
        t1_keys = ["sqA_q", "sqA_k", "ssA", "rstdA", ("tA1", 0), ("tA1", 512), ("tA2", 0, 0), ("tA2", 0, 1),
                   ("tA2", 512, 0), ("tA2", 512, 1), "tB1", ("tB2", 0), ("tB2", 1)]
        cat_keys = [("catT", c, qt) for c in range(8) for qt in range(4)]
        P.alias(cat_keys, wi_keys)
        denA = [f32v(T1 + i * 512, 512) for i in range(2)]
        sq_bf = bfv(T1 + 1024, 256)
        rstdB = f32v(T1 + 1280, 512)
        P.alias([("denA", 0), ("denA", 1), "sq_bf", "rstdB"], t1_keys)
        Bt = [f32v(TMP_OFF + i * 512, 512) for i in range(4)]
        P.alias([("Bt", i) for i in range(4)], [("xin", 0), ("xin", 1)])
        P.alias([("E", i) for i in range(4)], [("qkB", 0), ("qkB", 1)] + [("qkA", h) for h in range(10)])
        allT = list(range(NT))
        stepsA = [(j, qt, sc) for j in range(4) for qt in range(4) for sc in range(NT)]

        def A_S(i):
            j, qt, sc = stepsA[i]
            qs = slice(qt * 512, (qt + 1) * 512)
            pair = i % 2
            for g in range(2):
                sb = 2 * pair + g
                kp = slice(g * 64, (g + 1) * 64)
                P.op("pe", lambda e, sb=sb, kp=kp: e.matmul(ps[sb][:, :], lhsT=KTA[kp, sc * 128:(sc + 1) * 128], rhs=QTA[kp, j, qs],
                                                            start=True, stop=True),
                     reads=[("KTA", sc)] + [("QTA", t) for t in range(qt * 4, qt * 4 + 4)], writes=[("ps", sb)])
            for g in range(2):
                sb = 2 * pair + g
                P.op("act", lambda e, sb=sb: e.activation(out=Ering[sb], in_=ps[sb][:, :], func=AF.Exp, scale=0.125),
                     reads=[("ps", sb)], writes=[("E", sb)])

        def A_PV(i):
            j, qt, sc = stepsA[i]
            qs = slice(qt * 512, (qt + 1) * 512)
            pair = i % 2
            grp = i // NT
            odd = j % 2
            for g in range(2):
                h = j + 4 * g
                c = h // 2
                es = 2 * pair + g
                ob = 4 + 2 * (grp % 2) + g
                vsl = VA[:, sc, g, 0:128] if odd else VA[:, sc, g, 64:192]
                P.op("pe", lambda e, ob=ob, vsl=vsl, es=es: e.matmul(ps[ob][:, :], lhsT=vsl, rhs=Ering[es], start=(sc == 0), stop=(sc == NT - 1)),
                     reads=[("VA", sc), "VAones0", "VAones1", ("E", es)], writes=[("ps", ob)])
            if sc == NT - 1:
                op_ = slice(odd * 64, odd * 64 + 64)
                dp_ = slice((1 - odd) * 64, (1 - odd) * 64 + 64)
                for g in range(2):
                    h = j + 4 * g
                    c = h // 2
                    ob = 4 + 2 * (grp % 2) + g
                    ds = g
                    P.op("act", lambda e, ob=ob, ds=ds: e.activation(out=denA[ds][op_, :], in_=ps[ob][dp_, :], func=AF.Copy),
                         reads=[("ps", ob)], writes=[("denA", ds)])
                    P.op("dve", lambda e, ds=ds: e.reciprocal(out=denA[ds][op_, :], in_=denA[ds][op_, :]),
                         reads=[("denA", ds)], writes=[("denA", ds)])
                    P.op("dve", lambda e, ob=ob, ds=ds, c=c: e.tensor_tensor(out=catT[op_, c, qs], in0=ps[ob][op_, :], in1=denA[ds][op_, :], op=ALU.mult),
                         reads=[("ps", ob), ("denA", ds)], writes=[("catT", c, qt)])

        for i in range(-1, len(stepsA)):
            if i + 1 < len(stepsA):
                A_S(i + 1)
            if i >= 0:
                A_PV(i)

        stepsB = [(h, qt, sc) for h in range(4) for qt in range(4) for sc in range(NT)]

        def B_S(i):
            h, qt, sc = stepsB[i]
            qs = slice(qt * 512, (qt + 1) * 512)
            pair = i % 2
            s1, s2 = 2 * pair, 2 * pair + 1
            rq = [("QKTB", t) for t in range(qt * 4, qt * 4 + 4)] + [("QKTB", sc)]
            P.op("pe", lambda e: e.matmul(ps[s1][:, :], lhsT=QKTB[0:64, 4 + h, sc * 128:(sc + 1) * 128], rhs=QKTB[0:64, h, qs], start=True, stop=True),
                 reads=rq, writes=[("ps", s1)])
            P.op("pe", lambda e: e.matmul(ps[s2][:, :], lhsT=QKTB[64:128, 4 + h, sc * 128:(sc + 1) * 128], rhs=QKTB[64:128, h, qs], start=True, stop=True),
                 reads=rq, writes=[("ps", s2)])
            P.op("act", lambda e: e.activation(out=Ering[s1], in_=ps[s1][:, :], func=AF.Exp, scale=0.125),
                 reads=[("ps", s1)], writes=[("E", s1)])
            P.op("act", lambda e: e.activation(out=Ering[s2], in_=ps[s2][:, :], func=AF.Exp, scale=0.125),
                 reads=[("ps", s2)], writes=[("E", s2)])

        def B_PV(i):
            h, qt, sc = stepsB[i]
            qs = slice(qt * 512, (qt + 1) * 512)
            pair = i % 2
            e1, e2 = 2 * pair, 2 * pair + 1
            vsl = VB[:, sc, h * 128:(h + 1) * 128]
            st_, sp_ = (sc == 0), (sc == NT - 1)
            P.op("pe", lambda e: e.matmul(ps[4][:, :], lhsT=vsl, rhs=Ering[e1], start=st_, stop=sp_),
                 reads=[("VB", sc), ("E", e1)], writes=[("ps", 4)])
            P.op("pe", lambda e: e.matmul(ps[6][:, :], lhsT=onesB, rhs=Ering[e1], start=st_, stop=sp_),
                 reads=["onesB", ("E", e1)], writes=[("ps", 6)])
            P.op("pe", lambda e: e.matmul(ps[5][:, :], lhsT=vsl, rhs=Ering[e2], start=st_, stop=sp_),
                 reads=[("VB", sc), ("E", e2)], writes=[("ps", 5)])
            P.op("pe", lambda e: e.matmul(ps[7][:, :], lhsT=onesB, rhs=Ering[e2], start=st_, stop=sp_),
                 reads=["onesB", ("E", e2)], writes=[("ps", 7)])
            if sc == NT - 1:
                P.op("dve", lambda e: e.reciprocal(out=Bt[0], in_=ps[6][:, :]), reads=[("ps", 6)], writes=[("Bt", 0)])
                P.op("dve", lambda e: e.tensor_tensor(out=Bt[2], in0=ps[4][:, :], in1=Bt[0], op=ALU.mult),
                     reads=[("ps", 4), ("Bt", 0)], writes=[("Bt", 2)])
                P.op("dve", lambda e: e.reciprocal(out=Bt[1], in_=ps[7][:, :]), reads=[("ps", 7)], writes=[("Bt", 1)])
                P.op("dve", lambda e: e.scalar_tensor_tensor(out=Bt[3], in0=ps[5][:, :], scalar=nlam_col, in1=Bt[1], op0=ALU.mult, op1=ALU.mult),
                     reads=[("ps", 5), ("Bt", 1), "nlam"], writes=[("Bt", 3)])
                P.op("dve", lambda e: e.tensor_tensor(out=Bt[2], in0=Bt[2], in1=Bt[3], op=ALU.add),
                     reads=[("Bt", 2), ("Bt", 3)], writes=[("Bt", 2)])
                P.op("dve", lambda e: e.tensor_tensor(out=sq_bf, in0=Bt[2], in1=Bt[2], op=ALU.mult),
                     reads=[("Bt", 2)], writes=["sq_bf"])
                P.op("pe", lambda e: e.matmul(ps[6][:, :], lhsT=onesB, rhs=sq_bf, start=True, stop=True),
                     reads=["onesB", "sq_bf"], writes=[("ps", 6)])
                P.op("act", lambda e: e.activation(out=rstdB, in_=ps[6][:, :], func=AF.Ln, scale=1.0 / 128.0, bias=rmseps_col),
                     reads=[("ps", 6), "eps2"], writes=["rstdB"])
                P.op("act", lambda e: e.activation(out=rstdB, in_=rstdB, func=AF.Exp, scale=-0.5), reads=["rstdB"], writes=["rstdB"])
                P.op("dve", lambda e: e.scalar_tensor_tensor(
                    out=catT[:, 4 + h, qs], in0=Bt[2], scalar=gs_col, in1=rstdB, op0=ALU.mult, op1=ALU.mult),
                    reads=[("Bt", 2), "rstdB", "gs"], writes=[("catT", 4 + h, qt)])

        for i in range(-1, len(stepsB)):
            if i + 1 < len(stepsB):
                B_S(i + 1)
            if i >= 0:
                B_PV(i)

        if stage == "p1d":
            P.op("pool", lambda e: e.dma_start(out=dbg_d[:, 0:16384], in_=bfv(WR_OFF, 8192)), reads=cat_keys, writes=["dbgout"], dma=True)
            return finish([P.writer["dbgout"]])

        mem_st = f32v(TMP_OFF, 2048).rearrange("p (c d) -> p c d", c=2)
        P.alias(["mem_st"], [("Bt", i) for i in range(4)])
        P.op("sp", lambda e: e.dma_start(out=mem_st, in_=mem_d.rearrange("(c p) d -> p c d", p=128)), writes=["mem_st"], dma=True)
        KmT = bfv(HT_OFF, 1024).rearrange("p (h j m) -> p h j m", h=4, j=2)
        Vm = bfv(HT_OFF + 1024, 1024).rearrange("p (c n) -> p c n", c=2)
        memT = bfv(HT_OFF + 2048, 1024).rearrange("p (c m) -> p c m", c=8)
        P.alias(["KmT", "Vm", "memT"], [("VB", t) for t in allT])
        for mc in range(2):
            for half in range(2):
                b = next_bank()
                for jj in range(4):
                    dc = half * 4 + jj
                    P.op("pe", lambda e, b=b, jj=jj, dc=dc, mc=mc: e.transpose(
                        ps[b][:, jj * 128:(jj + 1) * 128], mem_st[:, mc, dc * 128:(dc + 1) * 128], identF),
                        reads=["mem_st", "identF"], writes=[("ps", b)])
                P.op("act", lambda e, b=b, half=half, mc=mc: e.activation(
                    out=memT[:, half * 4:(half + 1) * 4, mc * 128:(mc + 1) * 128],
                    in_=ps[b][:].rearrange("p (j t) -> p j t", j=4), func=AF.Copy),
                    reads=[("ps", b)], writes=[("memT", mc, half)])
        memT_keys = [("memT", mc, half) for mc in range(2) for half in range(2)]
        for h in range(4):
            for j in range(2):
                b = next_bank()
                for dc in range(8):
                    P.op("pe", lambda e, b=b, dc=dc, h=h, j=j: e.matmul(
                        ps[b][:, 0:256], lhsT=wkv[:, dc, h * 256 + j * 128:h * 256 + (j + 1) * 128], rhs=memT[:, dc, :],
                        start=(dc == 0), stop=(dc == 7)), reads=["wkv"] + memT_keys, writes=[("ps", b)])
                P.op("act", lambda e, b=b, h=h, j=j: e.activation(out=KmT[:, h, j, :], in_=ps[b][:, 0:256], func=AF.Copy),
                     reads=[("ps", b)], writes=[("KmT", h, j)])
        for mc in range(2):
            for half in range(2):
                b = next_bank()
                for dc in range(8):
                    P.op("pe", lambda e, b=b, dc=dc, mc=mc, half=half: e.matmul(
                        ps[b][:, :], lhsT=memT[:, dc, mc * 128:(mc + 1) * 128], rhs=wkv[:, dc, 1024 + half * 512:1024 + (half + 1) * 512],
                        start=(dc == 0), stop=(dc == 7)), reads=["wkv"] + memT_keys, writes=[("ps", b)])
                P.op("dve", lambda e, b=b, mc=mc, half=half: e.tensor_copy(out=Vm[:, mc, half * 512:(half + 1) * 512], in_=ps[b][:, :]),
                     reads=[("ps", b)], writes=[("Vm", mc, half)])
        KmT_keys = [("KmT", h, j) for h in range(4) for j in range(2)]
        Vm_keys = [("Vm", mc, half) for mc in range(2) for half in range(2)]
        P.alias(XTk(range(NT)), ["wkv"])

        P.alias(["lnp"], [("ropeA", 0), ("ropeA", 1), ("ropeB", 0), ("ropeB", 1), "lamv"])
        P.op("sp", lambda e: e.dma_start(out=lnp, in_=ln_d[0:2, :].partition_broadcast(128)), writes=["lnp"], dma=True)
        att_keys = ([("QTA", t) for t in allT] + [("KTA", t) for t in allT] + [("QKTB", t) for t in allT] +
                    [("VA", t) for t in allT] + ["VAones0", "VAones1"])
        P.alias([("R", t) for t in allT], att_keys)
        ytile = [f32v(T1 + i * 1024, 1024) for i in range(2)]
        xn = f32v(T1 + 2048, 1024)
        P.alias([("y", 0), ("y", 1), "xn"], [("denA", 0), ("denA", 1), "sq_bf", "rstdB"])
        P.alias([("xin", 0), ("xin", 1)], [("Bt", i) for i in range(4)] + ["mem_st"])
        stats = small[:, 16:28].rearrange("p (c s) -> p c s", c=2)
        mv = small[:, 28:30]
        rstd1 = small[:, 30:31]
        nmr1 = small[:, 31:32]

        small2 = f32v(misc(64), 64)

        def ln_stats(ysrc, ykey, par):
            st_ = small2[:, par * 16:par * 16 + 12].rearrange("p (c s) -> p c s", c=2)
            mv_ = small2[:, par * 16 + 12:par * 16 + 14]
            rs_ = small2[:, par * 16 + 14:par * 16 + 15]
            nm_ = small2[:, par * 16 + 15:par * 16 + 16]
            P.op("dve", lambda e: e.bn_stats(out=st_[:, 0, :], in_=ysrc[:, 0:512]), reads=[ykey], writes=[("stats0", par)])
            P.op("dve", lambda e: e.bn_stats(out=st_[:, 1, :], in_=ysrc[:, 512:1024]), reads=[ykey], writes=[("stats1", par)])
            P.op("dve", lambda e: e.bn_aggr(out=mv_, in_=st_), reads=[("stats0", par), ("stats1", par)], writes=[("mv", par)])
            P.op("act", lambda e: e.activation(out=rs_, in_=mv_[:, 1:2], func=AF.Ln, bias=eps_col), reads=[("mv", par), "eps"], writes=[("rstd1", par)])
            P.op("act", lambda e: e.activation(out=rs_, in_=rs_, func=AF.Exp, scale=-0.5), reads=[("rstd1", par)], writes=[("rstd1", par)])
            P.op("dve", lambda e: e.scalar_tensor_tensor(out=nm_, in0=mv_[:, 0:1], scalar=-1.0, in1=rs_, op0=ALU.mult, op1=ALU.mult),
                 reads=[("mv", par), ("rstd1", par)], writes=[("nmr1", par)])

        def ln_apply(ysrc, ykey, par, dst, dst_keys, lnkey, xnb, xnk):
            rs_ = small2[:, par * 16 + 14:par * 16 + 15]
            nm_ = small2[:, par * 16 + 15:par * 16 + 16]
            P.op("act", lambda e: e.activation(out=xnb, in_=ysrc, func=AF.Identity, scale=rs_, bias=nm_),
                 reads=[ykey, ("rstd1", par), ("nmr1", par)], writes=[xnk])
            P.op("dve", lambda e: e.tensor_tensor(out=xnb, in0=xnb, in1=lnp[:, 0, :], op=ALU.mult), reads=[xnk, lnkey], writes=[xnk])
            P.op("dve", lambda e: e.tensor_tensor(out=dst, in0=xnb, in1=lnp[:, 1, :], op=ALU.add), reads=[xnk, lnkey], writes=dst_keys)

        def skewed(n, stages):
            ns = len(stages)
            for t in range(n + ns - 1):
                for si in range(ns - 1, -1, -1):
                    k = t - si
                    if 0 <= k < n:
                        stages[si](k)

        xnA = [xn, f32v(TMP_OFF + 2048, 1024)]
        P.alias([("xnA", 0), ("xnA", 1)], ["xn"] + [("E", i) for i in range(4)])

        def l1_s0(tc):
            par = tc % 2
            load_x(tc, par)
            for dh in range(2):
                b = par * 2 + dh
                for c in range(8):
                    P.op("pe", lambda e, b=b, c=c, dh=dh: e.matmul(
                        ps[b][:, :], lhsT=catT[:, c, tc * 128:(tc + 1) * 128], rhs=wmix[:, c, dh * 512:(dh + 1) * 512],
                        start=(c == 0), stop=(c == 7)),
                        reads=[("catT", c, tc // 4), "wmix"], writes=[("ps", b)])
                P.op("dve", lambda e, b=b, dh=dh: e.scalar_tensor_tensor(
                    out=ytile[par][:, dh * 512:(dh + 1) * 512], in0=xin[par][:, dh * 512:(dh + 1) * 512], scalar=ALPHA,
                    in1=ps[b][:, :], op0=ALU.mult, op1=ALU.add),
                    reads=[("xin", par), ("ps", b)], writes=[("y", par, dh)])

        def l1_s1(tc):
            par = tc % 2
            P.op("dve", lambda e: e.engine_nop(), reads=[("y", par, 0), ("y", par, 1)], writes=[("y", par)])
            ln_stats(ytile[par], ("y", par), par)

        def l1_s2(tc):
            par = tc % 2
            ln_apply(ytile[par], ("y", par), par, Rt[:, tc, :], [("R", tc)], "lnp", xnA[par], ("xnA", par))
            P.alias([("y", par, 0), ("y", par, 1)], [("y", par)])

        def l1_s3(tc):
            par = tc % 2
            transpose_tile_to_XT(Rt[:, tc, :], ("R", tc), tc, (4 + par * 2, 5 + par * 2))

        skewed(NT, [l1_s0, l1_s1, l1_s2, l1_s3])

        if stage == "p1":
            toks = []
            for tc in range(NT):
                toks.append(P.op("sp", lambda e, tc=tc: e.dma_start(out=out_d[tc * 128:(tc + 1) * 128, :], in_=Rt[:, tc, :]),
                                 reads=[("R", tc)], writes=[("out", tc)], dma=True))
            return finish(toks)


        wq = bfv(WR_OFF, 4096).rearrange("p (c n) -> p c n", c=8)
        wo = bfv(WR_OFF + 4096, 4096).rearrange("p (c n) -> p c n", c=8)
        P.alias(["wq", "wo"], cat_keys)
        P.op("pool", lambda e: e.dma_start(out=wq, in_=wq_d.rearrange("(c p) n -> p c n", p=128)), writes=["wq"], dma=True)
        P.op("pool", lambda e: e.dma_start(out=wo, in_=wo_d.rearrange("(c p) n -> p c n", p=128)), writes=["wo"], dma=True)
        qT_tile = bfv(WR_OFF + 8192, 2048).rearrange("p (c t) -> p c t", c=8)
        oT_tile = bfv(WR_OFF + 10240, 2048).rearrange("p (c t) -> p c t", c=8)
        P.alias([("qT", c) for c in range(8)] + [("oT", c) for c in range(8)], [("y", 0), ("y", 1), ("y", 0, 0), ("y", 0, 1), ("y", 1, 0), ("y", 1, 1), "xn"])
        bd_sb = f32v(HT_OFF + 2048, 1024)
        y2 = f32v(HT_OFF + 3072, 1024)
        xn2 = f32v(HT_OFF + 4096, 1024)
        x2t = f32v(HT_OFF + 5120, 1024)
        x2Tf = f32v(HT_OFF + 6144, 1024).rearrange("p (c t) -> p c t", c=8)
        CTc = f32v(HT_OFF + 7168, 128)
        gsm = f32v(HT_OFF + 7296, 128)
        gsm2 = f32v(HT_OFF + 7424, 256)
        idx8 = gsm2[:, 0:8].bitcast(U32)
        idxf = gsm2[:, 8:12]
        posk = gsm2[:, 12:16]
        destf = gsm2[:, 16:20]
        ovf = gsm2[:, 20:24]
        gk4 = gsm2[:, 24:28]
        msk_bf = gsm2[:, 32:48].bitcast(BF16)
        posf = gsm2[:, 48:80]
        sel4 = gsm2[:, 96:224].rearrange("p (k e) -> p k e", k=4)
        lg = gsm[:, 0:32]
        top8 = gsm[:, 32:40]
        negm = gsm[:, 40:41]
        ssum = gsm[:, 41:42]
        msk = gsm[:, 48:80]
        exg = gsm[:, 80:112]
        P.alias(["bd", "y2", "xn2", "x2t", "x2Tf", "CTc", "gsm", "idx8", "idxf", "posk", "destf", "ovf", "gk4", "msk_bf", "posf", "sel4"], ["memT", "wmix"] + memT_keys)
        P.op("sp", lambda e: e.dma_start(out=wr_sb, in_=wr_d.rearrange("(c p) n -> p c n", p=128)), writes=["wr"], dma=True)
        P.op("sp", lambda e: e.dma_start(out=br_sb, in_=br_d.partition_broadcast(128)), writes=["br"], dma=True)
        P.op("sp", lambda e: e.dma_start(out=lnp, in_=ln_d[2:4, :].partition_broadcast(128)), writes=["lnp"], dma=True)
        dent = [f32v(TMP_OFF + i * 512, 512) for i in range(2)]
        P.alias([("dent", 0), ("dent", 1)], [("xin", 0), ("xin", 1), "mem_st"])
        e_rr2 = [0]

        xb_rr = [0]

        def xbank():
            v = xb_rr[0] % 6
            xb_rr[0] += 1
            return v

        def xattn_tile(tt):
            ts_ = slice(tt * 512, (tt + 1) * 512)
            tcs = list(range(tt * 4, tt * 4 + 4))
            for c in range(8):
                b = xbank()
                for dc in range(8):
                    P.op("pe", lambda e, b=b, dc=dc, c=c, ts_=ts_: e.matmul(
                        ps[b][:, :], lhsT=wq[:, dc, c * 128:(c + 1) * 128], rhs=XT[:, dc, ts_], start=(dc == 0), stop=(dc == 7)),
                        reads=["wq"] + XTk(tcs), writes=[("ps", b)])
                if c % 2 == 0:
                    P.op("act", lambda e, b=b, c=c: e.activation(out=qT_tile[:, c, :], in_=ps[b][:, :], func=AF.Copy),
                         reads=[("ps", b)], writes=[("qT", c)])
                else:
                    P.op("dve", lambda e, b=b, c=c: e.tensor_copy(out=qT_tile[:, c, :], in_=ps[b][:, :]),
                         reads=[("ps", b)], writes=[("qT", c)])
            for h in range(4):
                eslots = []
                for mc in range(2):
                    sb = xbank()
                    for j in range(2):
                        P.op("pe", lambda e, sb=sb, h=h, j=j, mc=mc: e.matmul(
                            ps[sb][:, :], lhsT=KmT[:, h, j, mc * 128:(mc + 1) * 128], rhs=qT_tile[:, h * 2 + j, :],
                            start=(j == 0), stop=(j == 1)), reads=KmT_keys + [("qT", h * 2 + j)], writes=[("ps", sb)])
                    es = e_rr2[0] % 4
                    e_rr2[0] += 1
                    eslots.append(es)
                    P.op("act", lambda e, sb=sb, es=es: e.activation(out=Ering[es], in_=ps[sb][:, :], func=AF.Exp, scale=1.0 / 16.0),
                         reads=[("ps", sb)], writes=[("E", es)])
                obs = []
                for j in range(2):
                    ob = xbank()
                    obs.append(ob)
                    for mc in range(2):
                        P.op("pe", lambda e, ob=ob, mc=mc, h=h, j=j, es=eslots[mc]: e.matmul(
                            ps[ob][:, :], lhsT=Vm[:, mc, h * 256 + j * 128:h * 256 + (j + 1) * 128], rhs=Ering[es],
                            start=(mc == 0), stop=(mc == 1)), reads=Vm_keys + [("E", eslots[mc])], writes=[("ps", ob)])
                db = xbank()
                for mc in range(2):
                    P.op("pe", lambda e, db=db, mc=mc, es=eslots[mc]: e.matmul(
                        ps[db][:, :], lhsT=onesB, rhs=Ering[es], start=(mc == 0), stop=(mc == 1)),
                        reads=["onesB", ("E", eslots[mc])], writes=[("ps", db)])
                ds = h % 2
                P.op("dve", lambda e, db=db, ds=ds: e.reciprocal(out=dent[ds], in_=ps[db][:, :]), reads=[("ps", db)], writes=[("dent", ds)])
                for j in range(2):
                    P.op("dve", lambda e, ob=obs[j], ds=ds, h=h, j=j: e.tensor_tensor(
                        out=oT_tile[:, h * 2 + j, :], in0=ps[ob][:, :], in1=dent[ds], op=ALU.mult),
                        reads=[("ps", obs[j]), ("dent", ds)], writes=[("oT", h * 2 + j)])

        y2b = [f32v(HT_OFF + 3072 + i * 1024, 1024) for i in range(2)]
        x2tb = [f32v(HT_OFF + 5120 + i * 1024, 1024) for i in range(2)]
        x2Tf = f32v(TMP_OFF + 1024, 1024).rearrange("p (c t) -> p c t", c=8)
        P.alias([("y2b", 0), ("y2b", 1), ("x2tb", 0), ("x2tb", 1), ("x2Tf", 0), ("x2Tf", 1)],
                ["y2", "xn2", "x2t", "x2Tf", "memT", "wmix"] + memT_keys + [("xnA", 1)])

        Wring = [bfv(WR_OFF + i * 2048, 2048).rearrange("p (c n) -> p c n", c=8) for i in range(6)]
        wsrc = (weg_d, weu_d, wed_d)

        def unit_src(e_, k):
            if k < 4:
                t = wsrc[k % 2][e_]
                half = k // 2
            else:
                t = wsrc[2][e_]
                half = k - 4
            return t.rearrange("(c p) n -> p c n", p=128)[:, :, half * 512:(half + 1) * 512]

        def load_unit(e_, k):
            P.op("pool", lambda e, e_=e_, k=k: e.dma_start(out=Wring[k], in_=unit_src(e_, k)), writes=[("W", k)], dma=True)


        def p2_s0(tc):
            par = tc % 2
            if tc % 4 == 0:
                xattn_tile(tc // 4)
                if tc == 12:
                    P.alias([("W", 0), ("W", 1)], ["wq"])
                    load_unit(0, 0)
                    load_unit(0, 1)
            tcl = tc % 4
            for dh in range(2):
                b = par * 2 + dh
                for c in range(8):
                    P.op("pe", lambda e, b=b, c=c, dh=dh: e.matmul(
                        ps[b][:, :], lhsT=oT_tile[:, c, tcl * 128:(tcl + 1) * 128], rhs=wo[:, c, dh * 512:(dh + 1) * 512],
                        start=(c == 0), stop=(c == 7)), reads=[("oT", c), "wo"], writes=[("ps", b)])
                P.op("dve", lambda e, b=b, dh=dh: e.scalar_tensor_tensor(
                    out=y2b[par][:, dh * 512:(dh + 1) * 512], in0=Rt[:, tc, dh * 512:(dh + 1) * 512], scalar=ALPHA,
                    in1=ps[b][:, :], op0=ALU.mult, op1=ALU.add), reads=[("R", tc), ("ps", b)], writes=[("y2b", par, dh)])

        def p2_s1(tc):
            par = tc % 2
            P.op("dve", lambda e: e.engine_nop(), reads=[("y2b", par, 0), ("y2b", par, 1)], writes=[("y2b", par)])
            ln_stats(y2b[par], ("y2b", par), par)

        def p2_s2(tc):
            par = tc % 2
            ln_apply(y2b[par], ("y2b", par), par, x2tb[par], [("x2tb", par)], "lnp", y2b[par], ("y2b", par))
            P.alias([("y2b", par, 0), ("y2b", par, 1)], [("y2b", par)])

        def p2_s2b(tc):
            par = tc % 2
            P.op("act", lambda e: e.activation(out=Rt[:, tc, :], in_=x2tb[par], func=AF.Copy, scale=ALPHA),
                 reads=[("x2tb", par)], writes=[("R", tc)])
            P.op("pool", lambda e: e.dma_start(out=xrows_d[tc * 128:(tc + 1) * 128, :], in_=x2tb[par]),
                 reads=[("x2tb", par)], writes=[("XROWS", tc)], dma=True)
            for half in range(2):
                b = 4 + half
                for jj in range(4):
                    dc = half * 4 + jj
                    P.op("pe", lambda e, b=b, jj=jj, dc=dc: e.transpose(ps[b][:, jj * 128:(jj + 1) * 128],
                                                                        x2tb[par][:, dc * 128:(dc + 1) * 128], identF),
                         reads=[("x2tb", par), "identF"], writes=[("ps", b)])
                srcp = ps[b][:].rearrange("p (j t) -> p j t", j=4)
                P.op("act", lambda e, half=half, srcp=srcp: e.activation(
                    out=x2Tf[:, half * 4:(half + 1) * 4, :], in_=srcp, func=AF.Copy),
                    reads=[("ps", b)], writes=[("x2Tf", half)])

        def p2_s2c(tc):
            par = tc % 2
            lb = 6 + par
            for dc in range(8):
                P.op("pe", lambda e, dc=dc: e.matmul(ps[lb][:, 0:32], lhsT=x2Tf[:, dc, :], rhs=wr_sb[:, dc, :],
                                                     start=(dc == 0), stop=(dc == 7)),
                     reads=[("x2Tf", 0), ("x2Tf", 1), "wr"], writes=[("ps", lb)])

        def p2_s3a(tc):
            par = tc % 2
            lb = 6 + par
            P.op("dve", lambda e: e.tensor_tensor(out=lg, in0=ps[lb][:, 0:32], in1=br_sb, op=ALU.add),
                 reads=[("ps", lb), "br"], writes=["lg"])
            P.op("dve", lambda e: e.max(out=top8, in_=lg), reads=["lg"], writes=["top8"])
            P.op("dve", lambda e: e.tensor_scalar(out=msk, in0=lg, scalar1=top8[:, 3:4], scalar2=None, op0=ALU.is_ge),
                 reads=["lg", "top8"], writes=["msk"])
            P.op("dve", lambda e: e.tensor_scalar(out=negm, in0=top8[:, 0:1], scalar1=-1.0, scalar2=None, op0=ALU.mult),
                 reads=["top8"], writes=["negm"])
            P.op("act", lambda e: e.activation(out=exg, in_=lg, func=AF.Exp, bias=negm), reads=["lg", "negm"], writes=["exg"])
            P.op("dve", lambda e: e.tensor_copy(out=msk_bf, in_=msk), reads=["msk"], writes=["msk_bf"])
            P.op("dve", lambda e: e.max_index(out=idx8, in_max=top8, in_values=lg), reads=["top8", "lg"], writes=["idx8"])
            P.op("dve", lambda e: e.tensor_copy(out=idxf, in_=idx8[:, 0:4]), reads=["idx8"], writes=["idxf"])
            P.op("dve", lambda e: e.tensor_tensor(out=exg, in0=exg, in1=msk, op=ALU.mult), reads=["exg", "msk"], writes=["exg"])
            P.op("dve", lambda e: e.reduce_sum(out=ssum, in_=exg, axis=AX.X), reads=["exg"], writes=["ssum"])
            P.op("dve", lambda e: e.reciprocal(out=ssum, in_=ssum), reads=["ssum"], writes=["ssum"])
            P.op("act", lambda e: e.activation(out=gk4, in_=top8[:, 0:4], func=AF.Exp, bias=negm), reads=["top8", "negm"], writes=["gk4"])
            P.op("dve", lambda e: e.tensor_scalar(out=gk4, in0=gk4, scalar1=ssum, scalar2=None, op0=ALU.mult),
                 reads=["gk4", "ssum"], writes=["gk4"])
            P.op("pe", lambda e: e.matmul(ps[lb][:, 64:96], lhsT=Ltri, rhs=msk_bf, start=True, stop=True),
                 reads=["Ltri", "msk_bf", "lg"], writes=[("ps", lb)])
            P.op("pe", lambda e: e.matmul(ps[lb][:, 96:128], lhsT=onesB, rhs=msk_bf, start=True, stop=True),
                 reads=["onesB", "msk_bf"], writes=[("ps", lb)])

        def p2_s3b(tc):
            par = tc % 2
            pb = 6 + par
            P.op("dve", lambda e: e.tensor_tensor(out=posf, in0=ps[pb][:, 64:96], in1=cnt, op=ALU.add),
                 reads=[("ps", pb), "cnt"], writes=["posf"])
            P.op("dve", lambda e: e.tensor_tensor(out=cnt, in0=ps[pb][:, 96:128], in1=cnt, op=ALU.add),
                 reads=[("ps", pb), "cnt", "posf"], writes=["cnt"])
            P.op("dve", lambda e: e.tensor_tensor(out=sel4, in0=iota32.unsqueeze(1).to_broadcast([128, 4, 32]),
                                                  in1=idxf.unsqueeze(2).to_broadcast([128, 4, 32]), op=ALU.is_equal),
                 reads=["iota32", "idxf"], writes=["sel4"])
            P.op("dve", lambda e: e.tensor_tensor(out=sel4, in0=sel4, in1=posf.unsqueeze(1).to_broadcast([128, 4, 32]), op=ALU.mult),
                 reads=["sel4", "posf"], writes=["sel4"])
            P.op("dve", lambda e: e.reduce_sum(out=posk, in_=sel4, axis=AX.X), reads=["sel4"], writes=["posk"])
            P.op("dve", lambda e: e.scalar_tensor_tensor(out=destf, in0=idxf, scalar=float(CAP), in1=posk, op0=ALU.mult, op1=ALU.add),
                 reads=["idxf", "posk"], writes=["destf"])
            P.op("dve", lambda e: e.tensor_scalar(out=ovf, in0=posk, scalar1=float(CAP), scalar2=None, op0=ALU.is_ge),
                 reads=["posk"], writes=["ovf"])
            P.op("dve", lambda e: e.scalar_tensor_tensor(out=destf, in0=ovf, scalar=4.0e6, in1=destf, op0=ALU.mult, op1=ALU.add),
                 reads=["ovf", "destf"], writes=["destf"])
            P.op("dve", lambda e: e.tensor_copy(out=DEST[:, tc, :], in_=destf), reads=["destf"], writes=[("DEST", tc)])
            P.op("dve", lambda e: e.tensor_scalar(out=destf, in0=destf, scalar1=float(NSLOT - 1), scalar2=None, op0=ALU.min),
                 reads=["destf", ("DEST", tc)], writes=["destf"])
            P.op("dve", lambda e: e.tensor_copy(out=DESTG[:, tc, :], in_=destf), reads=["destf"], writes=[("DESTG", tc)])
            P.op("dve", lambda e: e.tensor_scalar(out=ovf, in0=ovf, scalar1=-1.0, scalar2=1.0, op0=ALU.mult, op1=ALU.add),
                 reads=["ovf", "destf"], writes=["ovf"])
            P.op("dve", lambda e: e.tensor_tensor(out=Gk[:, tc, :], in0=gk4, in1=ovf, op=ALU.mult),
                 reads=["gk4", "ovf"], writes=[("Gk", tc)])
            for k in range(4):
                P.op("pool", lambda e, k=k: e.indirect_dma_start(
                    out=tok_d[:, :], out_offset=bass.IndirectOffsetOnAxis(ap=DEST_t[:, tc * 4 + k:tc * 4 + k + 1], axis=0),
                    in_=TOKID_t[:, tc:tc + 1], in_offset=None, bounds_check=pool_regs["bc"], oob_is_err=False),
                    reads=[("DEST", tc), "TOKID", "TOKZ"], writes=[("TOK", tc, k)], dma=True)

        def p2_s3c(tc):
            for dh in range(2):
                b = 4 + dh
                P.op("pe", lambda e, b=b, dh=dh: e.matmul(ps[b][:, :], lhsT=CTc[0:32, :], rhs=bd_sb[0:32, dh * 512:(dh + 1) * 512],
                                                           start=True, stop=True), reads=["CTc", "bd"], writes=[("ps", b)])
                P.op("dve", lambda e, b=b, dh=dh: e.tensor_tensor(
                    out=Rt[:, tc, dh * 512:(dh + 1) * 512], in0=ps[b][:, :], in1=Rt[:, tc, dh * 512:(dh + 1) * 512], op=ALU.add),
                    reads=[("ps", b), ("R", tc)], writes=[("R", tc)])

        skewed(NT, [p2_s0, p2_s1, p2_s2, p2_s2b, p2_s2c, p2_s3a, p2_s3b])

        if stage == "p2":
            toks = []
            for tc in range(NT):
                toks.append(P.op("sp", lambda e, tc=tc: e.dma_start(out=out_d[tc * 128:(tc + 1) * 128, :], in_=Rt[:, tc, :]),
                                 reads=[("R", tc), ("R", tc, 0), ("R", tc, 1)], writes=[("out", tc)], dma=True))
            return finish(toks)

        P.alias([("W", i) for i in range(2, 6)], ["wq", "wo"] + [("qT", c) for c in range(8)] + [("oT", c) for c in range(8)])
        xg_tok = [bfv(XT_OFF, 2560).rearrange("p (a d) -> p a d", a=NCH)] * 2
        xgTb = [bfv(XT_OFF + 2560 + i * 2560, 2560).rearrange("p (c j) -> p c j", c=8) for i in range(2)]
        P.alias([("xg", 0, a) for a in range(NCH)] + [("xgT", i, a) for i in range(2) for a in range(NCH)], XTk(range(NT)))
        hTs = bfv(HT_OFF, 2560).rearrange("p (f j) -> p f j", f=8)
        ystage = [f32v(HT_OFF + 2560 + i * 1024, 1024) for i in range(3)]
        bdb = [f32v(HT_OFF + 5632 + i * 1024, 1024) for i in range(2)]
        ph2_keys = (["bd", "y2", ("y2", 0), ("y2", 1), "xn2", "x2t", ("x2Tf", 0), ("x2Tf", 1), "CTc", "lg", "top8", "msk", "negm",
                     "exg", "ssum", "idx8", "idxf", "posk", "destf", "ovf", "gk4", "msk_bf", "posf", "sel4"] + KmT_keys + Vm_keys)
        P.alias([("hTs", f) for f in range(8)] + [("ys", i) for i in range(3)] + [("bdb", 0), ("bdb", 1)],
                ph2_keys + [("y2b", 0), ("y2b", 1), ("x2tb", 0), ("x2tb", 1), ("y2b", 0, 0), ("y2b", 0, 1), ("y2b", 1, 0), ("y2b", 1, 1)])
        HW_ = CAP // 2
        rg = [f32v(TMP_OFF + i * 512, HW_) for i in range(2)]
        sg = [f32v(TMP_OFF + 1024 + i * 512, HW_) for i in range(2)]
        pp = [f32v(TMP_OFF + 2048 + i * 512, HW_) for i in range(2)]
        P.alias([("rg", 0), ("rg", 1), ("sg", 0), ("sg", 1), ("pp", 0), ("pp", 1)],
                [("dent", 0), ("dent", 1)] + [("E", i) for i in range(4)])
        for k in range(2, 6):
            load_unit(0, k)
        yv = f32v(TMP_OFF, NE * NCH).rearrange("p (e a) -> p e a", e=NE)
        yb = f32v(TMP_OFF + 256, NE * NCH).rearrange("p (e a) -> p e a", e=NE)
        ysp = f32v(TMP_OFF + 512, NCH)
        yi_i = f32v(TMP_OFF + 768, NE * NCH).bitcast(I32)
        P.alias(["yv", "yb", "ysp", "yi_i"], [("dent", 0), ("dent", 1), ("x2Tf", 0), ("x2Tf", 1)] + [("E", i) for i in range(4)])
        P.op("pool", lambda e: e.iota(yi_i, pattern=[[CAP, NE], [1, NCH]], base=0, channel_multiplier=NCH), writes=["yi_i"])
        P.op("pool", lambda e: e.tensor_copy(out=yb, in_=yi_i.rearrange("p (e a) -> p e a", e=NE)), reads=["yi_i"], writes=["yb"])
        P.op("pool", lambda e: e.tensor_copy(out=ysp, in_=yi_i[:, 0:NCH]), reads=["yi_i"], writes=["ysp"])
        P.op("dve", lambda e: e.tensor_tensor(out=yv, in0=ysp.unsqueeze(1).to_broadcast([128, NE, NCH]),
                                              in1=cnt.unsqueeze(2).to_broadcast([128, NE, NCH]), op=ALU.is_lt),
             reads=["ysp", "cnt"], writes=["yv"])
        P.op("dve", lambda e: e.tensor_scalar(out=yv, in0=yv, scalar1=-30000.0, scalar2=30000.0, op0=ALU.mult, op1=ALU.add),
             reads=["yv"], writes=["yv"])
        P.op("dve", lambda e: e.tensor_tensor(out=yv, in0=yv, in1=yb, op=ALU.add), reads=["yv", "yb"], writes=["yv"])
        P.op("dve", lambda e: e.tensor_copy(out=YIDX_t[:, :].rearrange("p (e a) -> p e a", e=NE), in_=yv), reads=["yv"], writes=["YIDX"])
        P.alias([("rg", 0), ("rg", 1), ("sg", 0), ("sg", 1), ("pp", 0), ("pp", 1)], ["yv", "yb", "ysp", "yi_i"])
        tok_keys = ["TOKZ"] + [("TOK", tc, k) for tc in range(NT) for k in range(4)]
        P.op("pool", lambda e: e.dma_start(out=gidx_t[:, :].rearrange("p (e a) -> p e a", e=NE),
                                           in_=tok_d.rearrange("(e p a) o -> p e (a o)", e=NE, p=128)),
             reads=tok_keys + ["gidx"], writes=["gidx"], dma=True)
        xrow_keys = [("XROWS", tc) for tc in range(NT)]

        def gather_expert(ex):
            for a in range(NCH):
                P.op("pool", lambda e, ex=ex, a=a: e.indirect_dma_start(
                    out=xg_tok[0][:, a, :], out_offset=None, in_=xrows_d[:, :],
                    in_offset=bass.IndirectOffsetOnAxis(ap=gidx_t[:, ex * NCH + a:ex * NCH + a + 1], axis=0),
                    bounds_check=pool_regs["bx"], oob_is_err=False),
                    reads=["gidx", "xgzero"] + xrow_keys, writes=[("xg", 0, a)], dma=True)

        evf = [0]

        def transpose_expert(ex):
            tb = ex % 2
            for a in range(NCH):
                b = next_bank()
                for dc in range(8):
                    P.op("pe", lambda e, b=b, dc=dc, a=a: e.transpose(
                        psb[b][:, dc * 128:(dc + 1) * 128], xg_tok[0][:, a, dc * 128:(dc + 1) * 128], identB),
                        reads=[("xg", 0, a), "identB"], writes=[("ps", b)])
                src3 = psb[b][:].rearrange("p (c j) -> p c j", c=8)
                dst3 = xgTb[tb][:, :, a * 128:(a + 1) * 128]
                if evf[0] % 2 == 0:
                    P.op("act", lambda e, src3=src3, dst3=dst3: e.activation(out=dst3, in_=src3, func=AF.Copy),
                         reads=[("ps", b)], writes=[("xgT", tb, a)])
                else:
                    P.op("dve", lambda e, src3=src3, dst3=dst3: e.tensor_copy(out=dst3, in_=src3),
                         reads=[("ps", b)], writes=[("xgT", tb, a)])
                evf[0] += 1

        P.alias(["xgzero"], XTk(range(NT)))
        P.op("pool", lambda e: e.memset(xg_tok[0], 0.0), reads=[("xg", 0, a) for a in range(NCH)], writes=["xgzero"] + [("xg", 0, a) for a in range(NCH)])
        gather_expert(0)
        transpose_expert(0)
        if NE > 1:
            gather_expert(1)
        sl = [0]
        ysl = [0]
        for ex in range(NE):
            xgT = xgTb[ex % 2]
            xgT_keys = [("xgT", ex % 2, a) for a in range(NCH)]
            for fc in range(8):
                half = fc // 4
                gslot, uslot = 2 * half, 2 * half + 1
                cl = (fc % 4) * 128
                for hh in range(2):
                    js = slice(hh * HW_, (hh + 1) * HW_)
                    bG, bU = next_bank(), next_bank()
                    for (b, ws) in ((bG, gslot), (bU, uslot)):
                        for dc in range(8):
                            P.op("pe", lambda e, b=b, ws=ws, dc=dc, cl=cl, js=js, xgT=xgT: e.matmul(
                                ps[b][:, 0:HW_], lhsT=Wring[ws][:, dc, cl:cl + 128], rhs=xgT[:, dc, js], start=(dc == 0), stop=(dc == 7)),
                                reads=[("W", ws)] + xgT_keys, writes=[("ps", b)])
                    s_ = sl[0] % 2
                    sl[0] += 1
                    P.op("act", lambda e, bG=bG, s_=s_, ex=ex, fc=fc: e.activation(
                        out=rg[s_], in_=ps[bG][:, 0:HW_], func=AF.Relu, scale=-1.0, bias=bgT[:, ex, fc:fc + 1]),
                        reads=[("ps", bG), ("bT", 0)], writes=[("rg", s_)])
                    P.op("act", lambda e, s_=s_: e.activation(out=sg[s_], in_=rg[s_], func=AF.Sigmoid, scale=-1.702, bias=sig_bias_col),
                         reads=[("rg", s_), "sigb"], writes=[("sg", s_)])
                    P.op("act", lambda e, bU=bU, s_=s_, ex=ex, fc=fc: e.activation(
                        out=pp[s_], in_=ps[bU][:, 0:HW_], func=AF.Relu, bias=buT[:, ex, fc:fc + 1]),
                        reads=[("ps", bU), ("bT", 1)], writes=[("pp", s_)])
                    P.op("dve", lambda e, s_=s_: e.scalar_tensor_tensor(out=rg[s_], in0=rg[s_], scalar=-7.0, in1=sg[s_], op0=ALU.add, op1=ALU.mult),
                         reads=[("rg", s_), ("sg", s_)], writes=[("rg", s_)])
                    P.op("dve", lambda e, s_=s_: e.scalar_tensor_tensor(out=pp[s_], in0=pp[s_], scalar=14.0, in1=rg[s_], op0=ALU.min, op1=ALU.mult),
                         reads=[("pp", s_), ("rg", s_)], writes=[("pp", s_)])
                    P.op("dve", lambda e, s_=s_, fc=fc, js=js: e.scalar_tensor_tensor(
                        out=hTs[:, fc, js], in0=rg[s_], scalar=6.0, in1=pp[s_], op0=ALU.mult, op1=ALU.subtract),
                        reads=[("rg", s_), ("pp", s_)], writes=[("hTs", fc, hh)])
                if fc == 3 and ex + 1 < NE:
                    load_unit(ex + 1, 0)
                    load_unit(ex + 1, 1)
            if ex + 1 < NE:
                load_unit(ex + 1, 2)
                load_unit(ex + 1, 3)
            hT_keys = [("hTs", f, hh) for f in range(8) for hh in range(2)]
            if ex + 1 < NE:
                transpose_expert(ex + 1)
                if ex + 2 < NE:
                    gather_expert(ex + 2)
            bpar = ex % 2
            P.op("sp", lambda e, ex=ex, bpar=bpar: e.dma_start(out=bdb[bpar], in_=bed_d[ex:ex + 1, :].partition_broadcast(128)),
                 writes=[("bdb", bpar)], dma=True)
            ygv = yg_d[ex * CAP:(ex + 1) * CAP, :].rearrange("(p a) d -> p a d", a=NCH)
            for a in range(NCH):
                ys_ = ysl[0] % 3
                ysl[0] += 1
                for dh in range(2):
                    b = next_bank()
                    for fc in range(8):
                        P.op("pe", lambda e, b=b, fc=fc, a=a, dh=dh: e.matmul(
                            ps[b][:, :], lhsT=hTs[:, fc, a * 128:(a + 1) * 128], rhs=Wring[4 + dh][:, fc, :],
                            start=(fc == 0), stop=(fc == 7)), reads=hT_keys + [("W", 4 + dh)], writes=[("ps", b)])
                    P.op("dve", lambda e, b=b, ys_=ys_, dh=dh, bpar=bpar: e.tensor_tensor(
                        out=ystage[ys_][:, dh * 512:(dh + 1) * 512], in0=ps[b][:, :], in1=bdb[bpar][:, dh * 512:(dh + 1) * 512], op=ALU.add),
                        reads=[("ps", b), ("bdb", bpar)], writes=[("ys", ys_, dh)])
                P.op("pool", lambda e, ys_=ys_, a=a, ex=ex: e.indirect_dma_start(
                    out=yg_d[:, :], out_offset=bass.IndirectOffsetOnAxis(ap=YIDX_t[:, ex * NCH + a:ex * NCH + a + 1], axis=0),
                    in_=ystage[ys_], in_offset=None, bounds_check=pool_regs["bc"], oob_is_err=False),
                    reads=[("ys", ys_, 0), ("ys", ys_, 1), "YIDX"] + [("YGZ", g8) for g8 in range(NSLOT // 2560)],
                    writes=[("YG", ex, a)], dma=True)
            if ex + 1 < NE:
                load_unit(ex + 1, 4)
                load_unit(ex + 1, 5)

        P.op("sp", lambda e: e.dma_start(out=lnp, in_=ln_d[4:6, :].partition_broadcast(128)), writes=["lnp"], dma=True)
        yg_keys = [("YG", ex, a) for ex in range(NE) for a in range(NCH)]
        yk = [f32v(WR_OFF + i * 1024, 1024) for i in range(8)]
        xn3 = f32v(WR_OFF + 8192, 1024)
        otile = [f32v(WR_OFF + 9216 + i * 1024, 1024) for i in range(2)]
        P.alias([("yk", i) for i in range(8)] + ["xn3", ("ot", 0), ("ot", 1)], [("W", i) for i in range(6)])
        toks = []
        xn3b = [xn3, f32v(WR_OFF + 11264, 1024)]
        P.alias([("xn3b", 0), ("xn3b", 1)], ["xn3"] + [("W", i) for i in range(6)])
        for tc in range(NT):
            P.alias([("R", tc)], [("R", tc, 0), ("R", tc, 1)])

        def t_s0(tc):
            for k in range(4):
                yi = (tc % 2) * 4 + k
                P.op("pool", lambda e, k=k, yi=yi: e.indirect_dma_start(
                    out=yk[yi], out_offset=None, in_=yg_d[:, :],
                    in_offset=bass.IndirectOffsetOnAxis(ap=DESTG_t[:, tc * 4 + k:tc * 4 + k + 1], axis=0)),
                    reads=yg_keys + [("DESTG", tc)], writes=[("yk", yi)], dma=True)

        def t_s0b(tc):
            for k in range(4):
                yi = (tc % 2) * 4 + k
                P.op("dve", lambda e, k=k, yi=yi: e.scalar_tensor_tensor(
                    out=Rt[:, tc, :], in0=yk[yi], scalar=Gk[:, tc, k:k + 1], in1=Rt[:, tc, :], op0=ALU.mult, op1=ALU.add),
                    reads=[("yk", yi), ("Gk", tc), ("R", tc)], writes=[("R", tc)])

        def t_s1(tc):
            ln_stats(Rt[:, tc, :], ("R", tc), tc % 2)

        def t_s2(tc):
            par = tc % 2
            ln_apply(Rt[:, tc, :], ("R", tc), par, otile[par], [("ot", par)], "lnp", xn3b[par], ("xn3b", par))
            toks.append(P.op("sp", lambda e: e.dma_start(out=out_d[tc * 128:(tc + 1) * 128, :], in_=otile[par]),
                             reads=[("ot", par)], writes=[("out", tc)], dma=True))

        skewed(NT, [t_s0, t_s0b, t_s1, t_s2])
        return finish(toks)

    return nc


def _rope_tables():
    t = np.arange(S)
    def cs(pos, dim):
        inv = (10000.0 ** (-np.arange(0, dim, 2, dtype=np.float32) / dim)).astype(np.float32)
        ang = pos.astype(np.float32)[:, None] * inv[None, :]
        return np.cos(ang).astype(np.float32), np.sin(ang).astype(np.float32)
    cr, sr = cs(t // 64, 32)
    cc, sc = cs(t % 64, 32)
    cq, sq = cs(t, 64)
    A = np.zeros((S, 2, 64), np.float32)
    A[:, 0] = np.concatenate([cr, cr, cc, cc], -1)
    A[:, 1] = np.concatenate([-sr, sr, -sc, sc], -1)
    B = np.zeros((S, 2, 64), np.float32)
    B[:, 0] = np.concatenate([cq, cq], -1)
    B[:, 1] = np.concatenate([-sq, sq], -1)
    return A, B


def make_in_maps(inputs, cores):
    f = lambda k: np.ascontiguousarray(np.asarray(inputs[k], dtype=np.float32))
    A, B = _rope_tables()
    shared = {
        "w_in": f("w_in")[0],
        "a_q_norm": f("a_q_norm"),
        "a_k_norm": f("a_k_norm"),
        "b_lambda": np.ascontiguousarray(np.concatenate(
            [f("b_lambda_q1"), f("b_lambda_k1"), f("b_lambda_q2"), f("b_lambda_k2")], 0)),
        "b_subln": np.ascontiguousarray(f("b_subln").reshape(128, 1)),
        "w_mix_out": f("w_mix_out")[0],
        "ln_gb": np.ascontiguousarray(np.concatenate(
            [f("ln1_g"), f("ln1_b"), f("ln2_g"), f("ln2_b"), f("ln3_g"), f("ln3_b")], 0)),
        "w_mem_q": f("w_mem_q")[0],
        "w_mem_kv": f("w_mem_kv")[0],
        "w_mem_out": f("w_mem_out")[0],
        "w_router": f("w_router")[0],
        "b_router": f("b_router"),
        "w_e_gate": f("w_e_gate")[0],
        "b_e_gate": f("b_e_gate")[0],
        "w_e_up": f("w_e_up")[0],
        "b_e_up": f("b_e_up")[0],
        "w_e_down": f("w_e_down")[0],
        "b_e_down": f("b_e_down")[0],
        "ropeA": A,
        "ropeB": B,
        "zeros_rows": np.zeros((2560, D), np.float32),
    }
    x = f("x")
    mem = f("mem")
    maps = []
    for c in cores:
        m = dict(shared)
        m["x"] = x[c]
        m["mem"] = mem[c]
        maps.append(m)
    return maps


def kernel(**inputs):
    nc = build_program("full")
    cores = list(range(8))
    in_maps = make_in_maps(inputs, cores)
    res = run_bass_kernel_spmd(nc, in_maps, core_ids=cores)
    out = np.stack([np.asarray(r["out"], dtype=np.float32) for r in res.results], 0)
    return out
```

```python
import contextlib
import numpy as np
import concourse.bass as bass
import concourse.mybir as mybir
from concourse.bass_utils import run_bass_kernel_spmd

F32 = mybir.dt.float32
BF16 = mybir.dt.bfloat16
I32 = mybir.dt.int32
U32 = mybir.dt.uint32
AF = mybir.ActivationFunctionType
ALU = mybir.AluOpType
AX = mybir.AxisListType

S = 2048
D = 1024
NT = 16
NE = 32
ALPHA = 2.0 ** 0.25
LAMBDA_INIT = 0.2
LN_EPS = 1e-5
RMS_EPS = 1e-6
DMA_RING = 16
ARENA_W = 52600
CAP = 640
NCH = CAP // 128
NSLOT = NE * CAP


class Prog:
    ENG = ("pe", "act", "dve", "pool", "sp")

    def __init__(self):
        self.stream = {e: [] for e in self.ENG}
        self.nops = {e: 0 for e in self.ENG}
        self.writer = {}
        self.readers = {}
        self.dma_n = {e: 0 for e in self.ENG}
        self.milestones = {e: set() for e in self.ENG}

    @staticmethod
    def _ident(tok):
        return (tok[0], tok[1])

    def _merge(self, d, tok):
        i = self._ident(tok)
        if i not in d or d[i][2] < tok[2]:
            d[i] = tok

    def alias(self, new_keys, old_keys):
        acc = {}
        for k in old_keys:
            w = self.writer.get(k)
            if w is not None:
                self._merge(acc, w)
            for t in self.readers.get(k, {}).values():
                self._merge(acc, t)
        for k in new_keys:
            r = self.readers.setdefault(k, {})
            for t in acc.values():
                self._merge(r, t)

    def op(self, eng, fn, reads=(), writes=(), dma=False):
        deps = {}
        for k in reads:
            w = self.writer.get(k)
            if w is not None:
                self._merge(deps, w)
        for k in writes:
            w = self.writer.get(k)
            if w is not None:
                self._merge(deps, w)
            for t in self.readers.get(k, {}).values():
                self._merge(deps, t)
        waits = []
        for t in deps.values():
            if t[0] == "c" and t[1] == eng and eng == "pe":
                continue
            waits.append(t)
            if t[0] == "c":
                self.milestones[t[1]].add(t[2])
        if dma:
            n = self.dma_n[eng]
            self.dma_n[eng] = n + 1
            sem = (eng, n % DMA_RING)
            if n >= DMA_RING:
                waits.append(("d", sem, 16 * (n // DMA_RING)))
            tok = ("d", sem, 16 * (n // DMA_RING + 1))
            self.stream[eng].append(("dma", fn, waits, tok))
        else:
            self.nops[eng] += 1
            tok = ("c", eng, self.nops[eng])
            self.stream[eng].append(("op", fn, waits, tok))
        for k in writes:
            self.writer[k] = tok
            self.readers[k] = {}
        for k in reads:
            if k in writes:
                continue
            self._merge(self.readers.setdefault(k, {}), tok)
        return tok

    def wait_tokens(self, eng, toks):
        for t in toks:
            if t[0] == "c":
                self.milestones[t[1]].add(t[2])
        self.stream[eng].append(("wait", None, list(toks), None))

    def emit(self, block, nc, esem, dsem):
        rank = {}
        for e in self.ENG:
            ms = sorted(self.milestones[e])
            rank[e] = {idx: i + 1 for i, idx in enumerate(ms)}

        def run(eng_name, eng):
            known = {}
            for kind, fn, waits, tok in self.stream[eng_name]:
                for t in waits:
                    if t[0] == "c":
                        sem = esem[t[1]]
                        val = rank[t[1]][t[2]]
                        key = ("c", t[1])
                    else:
                        sem = dsem[t[1]]
                        val = t[2]
                        key = ("d", t[1])
                    if known.get(key, 0) >= val:
                        continue
                    known[key] = val
                    eng.wait_ge(sem, val)
                if kind == "wait":
                    continue
                ins = fn(eng)
                if kind == "dma":
                    ins.then_inc(dsem[tok[1]], 16)
                elif tok[2] in rank[eng_name]:
                    ins.then_inc(esem[eng_name], 1)

        @block.tensor
        def _(e):
            run("pe", e)

        @block.scalar
        def _(e):
            run("act", e)

        @block.vector
        def _(e):
            run("dve", e)

        @block.gpsimd
        def _(e):
            run("pool", e)

        @block.sync
        def _(e):
            run("sp", e)


def build_program(stage="full"):
    nc = bass.Bass("TRN2", target_bir_lowering=False)

    def din(name, shape):
        return nc.dram_tensor(name, list(shape), F32, kind="ExternalInput").ap()

    x_d = din("x", [S, D])
    mem_d = din("mem", [256, D])
    w_in_d = din("w_in", [D, 2304])
    aq_d = din("a_q_norm", [1, 64])
    ak_d = din("a_k_norm", [1, 64])
    lam_d = din("b_lambda", [4, 64])
    subln_d = din("b_subln", [128, 1])
    wmix_d = din("w_mix_out", [D, D])
    ln_d = din("ln_gb", [6, D])
    wq_d = din("w_mem_q", [D, D])
    wkv_d = din("w_mem_kv", [D, 2 * D])
    wo_d = din("w_mem_out", [D, D])
    wr_d = din("w_router", [D, NE])
    br_d = din("b_router", [1, NE])
    weg_d = din("w_e_gate", [NE, D, D])
    beg_d = din("b_e_gate", [NE, D])
    weu_d = din("w_e_up", [NE, D, D])
    beu_d = din("b_e_up", [NE, D])
    wed_d = din("w_e_down", [NE, D, D])
    bed_d = din("b_e_down", [NE, D])
    ropeA_d = din("ropeA", [S, 2, 64])
    ropeB_d = din("ropeB", [S, 2, 64])
    zeros_d = din("zeros_rows", [2560, D])
    out_d = nc.dram_tensor("out", [S, D], F32, kind="ExternalOutput").ap()
    xrows_d = nc.dram_tensor("xrows", [S, D], BF16).ap()
    tok_d = nc.dram_tensor("tokslots", [NSLOT, 1], I32).ap()
    yg_d = nc.dram_tensor("ygrows", [NSLOT, D], F32).ap()
    dbg_d = None
    if stage != "full":
        dbg_d = nc.dram_tensor("dbg", [128, 40960], F32, kind="ExternalOutput").ap()

    P = Prog()
    st = contextlib.ExitStack()
    with st:
        arena = st.enter_context(nc.sbuf_tensor("arena", [128, ARENA_W], F32))
        ps = [st.enter_context(nc.psum_tensor(f"ps{i}", [128, 512], F32)) for i in range(8)]
        esem = {e: st.enter_context(nc.semaphore(f"s_{e}")) for e in Prog.ENG}
        dsem = {}
        for e in ("sp", "pool"):
            for i in range(DMA_RING):
                dsem[(e, i)] = st.enter_context(nc.semaphore(f"d_{e}{i}"))

        def f32v(off, n):
            return arena[:, off:off + n]

        def bfv(off, n_words):
            return arena[:, off:off + n_words].bitcast(BF16)

        R_OFF, XT_OFF, WR_OFF, HT_OFF, TMP_OFF, MISC_OFF = 0, 16384, 24576, 36864, 45056, 48128
        Rt = f32v(R_OFF, 16384).rearrange("p (c d) -> p c d", c=NT)
        XT = bfv(XT_OFF, 8192).rearrange("p (c t) -> p c t", c=8)

        mo = [MISC_OFF]

        def misc(nw):
            o = mo[0]
            mo[0] += nw
            assert mo[0] <= ARENA_W
            return o

        identF = f32v(misc(128), 128)
        identB = bfv(misc(64), 64)
        onesB = bfv(misc(64), 64)
        lnp = f32v(misc(2048), 2048).rearrange("p (a d) -> p a d", a=2)
        Cmb = f32v(misc(512), 512).rearrange("p (c e) -> p c e", c=NT)
        bgT = f32v(misc(256), 256).rearrange("p (e f) -> p e f", e=NE)
        buT = f32v(misc(256), 256).rearrange("p (e f) -> p e f", e=NE)
        wr_sb = f32v(misc(256), 256).rearrange("p (c e) -> p c e", c=8)
        br_sb = f32v(misc(32), 32)
        gq_sb = f32v(misc(64), 64)
        gk_sb = f32v(misc(64), 64)
        small = f32v(misc(64), 64)
        LNP_OFF = MISC_OFF + 128 + 64 + 64
        ropeT = f32v(LNP_OFF, 512).rearrange("p (s k a d) -> p s k a d", s=2, k=2, a=2)

        gidx_t = st.enter_context(nc.sbuf_tensor("gidx_t", [128, NE * NCH], I32))
        DEST_t = st.enter_context(nc.sbuf_tensor("DEST_t", [128, NT * 4], I32))
        DESTG_t = st.enter_context(nc.sbuf_tensor("DESTG_t", [128, NT * 4], I32))
        TOKID_t = st.enter_context(nc.sbuf_tensor("TOKID_t", [128, NT], I32))
        YIDX_t = st.enter_context(nc.sbuf_tensor("YIDX_t", [128, NE * NCH], I32))
        gidx_all = gidx_t[:, :].rearrange("p (e a) -> p e a", e=NE)
        DEST = DEST_t[:, :].rearrange("p (c k) -> p c k", c=NT)
        DESTG = DESTG_t[:, :].rearrange("p (c k) -> p c k", c=NT)
        Gk = f32v(misc(64), 64).rearrange("p (c k) -> p c k", c=NT)
        TOKID = TOKID_t[:, :]
        cnt = f32v(misc(32), 32)
        iota32 = f32v(misc(32), 32)
        Ltri = bfv(misc(64), 64)
        lam_col = small[:, 0:1]
        nlam_col = small[:, 1:2]
        gs_col = small[:, 2:3]
        lsum = small[:, 4:8]
        eps_col = small[:, 8:9]
        rmseps_col = small[:, 9:10]
        sig_bias_col = small[:, 10:11]

        psb = [p[:].bitcast(BF16) for p in ps]

        pool_regs = {}

        def mk_bc_reg(e):
            pool_regs["bc"] = e.alloc_register("bc")
            pool_regs["bx"] = e.alloc_register("bx")
            e.reg_mov(pool_regs["bx"], S - 1)
            return e.reg_mov(pool_regs["bc"], NSLOT - 1)

        P.op("pool", mk_bc_reg, writes=["bcreg"])
        P.op("pool", lambda e: e.memset(identF, 0.0), writes=["identF"])
        P.op("pool", lambda e: e.affine_select(out=identF, in_=identF, pattern=[[-1, 128]],
                                               compare_op=ALU.not_equal, fill=1.0, base=0,
                                               channel_multiplier=1), reads=["identF"], writes=["identF"])
        P.op("pool", lambda e: e.tensor_copy(out=identB, in_=identF), reads=["identF"], writes=["identB"])
        P.op("pool", lambda e: e.memset(onesB, 1.0), writes=["onesB"])
        P.op("pool", lambda e: e.memset(eps_col, LN_EPS), writes=["eps"])
        P.op("pool", lambda e: e.memset(rmseps_col, RMS_EPS), writes=["eps2"])
        P.op("pool", lambda e: e.memset(sig_bias_col, 1.702 * 7.0), writes=["sigb"])

        iota_i = small[:, 32:64].bitcast(I32)
        P.op("pool", lambda e: e.iota(iota_i, pattern=[[1, 32]], base=0, channel_multiplier=0), writes=["iota_i"])
        P.op("pool", lambda e: e.tensor_copy(out=iota32, in_=iota_i), reads=["iota_i"], writes=["iota32"])
        P.op("pool", lambda e: e.iota(TOKID, pattern=[[128, NT]], base=0, channel_multiplier=1), writes=["TOKID"])
        P.op("pool", lambda e: e.memset(cnt, 0.0), writes=["cnt"])
        P.op("pool", lambda e: e.memset(Ltri, 1.0), writes=["Ltri"])
        P.op("pool", lambda e: e.affine_select(out=Ltri, in_=Ltri, pattern=[[1, 128]], compare_op=ALU.is_gt, fill=0.0,
                                               base=0, channel_multiplier=-1), reads=["Ltri"], writes=["Ltri"])
        P.op("pool", lambda e: e.memset(gidx_t[:, :], 30000), writes=["gidx"])
        P.op("pool", lambda e: e.dma_start(out=tok_d.rearrange("(p a) o -> p (a o)", p=128),
                                           in_=gidx_t[:, :]), reads=["gidx"], writes=["TOKZ"], dma=True)
        P.op("sp", lambda e: e.dma_start(out=gq_sb, in_=aq_d.partition_broadcast(128)), writes=["gq"], dma=True)
        P.op("sp", lambda e: e.dma_start(out=gk_sb, in_=ak_d.partition_broadcast(128)), writes=["gk"], dma=True)

        xin = [f32v(TMP_OFF + i * 1024, 1024) for i in range(2)]
        Ering = [bfv(TMP_OFF + 2048 + i * 256, 256) for i in range(4)]

        def load_x(tc, slot):
            P.op("sp", lambda e: e.dma_start(out=xin[slot], in_=x_d[tc * 128:(tc + 1) * 128, :]),
                 writes=[("xin", slot)], dma=True)

        evac_flip = [0]

        def transpose_tile_to_XT(src, src_key, tc, banks):
            for half in range(2):
                b = banks[half]
                for j in range(4):
                    dc = half * 4 + j
                    P.op("pe", lambda e, b=b, j=j, dc=dc: e.transpose(ps[b][:, j * 128:(j + 1) * 128],
                                                                      src[:, dc * 128:(dc + 1) * 128], identF),
                         reads=[src_key, "identF"], writes=[("ps", b)])
                dst = XT[:, half * 4:(half + 1) * 4, tc * 128:(tc + 1) * 128]
                srcp = ps[b][:].rearrange("p (j t) -> p j t", j=4)
                eng = "act" if evac_flip[0] % 2 == 0 else "dve"
                evac_flip[0] += 1
                if eng == "act":
                    P.op("act", lambda e, dst=dst, srcp=srcp: e.activation(out=dst, in_=srcp, func=AF.Copy),
                         reads=[("ps", b)], writes=[("XT", tc, half)])
                else:
                    P.op("dve", lambda e, dst=dst, srcp=srcp: e.tensor_copy(out=dst, in_=srcp),
                         reads=[("ps", b)], writes=[("XT", tc, half)])

        def XTk(tcs):
            return [("XT", tc, h) for tc in tcs for h in range(2)]

        wi = bfv(WR_OFF, 9216).rearrange("p (c n) -> p c n", c=8)
        col_tiles = [(0, 512), (512, 768), (768, 1280), (1280, 1792), (1792, 2304)]
        w_in_v = w_in_d.rearrange("(c p) n -> p c n", p=128)
        for ci, (c0, c1) in enumerate(col_tiles):
            P.op("pool", lambda e, c0=c0, c1=c1: e.dma_start(out=wi[:, :, c0:c1], in_=w_in_v[:, :, c0:c1]),
                 writes=[("wi", ci)], dma=True)

        for tc in range(NT):
            load_x(tc, tc % 2)
            transpose_tile_to_XT(xin[tc % 2], ("xin", tc % 2), tc, (2 * (tc % 2), 2 * (tc % 2) + 1))

        QTA = bfv(R_OFF + 0, 4096).rearrange("p (j t) -> p j t", j=4)
        KTA = bfv(R_OFF + 4096, 1024)
        QKTB = bfv(R_OFF + 5120, 8192).rearrange("p (j t) -> p j t", j=8)
        VA = bfv(R_OFF + 13312, 3072).rearrange("p (c k n) -> p c k n", c=NT, k=2)
        VB = bfv(HT_OFF, 4096).rearrange("p (c n) -> p c n", c=NT)
        T1 = WR_OFF + 9216
        sqA = f32v(T1, 640)
        tA1 = f32v(T1 + 640, 640)
        tA2 = f32v(T1 + 1280, 640)
        tB1 = f32v(T1 + 1920, 512)
        tB2 = f32v(T1 + 2432, 512)
        ssA = f32v(T1 + 2944, 16)
        rstdA = f32v(T1 + 2960, 16)
        qkA_bf = bfv(TMP_OFF + 2560, 320)
        qkB_bf = bfv(T1 + 1920 + 0, 0) if False else None

        P.op("pool", lambda e: e.memset(VA[:, :, :, 0:64], 1.0), writes=["VAones0"])
        P.op("pool", lambda e: e.memset(VA[:, :, :, 128:192], 1.0), writes=["VAones1"])

        def load_rope(tc, slot):
            P.op("sp", lambda e: e.dma_start(out=ropeT[:, slot, 0], in_=ropeA_d[tc * 128:(tc + 1) * 128]),
                 writes=[("ropeA", slot)], dma=True)
            P.op("sp", lambda e: e.dma_start(out=ropeT[:, slot, 1], in_=ropeB_d[tc * 128:(tc + 1) * 128]),
                 writes=[("ropeB", slot)], dma=True)

        qkB_bf = bfv(TMP_OFF + 2048, 512)

        bank_rr = [0]

        def next_bank():
            b = bank_rr[0] % 8
            bank_rr[0] += 1
            return b

        for tc in range(NT):
            slot = tc % 2
            load_rope(tc, slot)
            banks = [next_bank() for _ in range(5)]
            for ci, (c0, c1) in enumerate(col_tiles):
                b = banks[ci]
                n = c1 - c0
                for dc in range(8):
                    P.op("pe", lambda e, b=b, n=n, dc=dc, c0=c0, c1=c1, tc=tc: e.matmul(
                        ps[b][:, 0:n], lhsT=XT[:, dc, tc * 128:(tc + 1) * 128], rhs=wi[:, dc, c0:c1],
                        start=(dc == 0), stop=(dc == 7)),
                        reads=XTk([tc]) + [("wi", ci)], writes=[("ps", b)])
            bA, bKV, bQ, bK, bV = banks
            P.op("act", lambda e, bA=bA: e.activation(out=sqA[:, 0:512], in_=ps[bA][:, 0:512], func=AF.Square),
                 reads=[("ps", bA)], writes=["sqA_q"])
            P.op("act", lambda e, bKV=bKV: e.activation(out=sqA[:, 512:640], in_=ps[bKV][:, 0:128], func=AF.Square),
                 reads=[("ps", bKV)], writes=["sqA_k"])
            P.op("dve", lambda e: e.reduce_sum(out=ssA[:, 0:10], in_=sqA.rearrange("p (h d) -> p h d", h=10), axis=AX.X),
                 reads=["sqA_q", "sqA_k"], writes=["ssA"])
            P.op("act", lambda e: e.activation(out=rstdA[:, 0:10], in_=ssA[:, 0:10], func=AF.Ln, scale=1.0 / 64.0, bias=rmseps_col),
                 reads=["ssA", "eps2"], writes=["rstdA"])
            P.op("act", lambda e: e.activation(out=rstdA[:, 0:10], in_=rstdA[:, 0:10], func=AF.Exp, scale=-0.5),
                 reads=["rstdA"], writes=["rstdA"])
            Ctab = ropeT[:, slot, 0, 0, :]
            Stab = ropeT[:, slot, 0, 1, :]
            gtab = f32v(T1 + 3040, 0) if False else None
            for (src_b, c_lo, nh, dst_lo, gsb, gkey) in ((bA, 0, 8, 0, gq_sb, "gq"), (bKV, 0, 2, 512, gk_sb, "gk")):
                xv = ps[src_b][:, c_lo:c_lo + nh * 64].rearrange("p (h d) -> p h d", h=nh)
                xg = tA1[:, dst_lo:dst_lo + nh * 64].rearrange("p (h d) -> p h d", h=nh)
                P.op("dve", lambda e, xv=xv, xg=xg, gsb=gsb, nh=nh: e.tensor_tensor(
                    out=xg, in0=xv, in1=gsb.unsqueeze(1).to_broadcast([128, nh, 64]), op=ALU.mult),
                    reads=[("ps", src_b), gkey], writes=[("tA1", dst_lo)])
                xg4 = tA1[:, dst_lo:dst_lo + nh * 64].rearrange("p (h a b d) -> p h a b d", h=nh, a=2, b=2)
                t24 = tA2[:, dst_lo:dst_lo + nh * 64].rearrange("p (h a b d) -> p h a b d", h=nh, a=2, b=2)
                S4 = Stab.rearrange("p (a b d) -> p a b d", a=2, b=2)
                for bb in range(2):
                    P.op("dve", lambda e, bb=bb, xg4=xg4, t24=t24, S4=S4, nh=nh: e.tensor_tensor(
                        out=t24[:, :, :, bb, :], in0=xg4[:, :, :, 1 - bb, :],
                        in1=S4[:, :, bb, :].unsqueeze(1).to_broadcast([128, nh, 2, 16]), op=ALU.mult),
                        reads=[("tA1", dst_lo), ("ropeA", slot)], writes=[("tA2", dst_lo, bb)])
                P.op("dve", lambda e, xg=xg, nh=nh, Ctab=Ctab: e.tensor_tensor(
                    out=xg, in0=xg, in1=Ctab.unsqueeze(1).to_broadcast([128, nh, 64]), op=ALU.mult),
                    reads=[("tA1", dst_lo), ("ropeA", slot), ("tA2", dst_lo, 0), ("tA2", dst_lo, 1)], writes=[("tA1", dst_lo)])
                t2v = tA2[:, dst_lo:dst_lo + nh * 64].rearrange("p (h d) -> p h d", h=nh)
                P.op("dve", lambda e, xg=xg, t2v=t2v: e.tensor_tensor(out=xg, in0=xg, in1=t2v, op=ALU.add),
                     reads=[("tA1", dst_lo), ("tA2", dst_lo, 0), ("tA2", dst_lo, 1)], writes=[("tA1", dst_lo)])
            qk3 = qkA_bf.rearrange("p (j g d) -> p j g d", j=5, g=2)
            for h in range(10):
                if h < 8:
                    j, g = h % 4, h // 4
                else:
                    j, g = 4, h - 8
                P.op("act", lambda e, h=h, j=j, g=g: e.activation(
                    out=qk3[:, j, g, :], in_=tA1[:, h * 64:(h + 1) * 64], func=AF.Copy, scale=rstdA[:, h:h + 1]),
                    reads=[("tA1", 0), ("tA1", 512), "rstdA"], writes=[("qkA", h)])
            bT = next_bank()
            for j in range(5):
                P.op("pe", lambda e, j=j, bT=bT: e.transpose(psb[bT][:, j * 128:(j + 1) * 128],
                                                             qkA_bf[:, j * 128:(j + 1) * 128], identB),
                     reads=[("qkA", h) for h in range(10)] + ["identB"], writes=[("ps", bT)])
            P.op("act", lambda e, bT=bT, tc=tc: e.activation(
                out=QTA[:, :, tc * 128:(tc + 1) * 128],
                in_=psb[bT][:, 0:512].rearrange("p (j t) -> p j t", j=4), func=AF.Copy),
                reads=[("ps", bT)], writes=[("QTA", tc)])
            P.op("act", lambda e, bT=bT, tc=tc: e.activation(
                out=KTA[:, tc * 128:(tc + 1) * 128], in_=psb[bT][:, 512:640], func=AF.Copy),
                reads=[("ps", bT)], writes=[("KTA", tc)])
            P.op("act", lambda e, bKV=bKV, tc=tc: e.activation(
                out=VA[:, tc, :, 64:128], in_=ps[bKV][:, 128:256].rearrange("p (k d) -> p k d", k=2), func=AF.Copy),
                reads=[("ps", bKV)], writes=[("VA", tc)])
            CB = ropeT[:, slot, 1, 0, :]
            SB = ropeT[:, slot, 1, 1, :]
            for qi, bsrc in enumerate((bQ, bK)):
                xv = ps[bsrc][:, 0:512].rearrange("p (h d) -> p h d", h=8)
                xv4 = ps[bsrc][:, 0:512].rearrange("p (h b d) -> p h b d", h=8, b=2)
                t1v = tB1.rearrange("p (h d) -> p h d", h=8)
                t24 = tB2.rearrange("p (h b d) -> p h b d", h=8, b=2)
                S3 = SB.rearrange("p (b d) -> p b d", b=2)
                P.op("dve", lambda e, xv=xv, t1v=t1v, CB=CB: e.tensor_tensor(
                    out=t1v, in0=xv, in1=CB.unsqueeze(1).to_broadcast([128, 8, 64]), op=ALU.mult),
                    reads=[("ps", bsrc), ("ropeB", slot)], writes=["tB1"])
                for bb in range(2):
                    P.op("dve", lambda e, bb=bb, xv4=xv4, t24=t24, S3=S3: e.tensor_tensor(
                        out=t24[:, :, bb, :], in0=xv4[:, :, 1 - bb, :],
                        in1=S3[:, bb, :].unsqueeze(1).to_broadcast([128, 8, 32]), op=ALU.mult),
                        reads=[("ps", bsrc), ("ropeB", slot)], writes=[("tB2", bb)])
                dstb = qkB_bf[:, qi * 512:(qi + 1) * 512]
                P.op("dve", lambda e, dstb=dstb: e.tensor_tensor(out=dstb, in0=tB1, in1=tB2, op=ALU.add),
                     reads=["tB1", ("tB2", 0), ("tB2", 1)], writes=[("qkB", qi)])
            bT2 = next_bank()
            for j in range(8):
                P.op("pe", lambda e, j=j, bT2=bT2: e.transpose(psb[bT2][:, j * 128:(j + 1) * 128],
                                                               qkB_bf[:, j * 128:(j + 1) * 128], identB),
                     reads=[("qkB", 0), ("qkB", 1), "identB"], writes=[("ps", bT2)])
            P.op("act", lambda e, bT2=bT2, tc=tc: e.activation(
                out=QKTB[:, :, tc * 128:(tc + 1) * 128],
                in_=psb[bT2][:].rearrange("p (j t) -> p j t", j=8), func=AF.Copy),
                reads=[("ps", bT2)], writes=[("QKTB", tc)])
            P.op("act", lambda e, bV=bV, tc=tc: e.activation(out=VB[:, tc, :], in_=ps[bV][:, 0:512], func=AF.Copy),
                 reads=[("ps", bV)], writes=[("VB", tc)])

        if stage == "p1b":
            def dump(view, off, n, keys):
                P.op("pool", lambda e: e.dma_start(out=dbg_d[:, off:off + n], in_=view), reads=keys, writes=[("dbgout", off)], dma=True)
            dump(bfv(R_OFF, 4096), 0, 8192, [("QTA", t) for t in range(NT)])
            dump(KTA, 8192, 2048, [("KTA", t) for t in range(NT)])
            dump(bfv(R_OFF + 5120, 8192), 10240, 16384, [("QKTB", t) for t in range(NT)])
            dump(bfv(R_OFF + 13312, 3072), 26624, 6144, [("VA", t) for t in range(NT)] + ["VAones0", "VAones1"])
            dump(bfv(HT_OFF, 4096), 32768, 8192, [("VB", t) for t in range(NT)])
            P.wait_tokens("sp", [P.writer[("dbgout", o)] for o in (0, 8192, 10240, 26624, 32768)])
            with nc.Block() as block:
                P.emit(block, nc, esem, dsem)
            return nc


        def finish(final_toks):
            P.wait_tokens("sp", final_toks)
            print("milestones", {e: len(P.milestones[e]) for e in P.ENG}, "ops", P.nops, "dma", P.dma_n)
            with nc.Block() as block:
                P.emit(block, nc, esem, dsem)
            return nc

        bst = f32v(T1, 2048).rearrange("p (a d) -> p a d", a=2)
        P.alias(["bst"], ["sqA_q", "sqA_k", "ssA", "rstdA", ("tA1", 0), ("tA1", 512), ("tA2", 0, 0), ("tA2", 0, 1),
                          ("tA2", 512, 0), ("tA2", 512, 1), "tB1", ("tB2", 0), ("tB2", 1)])
        P.op("sp", lambda e: e.dma_start(out=bst[0:32, 0, :], in_=beg_d), writes=["bst"], dma=True)
        P.op("sp", lambda e: e.dma_start(out=bst[0:32, 1, :], in_=beu_d), reads=["bst"], writes=["bst2"], dma=True)
        for a, (dstT, sc1, sc2) in enumerate(((bgT, -1.0, 7.0), (buT, 1.0, 7.0))):
            b = next_bank()
            for fc in range(8):
                P.op("pe", lambda e, b=b, fc=fc, a=a: e.transpose(ps[b][:, fc * 32:(fc + 1) * 32],
                                                                  bst[0:32, a, fc * 128:(fc + 1) * 128], identF[0:32, 0:32]),
                     reads=["bst", "bst2", "identF"], writes=[("ps", b)])
            P.op("dve", lambda e, b=b, dstT=dstT, sc1=sc1, sc2=sc2: e.tensor_scalar(
                out=dstT, in0=ps[b][:, 0:256].rearrange("p (f e) -> p e f", f=8), scalar1=sc1, scalar2=sc2,
                op0=ALU.mult, op1=ALU.add), reads=[("ps", b)], writes=[("bT", a)])
        wkv = bfv(XT_OFF, 8192).rearrange("p (c n) -> p c n", c=8)
        P.alias(["wkv"], XTk(range(NT)))
        P.op("pool", lambda e: e.dma_start(out=wkv, in_=wkv_d.rearrange("(c p) n -> p c n", p=128)), writes=["wkv"], dma=True)

        lamv = f32v(LNP_OFF + 512, 256).rearrange("p (a b d) -> p a b d", a=2, b=2)
        P.op("sp", lambda e: e.dma_start(out=lamv, in_=lam_d.rearrange("(a b) d -> a b d", a=2).partition_broadcast(128)),
             writes=["lamv"], dma=True)
        P.op("sp", lambda e: e.dma_start(out=gs_col, in_=subln_d), writes=["gs"], dma=True)
        P.op("dve", lambda e: e.tensor_tensor(out=lamv[:, :, 0, :], in0=lamv[:, :, 0, :], in1=lamv[:, :, 1, :], op=ALU.mult),
             reads=["lamv"], writes=["lamv"])
        P.op("dve", lambda e: e.reduce_sum(out=lsum[:, 0:2], in_=lamv[:, :, 0, :], axis=AX.X), reads=["lamv"], writes=["lsum"])
        P.op("act", lambda e: e.activation(out=lsum[:, 2:4], in_=lsum[:, 0:2], func=AF.Exp), reads=["lsum"], writes=["lsum2"])
        P.op("dve", lambda e: e.tensor_tensor(out=lam_col, in0=lsum[:, 2:3], in1=lsum[:, 3:4], op=ALU.subtract),
             reads=["lsum2"], writes=["lam"])
        P.op("dve", lambda e: e.tensor_scalar(out=nlam_col, in0=lam_col, scalar1=LAMBDA_INIT, scalar2=-1.0, op0=ALU.add, op1=ALU.mult),
             reads=["lam"], writes=["nlam"])
        P.op("dve", lambda e: e.tensor_scalar(out=gs_col, in0=gs_col, scalar1=1.0 - LAMBDA_INIT, scalar2=None, op0=ALU.mult),
             reads=["gs"], writes=["gs"])

        wmix = bfv(HT_OFF + 4096, 4096).rearrange("p (c n) -> p c n", c=8)
        P.op("pool", lambda e: e.dma_start(out=wmix, in_=wmix_d.rearrange("(c p) n -> p c n", p=128)), writes=["wmix"], dma=True)

        for g8 in range(NSLOT // 2560):
            P.op("sp", lambda e, g8=g8: e.dma_start(out=yg_d[g8 * 2560:(g8 + 1) * 2560, :], in_=zeros_d[:, :]),
                 writes=[("YGZ", g8)], dma=True)
        catT = bfv(WR_OFF, 8192).rearrange("p (c t) -> p c t", c=8)
        wi_keys = [("wi", i) for i in range(5)]
        t1_keys = ["sqA_q", "sqA_k", "ssA", "rstdA", ("tA1", 0), ("tA1", 512), ("tA2", 0, 0), ("tA2", 0, 1),
                   ("tA2", 512, 0), ("tA2", 512, 1), "tB1", ("tB2", 0), ("tB2", 1)]
        cat_keys = [("catT", c, qt) for c in range(8) for qt in range(4)]
        P.alias(cat_keys, wi_keys)
        denA = [f32v(T1 + i * 512, 512) for i in range(2)]
        sq_bf = bfv(T1 + 1024, 256)
        rstdB = f32v(T1 + 1280, 512)
        P.alias([("denA", 0), ("denA", 1), "sq_bf", "rstdB"], t1_keys)
        Bt = [f32v(TMP_OFF + i * 512, 512) for i in range(4)]
        P.alias([("Bt", i) for i in range(4)], [("xin", 0), ("xin", 1)])
        P.alias([("E", i) for i in range(4)], [("qkB", 0), ("qkB", 1)] + [("qkA", h) for h in range(10)])
        allT = list(range(NT))
        stepsA = [(j, qt, sc) for j in range(4) for qt in range(4) for sc in range(NT)]

        def A_S(i):
            j, qt, sc = stepsA[i]
            qs = slice(qt * 512, (qt + 1) * 512)
            pair = i % 2
            for g in range(2):
                sb = 2 * pair + g
                kp = slice(g * 64, (g + 1) * 64)
                P.op("pe", lambda e, sb=sb, kp=kp: e.matmul(ps[sb][:, :], lhsT=KTA[kp, sc * 128:(sc + 1) * 128], rhs=QTA[kp, j, qs],
                                                            start=True, stop=True),
                     reads=[("KTA", sc)] + [("QTA", t) for t in range(qt * 4, qt * 4 + 4)], writes=[("ps", sb)])
            for g in range(2):
                sb = 2 * pair + g
                P.op("act", lambda e, sb=sb: e.activation(out=Ering[sb], in_=ps[sb][:, :], func=AF.Exp, scale=0.125),
                     reads=[("ps", sb)], writes=[("E", sb)])

        def A_PV(i):
            j, qt, sc = stepsA[i]
            qs = slice(qt * 512, (qt + 1) * 512)
            pair = i % 2
            grp = i // NT
            odd = j % 2
            for g in range(2):
                h = j + 4 * g
                c = h // 2
                es = 2 * pair + g
                ob = 4 + 2 * (grp % 2) + g
                vsl = VA[:, sc, g, 0:128] if odd else VA[:, sc, g, 64:192]
                P.op("pe", lambda e, ob=ob, vsl=vsl, es=es: e.matmul(ps[ob][:, :], lhsT=vsl, rhs=Ering[es], start=(sc == 0), stop=(sc == NT - 1)),
                     reads=[("VA", sc), "VAones0", "VAones1", ("E", es)], writes=[("ps", ob)])
            if sc == NT - 1:
                op_ = slice(odd * 64, odd * 64 + 64)
                dp_ = slice((1 - odd) * 64, (1 - odd) * 64 + 64)
                for g in range(2):
                    h = j + 4 * g
                    c = h // 2
                    ob = 4 + 2 * (grp % 2) + g
                    ds = g
                    P.op("act", lambda e, ob=ob, ds=ds: e.activation(out=denA[ds][op_, :], in_=ps[ob][dp_, :], func=AF.Copy),
                         reads=[("ps", ob)], writes=[("denA", ds)])
                    P.op("dve", lambda e, ds=ds: e.reciprocal(out=denA[ds][op_, :], in_=denA[ds][op_, :]),
                         reads=[("denA", ds)], writes=[("denA", ds)])
                    P.op("dve", lambda e, ob=ob, ds=ds, c=c: e.tensor_tensor(out=catT[op_, c, qs], in0=ps[ob][op_, :], in1=denA[ds][op_, :], op=ALU.mult),
                         reads=[("ps", ob), ("denA", ds)], writes=[("catT", c, qt)])

        for i in range(-1, len(stepsA)):
            if i + 1 < len(stepsA):
                A_S(i + 1)
            if i >= 0:
                A_PV(i)

        stepsB = [(h, qt, sc) for h in range(4) for qt in range(4) for sc in range(NT)]

        def B_S(i):
            h, qt, sc = stepsB[i]
            qs = slice(qt * 512, (qt + 1) * 512)
            pair = i % 2
            s1, s2 = 2 * pair, 2 * pair + 1
            rq = [("QKTB", t) for t in range(qt * 4, qt * 4 + 4)] + [("QKTB", sc)]
            P.op("pe", lambda e: e.matmul(ps[s1][:, :], lhsT=QKTB[0:64, 4 + h, sc * 128:(sc + 1) * 128], rhs=QKTB[0:64, h, qs], start=True, stop=True),
                 reads=rq, writes=[("ps", s1)])
            P.op("pe", lambda e: e.matmul(ps[s2][:, :], lhsT=QKTB[64:128, 4 + h, sc * 128:(sc + 1) * 128], rhs=QKTB[64:128, h, qs], start=True, stop=True),
                 reads=rq, writes=[("ps", s2)])
            P.op("act", lambda e: e.activation(out=Ering[s1], in_=ps[s1][:, :], func=AF.Exp, scale=0.125),
                 reads=[("ps", s1)], writes=[("E", s1)])
            P.op("act", lambda e: e.activation(out=Ering[s2], in_=ps[s2][:, :], func=AF.Exp, scale=0.125),
                 reads=[("ps", s2)], writes=[("E", s2)])

        def B_PV(i):
            h, qt, sc = stepsB[i]
            qs = slice(qt * 512, (qt + 1) * 512)
            pair = i % 2
            e1, e2 = 2 * pair, 2 * pair + 1
            vsl = VB[:, sc, h * 128:(h + 1) * 128]
            st_, sp_ = (sc == 0), (sc == NT - 1)
            P.op("pe", lambda e: e.matmul(ps[4][:, :], lhsT=vsl, rhs=Ering[e1], start=st_, stop=sp_),
                 reads=[("VB", sc), ("E", e1)], writes=[("ps", 4)])
            P.op("pe", lambda e: e.matmul(ps[6][:, :], lhsT=onesB, rhs=Ering[e1], start=st_, stop=sp_),
                 reads=["onesB", ("E", e1)], writes=[("ps", 6)])
            P.op("pe", lambda e: e.matmul(ps[5][:, :], lhsT=vsl, rhs=Ering[e2], start=st_, stop=sp_),
                 reads=[("VB", sc), ("E", e2)], writes=[("ps", 5)])
            P.op("pe", lambda e: e.matmul(ps[7][:, :], lhsT=onesB, rhs=Ering[e2], start=st_, stop=sp_),
                 reads=["onesB", ("E", e2)], writes=[("ps", 7)])
            if sc == NT - 1:
                P.op("dve", lambda e: e.reciprocal(out=Bt[0], in_=ps[6][:, :]), reads=[("ps", 6)], writes=[("Bt", 0)])
                P.op("dve", lambda e: e.tensor_tensor(out=Bt[2], in0=ps[4][:, :], in1=Bt[0], op=ALU.mult),
                     reads=[("ps", 4), ("Bt", 0)], writes=[("Bt", 2)])
                P.op("dve", lambda e: e.reciprocal(out=Bt[1], in_=ps[7][:, :]), reads=[("ps", 7)], writes=[("Bt", 1)])
                P.op("dve", lambda e: e.scalar_tensor_tensor(out=Bt[3], in0=ps[5][:, :], scalar=nlam_col, in1=Bt[1], op0=ALU.mult, op1=ALU.mult),
                     reads=[("ps", 5), ("Bt", 1), "nlam"], writes=[("Bt", 3)])
                P.op("dve", lambda e: e.tensor_tensor(out=Bt[2], in0=Bt[2], in1=Bt[3], op=ALU.add),
                     reads=[("Bt", 2), ("Bt", 3)], writes=[("Bt", 2)])
                P.op("dve", lambda e: e.tensor_tensor(out=sq_bf, in0=Bt[2], in1=Bt[2], op=ALU.mult),
                     reads=[("Bt", 2)], writes=["sq_bf"])
                P.op("pe", lambda e: e.matmul(ps[7][:, :], lhsT=onesB, rhs=sq_bf, start=True, stop=True),
                     reads=["onesB", "sq_bf"], writes=[("ps", 7)])
                P.op("act", lambda e: e.activation(out=rstdB, in_=ps[7][:, :], func=AF.Ln, scale=1.0 / 128.0, bias=rmseps_col),
                     reads=[("ps", 7), "eps2"], writes=["rstdB"])
                P.op("act", lambda e: e.activation(out=rstdB, in_=rstdB, func=AF.Exp, scale=-0.5), reads=["rstdB"], writes=["rstdB"])
                P.op("dve", lambda e: e.scalar_tensor_tensor(
                    out=catT[:, 4 + h, qs], in0=Bt[2], scalar=gs_col, in1=rstdB, op0=ALU.mult, op1=ALU.mult),
                    reads=[("Bt", 2), "rstdB", "gs"], writes=[("catT", 4 + h, qt)])

        for i in range(-1, len(stepsB)):
            if i + 1 < len(stepsB):
                B_S(i + 1)
            if i >= 0:
                B_PV(i)

        if stage == "p1d":
            P.op("pool", lambda e: e.dma_start(out=dbg_d[:, 0:16384], in_=bfv(WR_OFF, 8192)), reads=cat_keys, writes=["dbgout"], dma=True)
            return finish([P.writer["dbgout"]])

        mem_st = f32v(TMP_OFF, 2048).rearrange("p (c d) -> p c d", c=2)
        P.alias(["mem_st"], [("Bt", i) for i in range(4)])
        P.op("sp", lambda e: e.dma_start(out=mem_st, in_=mem_d.rearrange("(c p) d -> p c d", p=128)), writes=["mem_st"], dma=True)
        KmT = bfv(HT_OFF, 1024).rearrange("p (h j m) -> p h j m", h=4, j=2)
        Vm = bfv(HT_OFF + 1024, 1024).rearrange("p (c n) -> p c n", c=2)
        memT = bfv(HT_OFF + 2048, 1024).rearrange("p (c m) -> p c m", c=8)
        P.alias(["KmT", "Vm", "memT"], [("VB", t) for t in allT])
        for mc in range(2):
            for half in range(2):
                b = next_bank()
                for jj in range(4):
                    dc = half * 4 + jj
                    P.op("pe", lambda e, b=b, jj=jj, dc=dc, mc=mc: e.transpose(
                        ps[b][:, jj * 128:(jj + 1) * 128], mem_st[:, mc, dc * 128:(dc + 1) * 128], identF),
                        reads=["mem_st", "identF"], writes=[("ps", b)])
                P.op("act", lambda e, b=b, half=half, mc=mc: e.activation(
                    out=memT[:, half * 4:(half + 1) * 4, mc * 128:(mc + 1) * 128],
                    in_=ps[b][:].rearrange("p (j t) -> p j t", j=4), func=AF.Copy),
                    reads=[("ps", b)], writes=[("memT", mc, half)])
        memT_keys = [("memT", mc, half) for mc in range(2) for half in range(2)]
        for h in range(4):
            for j in range(2):
                b = next_bank()
                for dc in range(8):
                    P.op("pe", lambda e, b=b, dc=dc, h=h, j=j: e.matmul(
                        ps[b][:, 0:256], lhsT=wkv[:, dc, h * 256 + j * 128:h * 256 + (j + 1) * 128], rhs=memT[:, dc, :],
                        start=(dc == 0), stop=(dc == 7)), reads=["wkv"] + memT_keys, writes=[("ps", b)])
                P.op("act", lambda e, b=b, h=h, j=j: e.activation(out=KmT[:, h, j, :], in_=ps[b][:, 0:256], func=AF.Copy),
                     reads=[("ps", b)], writes=[("KmT", h, j)])
        for mc in range(2):
            for half in range(2):
                b = next_bank()
                for dc in range(8):
                    P.op("pe", lambda e, b=b, dc=dc, mc=mc, half=half: e.matmul(
                        ps[b][:, :], lhsT=memT[:, dc, mc * 128:(mc + 1) * 128], rhs=wkv[:, dc, 1024 + half * 512:1024 + (half + 1) * 512],
                        start=(dc == 0), stop=(dc == 7)), reads=["wkv"] + memT_keys, writes=[("ps", b)])
                P.op("dve", lambda e, b=b, mc=mc, half=half: e.tensor_copy(out=Vm[:, mc, half * 512:(half + 1) * 512], in_=ps[b][:, :]),
                     reads=[("ps", b)], writes=[("Vm", mc, half)])
        KmT_keys = [("KmT", h, j) for h in range(4) for j in range(2)]
        Vm_keys = [("Vm", mc, half) for mc in range(2) for half in range(2)]
        P.alias(XTk(range(NT)), ["wkv"])

        P.alias(["lnp"], [("ropeA", 0), ("ropeA", 1), ("ropeB", 0), ("ropeB", 1), "lamv"])
        P.op("sp", lambda e: e.dma_start(out=lnp, in_=ln_d[0:2, :].partition_broadcast(128)), writes=["lnp"], dma=True)
        att_keys = ([("QTA", t) for t in allT] + [("KTA", t) for t in allT] + [("QKTB", t) for t in allT] +
                    [("VA", t) for t in allT] + ["VAones0", "VAones1"])
        P.alias([("R", t) for t in allT], att_keys)
        ytile = [f32v(T1 + i * 1024, 1024) for i in range(2)]
        xn = f32v(T1 + 2048, 1024)
        P.alias([("y", 0), ("y", 1), "xn"], [("denA", 0), ("denA", 1), "sq_bf", "rstdB"])
        P.alias([("xin", 0), ("xin", 1)], [("Bt", i) for i in range(4)] + ["mem_st"])
        stats = small[:, 16:28].rearrange("p (c s) -> p c s", c=2)
        mv = small[:, 28:30]
        rstd1 = small[:, 30:31]
        nmr1 = small[:, 31:32]

        small2 = f32v(misc(64), 64)

        def ln_stats(ysrc, ykey, par):
            st_ = small2[:, par * 16:par * 16 + 12].rearrange("p (c s) -> p c s", c=2)
            mv_ = small2[:, par * 16 + 12:par * 16 + 14]
            rs_ = small2[:, par * 16 + 14:par * 16 + 15]
            nm_ = small2[:, par * 16 + 15:par * 16 + 16]
            P.op("dve", lambda e: e.bn_stats(out=st_[:, 0, :], in_=ysrc[:, 0:512]), reads=[ykey], writes=[("stats0", par)])
            P.op("dve", lambda e: e.bn_stats(out=st_[:, 1, :], in_=ysrc[:, 512:1024]), reads=[ykey], writes=[("stats1", par)])
            P.op("dve", lambda e: e.bn_aggr(out=mv_, in_=st_), reads=[("stats0", par), ("stats1", par)], writes=[("mv", par)])
            P.op("act", lambda e: e.activation(out=rs_, in_=mv_[:, 1:2], func=AF.Ln, bias=eps_col), reads=[("mv", par), "eps"], writes=[("rstd1", par)])
            P.op("act", lambda e: e.activation(out=rs_, in_=rs_, func=AF.Exp, scale=-0.5), reads=[("rstd1", par)], writes=[("rstd1", par)])
            P.op("dve", lambda e: e.scalar_tensor_tensor(out=nm_, in0=mv_[:, 0:1], scalar=-1.0, in1=rs_, op0=ALU.mult, op1=ALU.mult),
                 reads=[("mv", par), ("rstd1", par)], writes=[("nmr1", par)])

        def ln_apply(ysrc, ykey, par, dst, dst_keys, lnkey, xnb, xnk):
            rs_ = small2[:, par * 16 + 14:par * 16 + 15]
            nm_ = small2[:, par * 16 + 15:par * 16 + 16]
            P.op("act", lambda e: e.activation(out=xnb, in_=ysrc, func=AF.Identity, scale=rs_, bias=nm_),
                 reads=[ykey, ("rstd1", par), ("nmr1", par)], writes=[xnk])
            P.op("dve", lambda e: e.tensor_tensor(out=xnb, in0=xnb, in1=lnp[:, 0, :], op=ALU.mult), reads=[xnk, lnkey], writes=[xnk])
            P.op("dve", lambda e: e.tensor_tensor(out=dst, in0=xnb, in1=lnp[:, 1, :], op=ALU.add), reads=[xnk, lnkey], writes=dst_keys)

        def skewed(n, stages):
            ns = len(stages)
            for t in range(n + ns - 1):
                for si in range(ns - 1, -1, -1):
                    k = t - si
                    if 0 <= k < n:
                        stages[si](k)

        xnA = [xn, f32v(TMP_OFF + 2048, 1024)]
        P.alias([("xnA", 0), ("xnA", 1)], ["xn"] + [("E", i) for i in range(4)])

        def l1_s0(tc):
            par = tc % 2
            load_x(tc, par)
            for dh in range(2):
                b = par * 2 + dh
                for c in range(8):
                    P.op("pe", lambda e, b=b, c=c, dh=dh: e.matmul(
                        ps[b][:, :], lhsT=catT[:, c, tc * 128:(tc + 1) * 128], rhs=wmix[:, c, dh * 512:(dh + 1) * 512],
                        start=(c == 0), stop=(c == 7)),
                        reads=[("catT", c, tc // 4), "wmix"], writes=[("ps", b)])
                P.op("dve", lambda e, b=b, dh=dh: e.scalar_tensor_tensor(
                    out=ytile[par][:, dh * 512:(dh + 1) * 512], in0=xin[par][:, dh * 512:(dh + 1) * 512], scalar=ALPHA,
                    in1=ps[b][:, :], op0=ALU.mult, op1=ALU.add),
                    reads=[("xin", par), ("ps", b)], writes=[("y", par, dh)])

        def l1_s1(tc):
            par = tc % 2
            P.op("dve", lambda e: e.engine_nop(), reads=[("y", par, 0), ("y", par, 1)], writes=[("y", par)])
            ln_stats(ytile[par], ("y", par), par)

        def l1_s2(tc):
            par = tc % 2
            ln_apply(ytile[par], ("y", par), par, Rt[:, tc, :], [("R", tc)], "lnp", xnA[par], ("xnA", par))
            P.alias([("y", par, 0), ("y", par, 1)], [("y", par)])

        def l1_s3(tc):
            par = tc % 2
            transpose_tile_to_XT(Rt[:, tc, :], ("R", tc), tc, (4 + par * 2, 5 + par * 2))

        skewed(NT, [l1_s0, l1_s1, l1_s2, l1_s3])

        if stage == "p1":
            toks = []
            for tc in range(NT):
                toks.append(P.op("sp", lambda e, tc=tc: e.dma_start(out=out_d[tc * 128:(tc + 1) * 128, :], in_=Rt[:, tc, :]),
                                 reads=[("R", tc)], writes=[("out", tc)], dma=True))
            return finish(toks)


        wq = bfv(WR_OFF, 4096).rearrange("p (c n) -> p c n", c=8)
        wo = bfv(WR_OFF + 4096, 4096).rearrange("p (c n) -> p c n", c=8)
        P.alias(["wq", "wo"], cat_keys)
        P.op("pool", lambda e: e.dma_start(out=wq, in_=wq_d.rearrange("(c p) n -> p c n", p=128)), writes=["wq"], dma=True)
        P.op("pool", lambda e: e.dma_start(out=wo, in_=wo_d.rearrange("(c p) n -> p c n", p=128)), writes=["wo"], dma=True)
        qT_tile = bfv(WR_OFF + 8192, 2048).rearrange("p (c t) -> p c t", c=8)
        oT_tile = bfv(WR_OFF + 10240, 2048).rearrange("p (c t) -> p c t", c=8)
        P.alias([("qT", c) for c in range(8)] + [("oT", c) for c in range(8)], [("y", 0), ("y", 1), ("y", 0, 0), ("y", 0, 1), ("y", 1, 0), ("y", 1, 1), "xn"])
        bd_sb = f32v(HT_OFF + 2048, 1024)
        y2 = f32v(HT_OFF + 3072, 1024)
        xn2 = f32v(HT_OFF + 4096, 1024)
        x2t = f32v(HT_OFF + 5120, 1024)
        x2Tf = f32v(HT_OFF + 6144, 1024).rearrange("p (c t) -> p c t", c=8)
        CTc = f32v(HT_OFF + 7168, 128)
        gsm = f32v(HT_OFF + 7296, 128)
        gsm2 = f32v(HT_OFF + 7424, 256)
        idx8 = gsm2[:, 0:8].bitcast(U32)
        idxf = gsm2[:, 8:12]
        posk = gsm2[:, 12:16]
        destf = gsm2[:, 16:20]
        ovf = gsm2[:, 20:24]
        gk4 = gsm2[:, 24:28]
        msk_bf = gsm2[:, 32:48].bitcast(BF16)
        posf = gsm2[:, 48:80]
        sel4 = gsm2[:, 96:224].rearrange("p (k e) -> p k e", k=4)
        lg = gsm[:, 0:32]
        top8 = gsm[:, 32:40]
        negm = gsm[:, 40:41]
        ssum = gsm[:, 41:42]
        msk = gsm[:, 48:80]
        exg = gsm[:, 80:112]
        P.alias(["bd", "y2", "xn2", "x2t", "x2Tf", "CTc", "gsm", "idx8", "idxf", "posk", "destf", "ovf", "gk4", "msk_bf", "posf", "sel4"], ["memT", "wmix"] + memT_keys)
        P.op("sp", lambda e: e.dma_start(out=wr_sb, in_=wr_d.rearrange("(c p) n -> p c n", p=128)), writes=["wr"], dma=True)
        P.op("sp", lambda e: e.dma_start(out=br_sb, in_=br_d.partition_broadcast(128)), writes=["br"], dma=True)
        P.op("sp", lambda e: e.dma_start(out=lnp, in_=ln_d[2:4, :].partition_broadcast(128)), writes=["lnp"], dma=True)
        dent = [f32v(TMP_OFF + i * 512, 512) for i in range(2)]
        P.alias([("dent", 0), ("dent", 1)], [("xin", 0), ("xin", 1), "mem_st"])
        e_rr2 = [0]

        xb_rr = [0]

        def xbank():
            v = xb_rr[0] % 6
            xb_rr[0] += 1
            return v

        def xattn_tile(tt):
            ts_ = slice(tt * 512, (tt + 1) * 512)
            tcs = list(range(tt * 4, tt * 4 + 4))
            for c in range(8):
                b = xbank()
                for dc in range(8):
                    P.op("pe", lambda e, b=b, dc=dc, c=c, ts_=ts_: e.matmul(
                        ps[b][:, :], lhsT=wq[:, dc, c * 128:(c + 1) * 128], rhs=XT[:, dc, ts_], start=(dc == 0), stop=(dc == 7)),
                        reads=["wq"] + XTk(tcs), writes=[("ps", b)])
                if c % 2 == 0:
                    P.op("act", lambda e, b=b, c=c: e.activation(out=qT_tile[:, c, :], in_=ps[b][:, :], func=AF.Copy),
                         reads=[("ps", b)], writes=[("qT", c)])
                else:
                    P.op("dve", lambda e, b=b, c=c: e.tensor_copy(out=qT_tile[:, c, :], in_=ps[b][:, :]),
                         reads=[("ps", b)], writes=[("qT", c)])
            for h in range(4):
                eslots = []
                for mc in range(2):
                    sb = xbank()
                    for j in range(2):
                        P.op("pe", lambda e, sb=sb, h=h, j=j, mc=mc: e.matmul(
                            ps[sb][:, :], lhsT=KmT[:, h, j, mc * 128:(mc + 1) * 128], rhs=qT_tile[:, h * 2 + j, :],
                            start=(j == 0), stop=(j == 1)), reads=KmT_keys + [("qT", h * 2 + j)], writes=[("ps", sb)])
                    es = e_rr2[0] % 4
                    e_rr2[0] += 1
                    eslots.append(es)
                    P.op("act", lambda e, sb=sb, es=es: e.activation(out=Ering[es], in_=ps[sb][:, :], func=AF.Exp, scale=1.0 / 16.0),
                         reads=[("ps", sb)], writes=[("E", es)])
                obs = []
                for j in range(2):
                    ob = xbank()
                    obs.append(ob)
                    for mc in range(2):
                        P.op("pe", lambda e, ob=ob, mc=mc, h=h, j=j, es=eslots[mc]: e.matmul(
                            ps[ob][:, :], lhsT=Vm[:, mc, h * 256 + j * 128:h * 256 + (j + 1) * 128], rhs=Ering[es],
                            start=(mc == 0), stop=(mc == 1)), reads=Vm_keys + [("E", eslots[mc])], writes=[("ps", ob)])
                db = xbank()
                for mc in range(2):
                    P.op("pe", lambda e, db=db, mc=mc, es=eslots[mc]: e.matmul(
                        ps[db][:, :], lhsT=onesB, rhs=Ering[es], start=(mc == 0), stop=(mc == 1)),
                        reads=["onesB", ("E", eslots[mc])], writes=[("ps", db)])
                ds = h % 2
                P.op("dve", lambda e, db=db, ds=ds: e.reciprocal(out=dent[ds], in_=ps[db][:, :]), reads=[("ps", db)], writes=[("dent", ds)])
                for j in range(2):
                    P.op("dve", lambda e, ob=obs[j], ds=ds, h=h, j=j: e.tensor_tensor(
                        out=oT_tile[:, h * 2 + j, :], in0=ps[ob][:, :], in1=dent[ds], op=ALU.mult),
                        reads=[("ps", obs[j]), ("dent", ds)], writes=[("oT", h * 2 + j)])

        y2b = [f32v(HT_OFF + 3072 + i * 1024, 1024) for i in range(2)]
        x2tb = [f32v(HT_OFF + 5120 + i * 1024, 1024) for i in range(2)]
        x2Tf = f32v(TMP_OFF + 1024, 1024).rearrange("p (c t) -> p c t", c=8)
        P.alias([("y2b", 0), ("y2b", 1), ("x2tb", 0), ("x2tb", 1), ("x2Tf", 0), ("x2Tf", 1)],
                ["y2", "xn2", "x2t", "x2Tf", "memT", "wmix"] + memT_keys + [("xnA", 1)])

        Wring = [bfv(WR_OFF + i * 2048, 2048).rearrange("p (c n) -> p c n", c=8) for i in range(6)]
        wsrc = (weg_d, weu_d, wed_d)

        def unit_src(e_, k):
            if k < 4:
                t = wsrc[k % 2][e_]
                half = k // 2
            else:
                t = wsrc[2][e_]
                half = k - 4
            return t.rearrange("(c p) n -> p c n", p=128)[:, :, half * 512:(half + 1) * 512]

        def load_unit(e_, k):
            P.op("pool", lambda e, e_=e_, k=k: e.dma_start(out=Wring[k], in_=unit_src(e_, k)), writes=[("W", k)], dma=True)


        def p2_s0(tc):
            par = tc % 2
            if tc % 4 == 0:
                xattn_tile(tc // 4)
                if tc == 12:
                    P.alias([("W", 0), ("W", 1)], ["wq"])
                    load_unit(0, 0)
                    load_unit(0, 1)
            tcl = tc % 4
            for dh in range(2):
                b = par * 2 + dh
                for c in range(8):
                    P.op("pe", lambda e, b=b, c=c, dh=dh: e.matmul(
                        ps[b][:, :], lhsT=oT_tile[:, c, tcl * 128:(tcl + 1) * 128], rhs=wo[:, c, dh * 512:(dh + 1) * 512],
                        start=(c == 0), stop=(c == 7)), reads=[("oT", c), "wo"], writes=[("ps", b)])
                P.op("dve", lambda e, b=b, dh=dh: e.scalar_tensor_tensor(
                    out=y2b[par][:, dh * 512:(dh + 1) * 512], in0=Rt[:, tc, dh * 512:(dh + 1) * 512], scalar=ALPHA,
                    in1=ps[b][:, :], op0=ALU.mult, op1=ALU.add), reads=[("R", tc), ("ps", b)], writes=[("y2b", par, dh)])

        def p2_s1(tc):
            par = tc % 2
            P.op("dve", lambda e: e.engine_nop(), reads=[("y2b", par, 0), ("y2b", par, 1)], writes=[("y2b", par)])
            ln_stats(y2b[par], ("y2b", par), par)

        def p2_s2(tc):
            par = tc % 2
            ln_apply(y2b[par], ("y2b", par), par, x2tb[par], [("x2tb", par)], "lnp", y2b[par], ("y2b", par))
            P.alias([("y2b", par, 0), ("y2b", par, 1)], [("y2b", par)])

        def p2_s2b(tc):
            par = tc % 2
            P.op("act", lambda e: e.activation(out=Rt[:, tc, :], in_=x2tb[par], func=AF.Copy, scale=ALPHA),
                 reads=[("x2tb", par)], writes=[("R", tc)])
            P.op("pool", lambda e: e.dma_start(out=xrows_d[tc * 128:(tc + 1) * 128, :], in_=x2tb[par]),
                 reads=[("x2tb", par)], writes=[("XROWS", tc)], dma=True)
            for half in range(2):
                b = 4 + half
                for jj in range(4):
                    dc = half * 4 + jj
                    P.op("pe", lambda e, b=b, jj=jj, dc=dc: e.transpose(ps[b][:, jj * 128:(jj + 1) * 128],
                                                                        x2tb[par][:, dc * 128:(dc + 1) * 128], identF),
                         reads=[("x2tb", par), "identF"], writes=[("ps", b)])
                srcp = ps[b][:].rearrange("p (j t) -> p j t", j=4)
                P.op("act", lambda e, half=half, srcp=srcp: e.activation(
                    out=x2Tf[:, half * 4:(half + 1) * 4, :], in_=srcp, func=AF.Copy),
                    reads=[("ps", b)], writes=[("x2Tf", half)])

        def p2_s2c(tc):
            par = tc % 2
            lb = 6 + par
            for dc in range(8):
                P.op("pe", lambda e, dc=dc: e.matmul(ps[lb][:, 0:32], lhsT=x2Tf[:, dc, :], rhs=wr_sb[:, dc, :],
                                                     start=(dc == 0), stop=(dc == 7)),
                     reads=[("x2Tf", 0), ("x2Tf", 1), "wr"], writes=[("ps", lb)])

        def p2_s3a(tc):
            par = tc % 2
            lb = 6 + par
            P.op("dve", lambda e: e.tensor_tensor(out=lg, in0=ps[lb][:, 0:32], in1=br_sb, op=ALU.add),
                 reads=[("ps", lb), "br"], writes=["lg"])
            P.op("dve", lambda e: e.max(out=top8, in_=lg), reads=["lg"], writes=["top8"])
            P.op("dve", lambda e: e.tensor_scalar(out=msk, in0=lg, scalar1=top8[:, 3:4], scalar2=None, op0=ALU.is_ge),
                 reads=["lg", "top8"], writes=["msk"])
            P.op("dve", lambda e: e.tensor_scalar(out=negm, in0=top8[:, 0:1], scalar1=-1.0, scalar2=None, op0=ALU.mult),
                 reads=["top8"], writes=["negm"])
            P.op("act", lambda e: e.activation(out=exg, in_=lg, func=AF.Exp, bias=negm), reads=["lg", "negm"], writes=["exg"])
            P.op("dve", lambda e: e.tensor_copy(out=msk_bf, in_=msk), reads=["msk"], writes=["msk_bf"])
            P.op("dve", lambda e: e.max_index(out=idx8, in_max=top8, in_values=lg), reads=["top8", "lg"], writes=["idx8"])
            P.op("dve", lambda e: e.tensor_copy(out=idxf, in_=idx8[:, 0:4]), reads=["idx8"], writes=["idxf"])
            P.op("dve", lambda e: e.tensor_tensor(out=exg, in0=exg, in1=msk, op=ALU.mult), reads=["exg", "msk"], writes=["exg"])
            P.op("dve", lambda e: e.reduce_sum(out=ssum, in_=exg, axis=AX.X), reads=["exg"], writes=["ssum"])
            P.op("dve", lambda e: e.reciprocal(out=ssum, in_=ssum), reads=["ssum"], writes=["ssum"])
            P.op("act", lambda e: e.activation(out=gk4, in_=top8[:, 0:4], func=AF.Exp, bias=negm), reads=["top8", "negm"], writes=["gk4"])
            P.op("dve", lambda e: e.tensor_scalar(out=gk4, in0=gk4, scalar1=ssum, scalar2=None, op0=ALU.mult),
                 reads=["gk4", "ssum"], writes=["gk4"])
            P.op("pe", lambda e: e.matmul(ps[lb][:, 64:96], lhsT=Ltri, rhs=msk_bf, start=True, stop=True),
                 reads=["Ltri", "msk_bf", "lg"], writes=[("ps", lb)])
            P.op("pe", lambda e: e.matmul(ps[lb][:, 96:128], lhsT=onesB, rhs=msk_bf, start=True, stop=True),
                 reads=["onesB", "msk_bf"], writes=[("ps", lb)])

        def p2_s3b(tc):
            par = tc % 2
            pb = 6 + par
            P.op("dve", lambda e: e.tensor_tensor(out=posf, in0=ps[pb][:, 64:96], in1=cnt, op=ALU.add),
                 reads=[("ps", pb), "cnt"], writes=["posf"])
            P.op("dve", lambda e: e.tensor_tensor(out=cnt, in0=ps[pb][:, 96:128], in1=cnt, op=ALU.add),
                 reads=[("ps", pb), "cnt", "posf"], writes=["cnt"])
            P.op("dve", lambda e: e.tensor_tensor(out=sel4, in0=iota32.unsqueeze(1).to_broadcast([128, 4, 32]),
                                                  in1=idxf.unsqueeze(2).to_broadcast([128, 4, 32]), op=ALU.is_equal),
                 reads=["iota32", "idxf"], writes=["sel4"])
            P.op("dve", lambda e: e.tensor_tensor(out=sel4, in0=sel4, in1=posf.unsqueeze(1).to_broadcast([128, 4, 32]), op=ALU.mult),
                 reads=["sel4", "posf"], writes=["sel4"])
            P.op("dve", lambda e: e.reduce_sum(out=posk, in_=sel4, axis=AX.X), reads=["sel4"], writes=["posk"])
            P.op("dve", lambda e: e.scalar_tensor_tensor(out=destf, in0=idxf, scalar=float(CAP), in1=posk, op0=ALU.mult, op1=ALU.add),
                 reads=["idxf", "posk"], writes=["destf"])
            P.op("dve", lambda e: e.tensor_scalar(out=ovf, in0=posk, scalar1=float(CAP), scalar2=None, op0=ALU.is_ge),
                 reads=["posk"], writes=["ovf"])
            P.op("dve", lambda e: e.scalar_tensor_tensor(out=destf, in0=ovf, scalar=4.0e6, in1=destf, op0=ALU.mult, op1=ALU.add),
                 reads=["ovf", "destf"], writes=["destf"])
            P.op("dve", lambda e: e.tensor_copy(out=DEST[:, tc, :], in_=destf), reads=["destf"], writes=[("DEST", tc)])
            P.op("dve", lambda e: e.tensor_scalar(out=destf, in0=destf, scalar1=float(NSLOT - 1), scalar2=None, op0=ALU.min),
                 reads=["destf", ("DEST", tc)], writes=["destf"])
            P.op("dve", lambda e: e.tensor_copy(out=DESTG[:, tc, :], in_=destf), reads=["destf"], writes=[("DESTG", tc)])
            P.op("dve", lambda e: e.tensor_scalar(out=ovf, in0=ovf, scalar1=-1.0, scalar2=1.0, op0=ALU.mult, op1=ALU.add),
                 reads=["ovf", "destf"], writes=["ovf"])
            P.op("dve", lambda e: e.tensor_tensor(out=Gk[:, tc, :], in0=gk4, in1=ovf, op=ALU.mult),
                 reads=["gk4", "ovf"], writes=[("Gk", tc)])
            for k in range(4):
                P.op("pool", lambda e, k=k: e.indirect_dma_start(
                    out=tok_d[:, :], out_offset=bass.IndirectOffsetOnAxis(ap=DEST_t[:, tc * 4 + k:tc * 4 + k + 1], axis=0),
                    in_=TOKID_t[:, tc:tc + 1], in_offset=None, bounds_check=pool_regs["bc"], oob_is_err=False),
                    reads=[("DEST", tc), "TOKID", "TOKZ"], writes=[("TOK", tc, k)], dma=True)

        def p2_s3c(tc):
            for dh in range(2):
                b = 4 + dh
                P.op("pe", lambda e, b=b, dh=dh: e.matmul(ps[b][:, :], lhsT=CTc[0:32, :], rhs=bd_sb[0:32, dh * 512:(dh + 1) * 512],
                                                           start=True, stop=True), reads=["CTc", "bd"], writes=[("ps", b)])
                P.op("dve", lambda e, b=b, dh=dh: e.tensor_tensor(
                    out=Rt[:, tc, dh * 512:(dh + 1) * 512], in0=ps[b][:, :], in1=Rt[:, tc, dh * 512:(dh + 1) * 512], op=ALU.add),
                    reads=[("ps", b), ("R", tc)], writes=[("R", tc)])

        skewed(NT, [p2_s0, p2_s1, p2_s2, p2_s2b, p2_s2c, p2_s3a, p2_s3b])

        if stage == "p2":
            toks = []
            for tc in range(NT):
                toks.append(P.op("sp", lambda e, tc=tc: e.dma_start(out=out_d[tc * 128:(tc + 1) * 128, :], in_=Rt[:, tc, :]),
                                 reads=[("R", tc), ("R", tc, 0), ("R", tc, 1)], writes=[("out", tc)], dma=True))
            return finish(toks)

        P.alias([("W", i) for i in range(2, 6)], ["wq", "wo"] + [("qT", c) for c in range(8)] + [("oT", c) for c in range(8)])
        xg_tok = [bfv(XT_OFF, 2560).rearrange("p (a d) -> p a d", a=NCH)] * 2
        xgTb = [bfv(XT_OFF + 2560 + i * 2560, 2560).rearrange("p (c j) -> p c j", c=8) for i in range(2)]
        P.alias([("xg", 0, a) for a in range(NCH)] + [("xgT", i, a) for i in range(2) for a in range(NCH)], XTk(range(NT)))
        hTs = bfv(HT_OFF, 2560).rearrange("p (f j) -> p f j", f=8)
        ystage = [f32v(HT_OFF + 2560 + i * 1024, 1024) for i in range(3)]
        bdb = [f32v(HT_OFF + 5632 + i * 1024, 1024) for i in range(2)]
        ph2_keys = (["bd", "y2", ("y2", 0), ("y2", 1), "xn2", "x2t", ("x2Tf", 0), ("x2Tf", 1), "CTc", "lg", "top8", "msk", "negm",
                     "exg", "ssum", "idx8", "idxf", "posk", "destf", "ovf", "gk4", "msk_bf", "posf", "sel4"] + KmT_keys + Vm_keys)
        P.alias([("hTs", f) for f in range(8)] + [("ys", i) for i in range(3)] + [("bdb", 0), ("bdb", 1)],
                ph2_keys + [("y2b", 0), ("y2b", 1), ("x2tb", 0), ("x2tb", 1), ("y2b", 0, 0), ("y2b", 0, 1), ("y2b", 1, 0), ("y2b", 1, 1)])
        HW_ = CAP // 2
        rg = [f32v(TMP_OFF + i * 512, HW_) for i in range(2)]
        sg = [f32v(TMP_OFF + 1024 + i * 512, HW_) for i in range(2)]
        pp = [f32v(TMP_OFF + 2048 + i * 512, HW_) for i in range(2)]
        P.alias([("rg", 0), ("rg", 1), ("sg", 0), ("sg", 1), ("pp", 0), ("pp", 1)],
                [("dent", 0), ("dent", 1)] + [("E", i) for i in range(4)])
        for k in range(2, 6):
            load_unit(0, k)
        yv = f32v(TMP_OFF, NE * NCH).rearrange("p (e a) -> p e a", e=NE)
        yb = f32v(TMP_OFF + 256, NE * NCH).rearrange("p (e a) -> p e a", e=NE)
        ysp = f32v(TMP_OFF + 512, NCH)
        yi_i = f32v(TMP_OFF + 768, NE * NCH).bitcast(I32)
        P.alias(["yv", "yb", "ysp", "yi_i"], [("dent", 0), ("dent", 1), ("x2Tf", 0), ("x2Tf", 1)] + [("E", i) for i in range(4)])
        P.op("pool", lambda e: e.iota(yi_i, pattern=[[CAP, NE], [1, NCH]], base=0, channel_multiplier=NCH), writes=["yi_i"])
        P.op("pool", lambda e: e.tensor_copy(out=yb, in_=yi_i.rearrange("p (e a) -> p e a", e=NE)), reads=["yi_i"], writes=["yb"])
        P.op("pool", lambda e: e.tensor_copy(out=ysp, in_=yi_i[:, 0:NCH]), reads=["yi_i"], writes=["ysp"])
        P.op("dve", lambda e: e.tensor_tensor(out=yv, in0=ysp.unsqueeze(1).to_broadcast([128, NE, NCH]),
                                              in1=cnt.unsqueeze(2).to_broadcast([128, NE, NCH]), op=ALU.is_lt),
             reads=["ysp", "cnt"], writes=["yv"])
        P.op("dve", lambda e: e.tensor_scalar(out=yv, in0=yv, scalar1=-30000.0, scalar2=30000.0, op0=ALU.mult, op1=ALU.add),
             reads=["yv"], writes=["yv"])
        P.op("dve", lambda e: e.tensor_tensor(out=yv, in0=yv, in1=yb, op=ALU.add), reads=["yv", "yb"], writes=["yv"])
        P.op("dve", lambda e: e.tensor_copy(out=YIDX_t[:, :].rearrange("p (e a) -> p e a", e=NE), in_=yv), reads=["yv"], writes=["YIDX"])
        P.alias([("rg", 0), ("rg", 1), ("sg", 0), ("sg", 1), ("pp", 0), ("pp", 1)], ["yv", "yb", "ysp", "yi_i"])
        tok_keys = ["TOKZ"] + [("TOK", tc, k) for tc in range(NT) for k in range(4)]
        P.op("pool", lambda e: e.dma_start(out=gidx_t[:, :].rearrange("p (e a) -> p e a", e=NE),
                                           in_=tok_d.rearrange("(e p a) o -> p e (a o)", e=NE, p=128)),
             reads=tok_keys + ["gidx"], writes=["gidx"], dma=True)
        xrow_keys = [("XROWS", tc) for tc in range(NT)]

        def gather_expert(ex):
            for a in range(NCH):
                P.op("pool", lambda e, ex=ex, a=a: e.indirect_dma_start(
                    out=xg_tok[0][:, a, :], out_offset=None, in_=xrows_d[:, :],
                    in_offset=bass.IndirectOffsetOnAxis(ap=gidx_t[:, ex * NCH + a:ex * NCH + a + 1], axis=0),
                    bounds_check=pool_regs["bx"], oob_is_err=False),
                    reads=["gidx", "xgzero"] + xrow_keys, writes=[("xg", 0, a)], dma=True)

        evf = [0]

        def transpose_expert(ex):
            tb = ex % 2
            for a in range(NCH):
                b = next_bank()
                for dc in range(8):
                    P.op("pe", lambda e, b=b, dc=dc, a=a: e.transpose(
                        psb[b][:, dc * 128:(dc + 1) * 128], xg_tok[0][:, a, dc * 128:(dc + 1) * 128], identB),
                        reads=[("xg", 0, a), "identB"], writes=[("ps", b)])
                src3 = psb[b][:].rearrange("p (c j) -> p c j", c=8)
                dst3 = xgTb[tb][:, :, a * 128:(a + 1) * 128]
                if evf[0] % 2 == 0:
                    P.op("act", lambda e, src3=src3, dst3=dst3: e.activation(out=dst3, in_=src3, func=AF.Copy),
                         reads=[("ps", b)], writes=[("xgT", tb, a)])
                else:
                    P.op("dve", lambda e, src3=src3, dst3=dst3: e.tensor_copy(out=dst3, in_=src3),
                         reads=[("ps", b)], writes=[("xgT", tb, a)])
                evf[0] += 1

        P.alias(["xgzero"], XTk(range(NT)))
        P.op("pool", lambda e: e.memset(xg_tok[0], 0.0), reads=[("xg", 0, a) for a in range(NCH)], writes=["xgzero"] + [("xg", 0, a) for a in range(NCH)])
        gather_expert(0)
        transpose_expert(0)
        if NE > 1:
            gather_expert(1)
        sl = [0]
        ysl = [0]
        for ex in range(NE):
            xgT = xgTb[ex % 2]
            xgT_keys = [("xgT", ex % 2, a) for a in range(NCH)]
            for fc in range(8):
                half = fc // 4
                gslot, uslot = 2 * half, 2 * half + 1
                cl = (fc % 4) * 128
                for hh in range(2):
                    js = slice(hh * HW_, (hh + 1) * HW_)
                    bG, bU = next_bank(), next_bank()
                    for (b, ws) in ((bG, gslot), (bU, uslot)):
                        for dc in range(8):
                            P.op("pe", lambda e, b=b, ws=ws, dc=dc, cl=cl, js=js, xgT=xgT: e.matmul(
                                ps[b][:, 0:HW_], lhsT=Wring[ws][:, dc, cl:cl + 128], rhs=xgT[:, dc, js], start=(dc == 0), stop=(dc == 7)),
                                reads=[("W", ws)] + xgT_keys, writes=[("ps", b)])
                    s_ = sl[0] % 2
                    sl[0] += 1
                    P.op("act", lambda e, bG=bG, s_=s_, ex=ex, fc=fc: e.activation(
                        out=rg[s_], in_=ps[bG][:, 0:HW_], func=AF.Relu, scale=-1.0, bias=bgT[:, ex, fc:fc + 1]),
                        reads=[("ps", bG), ("bT", 0)], writes=[("rg", s_)])
                    P.op("act", lambda e, s_=s_: e.activation(out=sg[s_], in_=rg[s_], func=AF.Sigmoid, scale=-1.702, bias=sig_bias_col),
                         reads=[("rg", s_), "sigb"], writes=[("sg", s_)])
                    P.op("act", lambda e, bU=bU, s_=s_, ex=ex, fc=fc: e.activation(
                        out=pp[s_], in_=ps[bU][:, 0:HW_], func=AF.Relu, bias=buT[:, ex, fc:fc + 1]),
                        reads=[("ps", bU), ("bT", 1)], writes=[("pp", s_)])
                    P.op("dve", lambda e, s_=s_: e.scalar_tensor_tensor(out=rg[s_], in0=rg[s_], scalar=-7.0, in1=sg[s_], op0=ALU.add, op1=ALU.mult),
                         reads=[("rg", s_), ("sg", s_)], writes=[("rg", s_)])
                    P.op("dve", lambda e, s_=s_: e.scalar_tensor_tensor(out=pp[s_], in0=pp[s_], scalar=14.0, in1=rg[s_], op0=ALU.min, op1=ALU.mult),
                         reads=[("pp", s_), ("rg", s_)], writes=[("pp", s_)])
                    P.op("dve", lambda e, s_=s_, fc=fc, js=js: e.scalar_tensor_tensor(
                        out=hTs[:, fc, js], in0=rg[s_], scalar=6.0, in1=pp[s_], op0=ALU.mult, op1=ALU.subtract),
                        reads=[("rg", s_), ("pp", s_)], writes=[("hTs", fc, hh)])
                if fc == 3 and ex + 1 < NE:
                    load_unit(ex + 1, 0)
                    load_unit(ex + 1, 1)
            if ex + 1 < NE:
                load_unit(ex + 1, 2)
                load_unit(ex + 1, 3)
            hT_keys = [("hTs", f, hh) for f in range(8) for hh in range(2)]
            if ex + 1 < NE:
                transpose_expert(ex + 1)
                if ex + 2 < NE:
                    gather_expert(ex + 2)
            bpar = ex % 2
            P.op("sp", lambda e, ex=ex, bpar=bpar: e.dma_start(out=bdb[bpar], in_=bed_d[ex:ex + 1, :].partition_broadcast(128)),
                 writes=[("bdb", bpar)], dma=True)
            ygv = yg_d[ex * CAP:(ex + 1) * CAP, :].rearrange("(p a) d -> p a d", a=NCH)
            for a in range(NCH):
                ys_ = ysl[0] % 3
                ysl[0] += 1
                for dh in range(2):
                    b = next_bank()
                    for fc in range(8):
                        P.op("pe", lambda e, b=b, fc=fc, a=a, dh=dh: e.matmul(
                            ps[b][:, :], lhsT=hTs[:, fc, a * 128:(a + 1) * 128], rhs=Wring[4 + dh][:, fc, :],
                            start=(fc == 0), stop=(fc == 7)), reads=hT_keys + [("W", 4 + dh)], writes=[("ps", b)])
                    P.op("dve", lambda e, b=b, ys_=ys_, dh=dh, bpar=bpar: e.tensor_tensor(
                        out=ystage[ys_][:, dh * 512:(dh + 1) * 512], in0=ps[b][:, :], in1=bdb[bpar][:, dh * 512:(dh + 1) * 512], op=ALU.add),
                        reads=[("ps", b), ("bdb", bpar)], writes=[("ys", ys_, dh)])
                P.op("pool", lambda e, ys_=ys_, a=a, ex=ex: e.indirect_dma_start(
                    out=yg_d[:, :], out_offset=bass.IndirectOffsetOnAxis(ap=YIDX_t[:, ex * NCH + a:ex * NCH + a + 1], axis=0),
                    in_=ystage[ys_], in_offset=None, bounds_check=pool_regs["bc"], oob_is_err=False),
                    reads=[("ys", ys_, 0), ("ys", ys_, 1), "YIDX"] + [("YGZ", g8) for g8 in range(NSLOT // 2560)],
                    writes=[("YG", ex, a)], dma=True)
            if ex + 1 < NE:
                load_unit(ex + 1, 4)
                load_unit(ex + 1, 5)

        P.op("sp", lambda e: e.dma_start(out=lnp, in_=ln_d[4:6, :].partition_broadcast(128)), writes=["lnp"], dma=True)
        yg_keys = [("YG", ex, a) for ex in range(NE) for a in range(NCH)]
        yk = [f32v(WR_OFF + i * 1024, 1024) for i in range(8)]
        xn3 = f32v(WR_OFF + 8192, 1024)
        otile = [f32v(WR_OFF + 9216 + i * 1024, 1024) for i in range(2)]
        P.alias([("yk", i) for i in range(8)] + ["xn3", ("ot", 0), ("ot", 1)], [("W", i) for i in range(6)])
        toks = []
        xn3b = [xn3, f32v(WR_OFF + 11264, 1024)]
        P.alias([("xn3b", 0), ("xn3b", 1)], ["xn3"] + [("W", i) for i in range(6)])
        for tc in range(NT):
            P.alias([("R", tc)], [("R", tc, 0), ("R", tc, 1)])

        def t_s0(tc):
            for k in range(4):
                yi = (tc % 2) * 4 + k
                P.op("pool", lambda e, k=k, yi=yi: e.indirect_dma_start(
                    out=yk[yi], out_offset=None, in_=yg_d[:, :],
                    in_offset=bass.IndirectOffsetOnAxis(ap=DESTG_t[:, tc * 4 + k:tc * 4 + k + 1], axis=0)),
                    reads=yg_keys + [("DESTG", tc)], writes=[("yk", yi)], dma=True)

        def t_s0b(tc):
            for k in range(4):
                yi = (tc % 2) * 4 + k
                P.op("dve", lambda e, k=k, yi=yi: e.scalar_tensor_tensor(
                    out=Rt[:, tc, :], in0=yk[yi], scalar=Gk[:, tc, k:k + 1], in1=Rt[:, tc, :], op0=ALU.mult, op1=ALU.add),
                    reads=[("yk", yi), ("Gk", tc), ("R", tc)], writes=[("R", tc)])

        def t_s1(tc):
            ln_stats(Rt[:, tc, :], ("R", tc), tc % 2)

        def t_s2(tc):
            par = tc % 2
            ln_apply(Rt[:, tc, :], ("R", tc), par, otile[par], [("ot", par)], "lnp", xn3b[par], ("xn3b", par))
            toks.append(P.op("sp", lambda e: e.dma_start(out=out_d[tc * 128:(tc + 1) * 128, :], in_=otile[par]),
                             reads=[("ot", par)], writes=[("out", tc)], dma=True))

        skewed(NT, [t_s0, t_s0b, t_s1, t_s2])
        return finish(toks)

    return nc


def _rope_tables():
    t = np.arange(S)
    def cs(pos, dim):
        inv = (10000.0 ** (-np.arange(0, dim, 2, dtype=np.float32) / dim)).astype(np.float32)
        ang = pos.astype(np.float32)[:, None] * inv[None, :]
        return np.cos(ang).astype(np.float32), np.sin(ang).astype(np.float32)
    cr, sr = cs(t // 64, 32)
    cc, sc = cs(t % 64, 32)
    cq, sq = cs(t, 64)
    A = np.zeros((S, 2, 64), np.float32)
    A[:, 0] = np.concatenate([cr, cr, cc, cc], -1)
    A[:, 1] = np.concatenate([-sr, sr, -sc, sc], -1)
    B = np.zeros((S, 2, 64), np.float32)
    B[:, 0] = np.concatenate([cq, cq], -1)
    B[:, 1] = np.concatenate([-sq, sq], -1)
    return A, B


def make_in_maps(inputs, cores):
    f = lambda k: np.ascontiguousarray(np.asarray(inputs[k], dtype=np.float32))
    A, B = _rope_tables()
    shared = {
        "w_in": f("w_in")[0],
        "a_q_norm": f("a_q_norm"),
        "a_k_norm": f("a_k_norm"),
        "b_lambda": np.ascontiguousarray(np.concatenate(
            [f("b_lambda_q1"), f("b_lambda_k1"), f("b_lambda_q2"), f("b_lambda_k2")], 0)),
        "b_subln": np.ascontiguousarray(f("b_subln").reshape(128, 1)),
        "w_mix_out": f("w_mix_out")[0],
        "ln_gb": np.ascontiguousarray(np.concatenate(
            [f("ln1_g"), f("ln1_b"), f("ln2_g"), f("ln2_b"), f("ln3_g"), f("ln3_b")], 0)),
        "w_mem_q": f("w_mem_q")[0],
        "w_mem_kv": f("w_mem_kv")[0],
        "w_mem_out": f("w_mem_out")[0],
        "w_router": f("w_router")[0],
        "b_router": f("b_router"),
        "w_e_gate": f("w_e_gate")[0],
        "b_e_gate": f("b_e_gate")[0],
        "w_e_up": f("w_e_up")[0],
        "b_e_up": f("b_e_up")[0],
        "w_e_down": f("w_e_down")[0],
        "b_e_down": f("b_e_down")[0],
        "ropeA": A,
        "ropeB": B,
        "zeros_rows": np.zeros((2560, D), np.float32),
    }
    x = f("x")
    mem = f("mem")
    maps = []
    for c in cores:
        m = dict(shared)
        m["x"] = x[c]
        m["mem"] = mem[c]
        maps.append(m)
    return maps


def kernel(**inputs):
    nc = build_program("full")
    cores = list(range(8))
    in_maps = make_in_maps(inputs, cores)
    res = run_bass_kernel_spmd(nc, in_maps, core_ids=cores)
    out = np.stack([np.asarray(r["out"], dtype=np.float32) for r in res.results], 0)
    return out
```

```python
import contextlib
import numpy as np
import concourse.bass as bass
import concourse.mybir as mybir
from concourse.bass_utils import run_bass_kernel_spmd

F32 = mybir.dt.float32
BF16 = mybir.dt.bfloat16
I32 = mybir.dt.int32
U32 = mybir.dt.uint32
AF = mybir.ActivationFunctionType
ALU = mybir.AluOpType
AX = mybir.AxisListType

S = 2048
D = 1024
NT = 16
NE = 32
ALPHA = 2.0 ** 0.25
LAMBDA_INIT = 0.2
LN_EPS = 1e-5
RMS_EPS = 1e-6
DMA_RING = 16
ARENA_W = 52600
CAP = 640
NCH = CAP // 128
NSLOT = NE * CAP


class Prog:
    ENG = ("pe", "act", "dve", "pool", "sp")

    def __init__(self):
        self.stream = {e: [] for e in self.ENG}
        self.nops = {e: 0 for e in self.ENG}
        self.writer = {}
        self.readers = {}
        self.dma_n = {e: 0 for e in self.ENG}
        self.milestones = {e: set() for e in self.ENG}

    @staticmethod
    def _ident(tok):
        return (tok[0], tok[1])

    def _merge(self, d, tok):
        i = self._ident(tok)
        if i not in d or d[i][2] < tok[2]:
            d[i] = tok

    def alias(self, new_keys, old_keys):
        acc = {}
        for k in old_keys:
            w = self.writer.get(k)
            if w is not None:
                self._merge(acc, w)
            for t in self.readers.get(k, {}).values():
                self._merge(acc, t)
        for k in new_keys:
            r = self.readers.setdefault(k, {})
            for t in acc.values():
                self._merge(r, t)

    def op(self, eng, fn, reads=(), writes=(), dma=False):
        deps = {}
        for k in reads:
            w = self.writer.get(k)
            if w is not None:
                self._merge(deps, w)
        for k in writes:
            w = self.writer.get(k)
            if w is not None:
                self._merge(deps, w)
            for t in self.readers.get(k, {}).values():
                self._merge(deps, t)
        waits = []
        for t in deps.values():
            if t[0] == "c" and t[1] == eng and eng == "pe":
                continue
            waits.append(t)
            if t[0] == "c":
                self.milestones[t[1]].add(t[2])
        if dma:
            n = self.dma_n[eng]
            self.dma_n[eng] = n + 1
            sem = (eng, n % DMA_RING)
            if n >= DMA_RING:
                waits.append(("d", sem, 16 * (n // DMA_RING)))
            tok = ("d", sem, 16 * (n // DMA_RING + 1))
            self.stream[eng].append(("dma", fn, waits, tok))
        else:
            self.nops[eng] += 1
            tok = ("c", eng, self.nops[eng])
            self.stream[eng].append(("op", fn, waits, tok))
        for k in writes:
            self.writer[k] = tok
            self.readers[k] = {}
        for k in reads:
            if k in writes:
                continue
            self._merge(self.readers.setdefault(k, {}), tok)
        return tok

    def wait_tokens(self, eng, toks):
        for t in toks:
            if t[0] == "c":
                self.milestones[t[1]].add(t[2])
        self.stream[eng].append(("wait", None, list(toks), None))

    def emit(self, block, nc, esem, dsem):
        rank = {}
        for e in self.ENG:
            ms = sorted(self.milestones[e])
            rank[e] = {idx: i + 1 for i, idx in enumerate(ms)}

        def run(eng_name, eng):
            known = {}
            for kind, fn, waits, tok in self.stream[eng_name]:
                for t in waits:
                    if t[0] == "c":
                        sem = esem[t[1]]
                        val = rank[t[1]][t[2]]
                        key = ("c", t[1])
                    else:
                        sem = dsem[t[1]]
                        val = t[2]
                        key = ("d", t[1])
                    if known.get(key, 0) >= val:
                        continue
                    known[key] = val
                    eng.wait_ge(sem, val)
                if kind == "wait":
                    continue
                ins = fn(eng)
                if kind == "dma":
                    ins.then_inc(dsem[tok[1]], 16)
                elif tok[2] in rank[eng_name]:
                    ins.then_inc(esem[eng_name], 1)

        @block.tensor
        def _(e):
            run("pe", e)

        @block.scalar
        def _(e):
            run("act", e)

        @block.vector
        def _(e):
            run("dve", e)

        @block.gpsimd
        def _(e):
            run("pool", e)

        @block.sync
        def _(e):
            run("sp", e)


def build_program(stage="full"):
    nc = bass.Bass("TRN2", target_bir_lowering=False)

    def din(name, shape):
        return nc.dram_tensor(name, list(shape), F32, kind="ExternalInput").ap()

    x_d = din("x", [S, D])
    mem_d = din("mem", [256, D])
    w_in_d = din("w_in", [D, 2304])
    aq_d = din("a_q_norm", [1, 64])
    ak_d = din("a_k_norm", [1, 64])
    lam_d = din("b_lambda", [4, 64])
    subln_d = din("b_subln", [128, 1])
    wmix_d = din("w_mix_out", [D, D])
    ln_d = din("ln_gb", [6, D])
    wq_d = din("w_mem_q", [D, D])
    wkv_d = din("w_mem_kv", [D, 2 * D])
    wo_d = din("w_mem_out", [D, D])
    wr_d = din("w_router", [D, NE])
    br_d = din("b_router", [1, NE])
    weg_d = din("w_e_gate", [NE, D, D])
    beg_d = din("b_e_gate", [NE, D])
    weu_d = din("w_e_up", [NE, D, D])
    beu_d = din("b_e_up", [NE, D])
    wed_d = din("w_e_down", [NE, D, D])
    bed_d = din("b_e_down", [NE, D])
    ropeA_d = din("ropeA", [S, 2, 64])
    ropeB_d = din("ropeB", [S, 2, 64])
    zeros_d = din("zeros_rows", [2560, D])
    out_d = nc.dram_tensor("out", [S, D], F32, kind="ExternalOutput").ap()
    xrows_d = nc.dram_tensor("xrows", [S, D], BF16).ap()
    tok_d = nc.dram_tensor("tokslots", [NSLOT, 1], I32).ap()
    yg_d = nc.dram_tensor("ygrows", [NSLOT, D], F32).ap()
    dbg_d = None
    if stage != "full":
        dbg_d = nc.dram_tensor("dbg", [128, 40960], F32, kind="ExternalOutput").ap()

    P = Prog()
    st = contextlib.ExitStack()
    with st:
        arena = st.enter_context(nc.sbuf_tensor("arena", [128, ARENA_W], F32))
        ps = [st.enter_context(nc.psum_tensor(f"ps{i}", [128, 512], F32)) for i in range(8)]
        esem = {e: st.enter_context(nc.semaphore(f"s_{e}")) for e in Prog.ENG}
        dsem = {}
        for e in ("sp", "pool"):
            for i in range(DMA_RING):
                dsem[(e, i)] = st.enter_context(nc.semaphore(f"d_{e}{i}"))

        def f32v(off, n):
            return arena[:, off:off + n]

        def bfv(off, n_words):
            return arena[:, off:off + n_words].bitcast(BF16)

        R_OFF, XT_OFF, WR_OFF, HT_OFF, TMP_OFF, MISC_OFF = 0, 16384, 24576, 36864, 45056, 48128
        Rt = f32v(R_OFF, 16384).rearrange("p (c d) -> p c d", c=NT)
        XT = bfv(XT_OFF, 8192).rearrange("p (c t) -> p c t", c=8)

        mo = [MISC_OFF]

        def misc(nw):
            o = mo[0]
            mo[0] += nw
            assert mo[0] <= ARENA_W
            return o

        identF = f32v(misc(128), 128)
        identB = bfv(misc(64), 64)
        onesB = bfv(misc(64), 64)
        lnp = f32v(misc(2048), 2048).rearrange("p (a d) -> p a d", a=2)
        Cmb = f32v(misc(512), 512).rearrange("p (c e) -> p c e", c=NT)
        bgT = f32v(misc(256), 256).rearrange("p (e f) -> p e f", e=NE)
        buT = f32v(misc(256), 256).rearrange("p (e f) -> p e f", e=NE)
        wr_sb = f32v(misc(256), 256).rearrange("p (c e) -> p c e", c=8)
        br_sb = f32v(misc(32), 32)
        gq_sb = f32v(misc(64), 64)
        gk_sb = f32v(misc(64), 64)
        small = f32v(misc(64), 64)
        LNP_OFF = MISC_OFF + 128 + 64 + 64
        ropeT = f32v(LNP_OFF, 512).rearrange("p (s k a d) -> p s k a d", s=2, k=2, a=2)

        gidx_t = st.enter_context(nc.sbuf_tensor("gidx_t", [128, NE * NCH], I32))
        DEST_t = st.enter_context(nc.sbuf_tensor("DEST_t", [128, NT * 4], I32))
        DESTG_t = st.enter_context(nc.sbuf_tensor("DESTG_t", [128, NT * 4], I32))
        TOKID_t = st.enter_context(nc.sbuf_tensor("TOKID_t", [128, NT], I32))
        YIDX_t = st.enter_context(nc.sbuf_tensor("YIDX_t", [128, NE * NCH], I32))
        gidx_all = gidx_t[:, :].rearrange("p (e a) -> p e a", e=NE)
        DEST = DEST_t[:, :].rearrange("p (c k) -> p c k", c=NT)
        DESTG = DESTG_t[:, :].rearrange("p (c k) -> p c k", c=NT)
        Gk = f32v(misc(64), 64).rearrange("p (c k) -> p c k", c=NT)
        TOKID = TOKID_t[:, :]
        cnt = f32v(misc(32), 32)
        iota32 = f32v(misc(32), 32)
        Ltri = bfv(misc(64), 64)
        lam_col = small[:, 0:1]
        nlam_col = small[:, 1:2]
        gs_col = small[:, 2:3]
        lsum = small[:, 4:8]
        eps_col = small[:, 8:9]
        rmseps_col = small[:, 9:10]
        sig_bias_col = small[:, 10:11]

        psb = [p[:].bitcast(BF16) for p in ps]

        pool_regs = {}

        def mk_bc_reg(e):
            pool_regs["bc"] = e.alloc_register("bc")
            pool_regs["bx"] = e.alloc_register("bx")
            e.reg_mov(pool_regs["bx"], S - 1)
            return e.reg_mov(pool_regs["bc"], NSLOT - 1)

        P.op("pool", mk_bc_reg, writes=["bcreg"])
        P.op("pool", lambda e: e.memset(identF, 0.0), writes=["identF"])
        P.op("pool", lambda e: e.affine_select(out=identF, in_=identF, pattern=[[-1, 128]],
                                               compare_op=ALU.not_equal, fill=1.0, base=0,
                                               channel_multiplier=1), reads=["identF"], writes=["identF"])
        P.op("pool", lambda e: e.tensor_copy(out=identB, in_=identF), reads=["identF"], writes=["identB"])
        P.op("pool", lambda e: e.memset(onesB, 1.0), writes=["onesB"])
        P.op("pool", lambda e: e.memset(eps_col, LN_EPS), writes=["eps"])
        P.op("pool", lambda e: e.memset(rmseps_col, RMS_EPS), writes=["eps2"])
        P.op("pool", lambda e: e.memset(sig_bias_col, 1.702 * 7.0), writes=["sigb"])

        iota_i = small[:, 32:64].bitcast(I32)
        P.op("pool", lambda e: e.iota(iota_i, pattern=[[1, 32]], base=0, channel_multiplier=0), writes=["iota_i"])
        P.op("pool", lambda e: e.tensor_copy(out=iota32, in_=iota_i), reads=["iota_i"], writes=["iota32"])
        P.op("pool", lambda e: e.iota(TOKID, pattern=[[128, NT]], base=0, channel_multiplier=1), writes=["TOKID"])
        P.op("pool", lambda e: e.memset(cnt, 0.0), writes=["cnt"])
        P.op("pool", lambda e: e.memset(Ltri, 1.0), writes=["Ltri"])
        P.op("pool", lambda e: e.affine_select(out=Ltri, in_=Ltri, pattern=[[1, 128]], compare_op=ALU.is_gt, fill=0.0,
                                               base=0, channel_multiplier=-1), reads=["Ltri"], writes=["Ltri"])
        P.op("pool", lambda e: e.memset(gidx_t[:, :], 30000), writes=["gidx"])
        P.op("pool", lambda e: e.dma_start(out=tok_d.rearrange("(p a) o -> p (a o)", p=128),
                                           in_=gidx_t[:, :]), reads=["gidx"], writes=["TOKZ"], dma=True)
        P.op("sp", lambda e: e.dma_start(out=gq_sb, in_=aq_d.partition_broadcast(128)), writes=["gq"], dma=True)
        P.op("sp", lambda e: e.dma_start(out=gk_sb, in_=ak_d.partition_broadcast(128)), writes=["gk"], dma=True)

        xin = [f32v(TMP_OFF + i * 1024, 1024) for i in range(2)]
        Ering = [bfv(TMP_OFF + 2048 + i * 256, 256) for i in range(4)]

        def load_x(tc, slot):
            P.op("sp", lambda e: e.dma_start(out=xin[slot], in_=x_d[tc * 128:(tc + 1) * 128, :]),
                 writes=[("xin", slot)], dma=True)

        evac_flip = [0]

        def transpose_tile_to_XT(src, src_key, tc, banks):
            for half in range(2):
                b = banks[half]
                for j in range(4):
                    dc = half * 4 + j
                    P.op("pe", lambda e, b=b, j=j, dc=dc: e.transpose(ps[b][:, j * 128:(j + 1) * 128],
                                                                      src[:, dc * 128:(dc + 1) * 128], identF),
                         reads=[src_key, "identF"], writes=[("ps", b)])
                dst = XT[:, half * 4:(half + 1) * 4, tc * 128:(tc + 1) * 128]
                srcp = ps[b][:].rearrange("p (j t) -> p j t", j=4)
                eng = "act" if evac_flip[0] % 2 == 0 else "dve"
                evac_flip[0] += 1
                if eng == "act":
                    P.op("act", lambda e, dst=dst, srcp=srcp: e.activation(out=dst, in_=srcp, func=AF.Copy),
                         reads=[("ps", b)], writes=[("XT", tc, half)])
                else:
                    P.op("dve", lambda e, dst=dst, srcp=srcp: e.tensor_copy(out=dst, in_=srcp),
                         reads=[("ps", b)], writes=[("XT", tc, half)])

        def XTk(tcs):
            return [("XT", tc, h) for tc in tcs for h in range(2)]

        wi = bfv(WR_OFF, 9216).rearrange("p (c n) -> p c n", c=8)
        col_tiles = [(0, 512), (512, 768), (768, 1280), (1280, 1792), (1792, 2304)]
        w_in_v = w_in_d.rearrange("(c p) n -> p c n", p=128)
        for ci, (c0, c1) in enumerate(col_tiles):
            P.op("pool", lambda e, c0=c0, c1=c1: e.dma_start(out=wi[:, :, c0:c1], in_=w_in_v[:, :, c0:c1]),
                 writes=[("wi", ci)], dma=True)

        for tc in range(NT):
            load_x(tc, tc % 2)
            transpose_tile_to_XT(xin[tc % 2], ("xin", tc % 2), tc, (2 * (tc % 2), 2 * (tc % 2) + 1))

        QTA = bfv(R_OFF + 0, 4096).rearrange("p (j t) -> p j t", j=4)
        KTA = bfv(R_OFF + 4096, 1024)
        QKTB = bfv(R_OFF + 5120, 8192).rearrange("p (j t) -> p j t", j=8)
        VA = bfv(R_OFF + 13312, 3072).rearrange("p (c k n) -> p c k n", c=NT, k=2)
        VB = bfv(HT_OFF, 4096).rearrange("p (c n) -> p c n", c=NT)
        T1 = WR_OFF + 9216
        sqA = f32v(T1, 640)
        tA1 = f32v(T1 + 640, 640)
        tA2 = f32v(T1 + 1280, 640)
        tB1 = f32v(T1 + 1920, 512)
        tB2 = f32v(T1 + 2432, 512)
        ssA = f32v(T1 + 2944, 16)
        rstdA = f32v(T1 + 2960, 16)
        qkA_bf = bfv(TMP_OFF + 2560, 320)
        qkB_bf = bfv(T1 + 1920 + 0, 0) if False else None

        P.op("pool", lambda e: e.memset(VA[:, :, :, 0:64], 1.0), writes=["VAones0"])
        P.op("pool", lambda e: e.memset(VA[:, :, :, 128:192], 1.0), writes=["VAones1"])

        def load_rope(tc, slot):
            P.op("sp", lambda e: e.dma_start(out=ropeT[:, slot, 0], in_=ropeA_d[tc * 128:(tc + 1) * 128]),
                 writes=[("ropeA", slot)], dma=True)
            P.op("sp", lambda e: e.dma_start(out=ropeT[:, slot, 1], in_=ropeB_d[tc * 128:(tc + 1) * 128]),
                 writes=[("ropeB", slot)], dma=True)

        qkB_bf = bfv(TMP_OFF + 2048, 512)

        bank_rr = [0]

        def next_bank():
            b = bank_rr[0] % 8
            bank_rr[0] += 1
            return b

        for tc in range(NT):
            slot = tc % 2
            load_rope(tc, slot)
            banks = [next_bank() for _ in range(5)]
            for ci, (c0, c1) in enumerate(col_tiles):
                b = banks[ci]
                n = c1 - c0
                for dc in range(8):
                    P.op("pe", lambda e, b=b, n=n, dc=dc, c0=c0, c1=c1, tc=tc: e.matmul(
                        ps[b][:, 0:n], lhsT=XT[:, dc, tc * 128:(tc + 1) * 128], rhs=wi[:, dc, c0:c1],
                        start=(dc == 0), stop=(dc == 7)),
                        reads=XTk([tc]) + [("wi", ci)], writes=[("ps", b)])
            bA, bKV, bQ, bK, bV = banks
            P.op("act", lambda e, bA=bA: e.activation(out=sqA[:, 0:512], in_=ps[bA][:, 0:512], func=AF.Square),
                 reads=[("ps", bA)], writes=["sqA_q"])
            P.op("act", lambda e, bKV=bKV: e.activation(out=sqA[:, 512:640], in_=ps[bKV][:, 0:128], func=AF.Square),
                 reads=[("ps", bKV)], writes=["sqA_k"])
            P.op("dve", lambda e: e.reduce_sum(out=ssA[:, 0:10], in_=sqA.rearrange("p (h d) -> p h d", h=10), axis=AX.X),
                 reads=["sqA_q", "sqA_k"], writes=["ssA"])
            P.op("act", lambda e: e.activation(out=rstdA[:, 0:10], in_=ssA[:, 0:10], func=AF.Ln, scale=1.0 / 64.0, bias=rmseps_col),
                 reads=["ssA", "eps2"], writes=["rstdA"])
            P.op("act", lambda e: e.activation(out=rstdA[:, 0:10], in_=rstdA[:, 0:10], func=AF.Exp, scale=-0.5),
                 reads=["rstdA"], writes=["rstdA"])
            Ctab = ropeT[:, slot, 0, 0, :]
            Stab = ropeT[:, slot, 0, 1, :]
            gtab = f32v(T1 + 3040, 0) if False else None
            for (src_b, c_lo, nh, dst_lo, gsb, gkey) in ((bA, 0, 8, 0, gq_sb, "gq"), (bKV, 0, 2, 512, gk_sb, "gk")):
                xv = ps[src_b][:, c_lo:c_lo + nh * 64].rearrange("p (h d) -> p h d", h=nh)
                xg = tA1[:, dst_lo:dst_lo + nh * 64].rearrange("p (h d) -> p h d", h=nh)
                P.op("dve", lambda e, xv=xv, xg=xg, gsb=gsb, nh=nh: e.tensor_tensor(
                    out=xg, in0=xv, in1=gsb.unsqueeze(1).to_broadcast([128, nh, 64]), op=ALU.mult),
                    reads=[("ps", src_b), gkey], writes=[("tA1", dst_lo)])
                xg4 = tA1[:, dst_lo:dst_lo + nh * 64].rearrange("p (h a b d) -> p h a b d", h=nh, a=2, b=2)
                t24 = tA2[:, dst_lo:dst_lo + nh * 64].rearrange("p (h a b d) -> p h a b d", h=nh, a=2, b=2)
                S4 = Stab.rearrange("p (a b d) -> p a b d", a=2, b=2)
                for bb in range(2):
                    P.op("dve", lambda e, bb=bb, xg4=xg4, t24=t24, S4=S4, nh=nh: e.tensor_tensor(
                        out=t24[:, :, :, bb, :], in0=xg4[:, :, :, 1 - bb, :],
                        in1=S4[:, :, bb, :].unsqueeze(1).to_broadcast([128, nh, 2, 16]), op=ALU.mult),
                        reads=[("tA1", dst_lo), ("ropeA", slot)], writes=[("tA2", dst_lo, bb)])
                P.op("dve", lambda e, xg=xg, nh=nh, Ctab=Ctab: e.tensor_tensor(
                    out=xg, in0=xg, in1=Ctab.unsqueeze(1).to_broadcast([128, nh, 64]), op=ALU.mult),
                    reads=[("tA1", dst_lo), ("ropeA", slot), ("tA2", dst_lo, 0), ("tA2", dst_lo, 1)], writes=[("tA1", dst_lo)])
                t2v = tA2[:, dst_lo:dst_lo + nh * 64].rearrange("p (h d) -> p h d", h=nh)
                P.op("dve", lambda e, xg=xg, t2v=t2v: e.tensor_tensor(out=xg, in0=xg, in1=t2v, op=ALU.add),
                     reads=[("tA1", dst_lo), ("tA2", dst_lo, 0), ("tA2", dst_lo, 1)], writes=[("tA1", dst_lo)])
            qk3 = qkA_bf.rearrange("p (j g d) -> p j g d", j=5, g=2)
            for h in range(10):
                if h < 8:
                    j, g = h % 4, h // 4
                else:
                    j, g = 4, h - 8
                P.op("act", lambda e, h=h, j=j, g=g: e.activation(
                    out=qk3[:, j, g, :], in_=tA1[:, h * 64:(h + 1) * 64], func=AF.Copy, scale=rstdA[:, h:h + 1]),
                    reads=[("tA1", 0), ("tA1", 512), "rstdA"], writes=[("qkA", h)])
            bT = next_bank()
            for j in range(5):
                P.op("pe", lambda e, j=j, bT=bT: e.transpose(psb[bT][:, j * 128:(j + 1) * 128],
                                                             qkA_bf[:, j * 128:(j + 1) * 128], identB),
                     reads=[("qkA", h) for h in range(10)] + ["identB"], writes=[("ps", bT)])
            P.op("act", lambda e, bT=bT, tc=tc: e.activation(
                out=QTA[:, :, tc * 128:(tc + 1) * 128],
                in_=psb[bT][:, 0:512].rearrange("p (j t) -> p j t", j=4), func=AF.Copy),
                reads=[("ps", bT)], writes=[("QTA", tc)])
            P.op("act", lambda e, bT=bT, tc=tc: e.activation(
                out=KTA[:, tc * 128:(tc + 1) * 128], in_=psb[bT][:, 512:640], func=AF.Copy),
                reads=[("ps", bT)], writes=[("KTA", tc)])
            P.op("act", lambda e, bKV=bKV, tc=tc: e.activation(
                out=VA[:, tc, :, 64:128], in_=ps[bKV][:, 128:256].rearrange("p (k d) -> p k d", k=2), func=AF.Copy),
                reads=[("ps", bKV)], writes=[("VA", tc)])
            CB = ropeT[:, slot, 1, 0, :]
            SB = ropeT[:, slot, 1, 1, :]
            for qi, bsrc in enumerate((bQ, bK)):
                xv = ps[bsrc][:, 0:512].rearrange("p (h d) -> p h d", h=8)
                xv4 = ps[bsrc][:, 0:512].rearrange("p (h b d) -> p h b d", h=8, b=2)
                t1v = tB1.rearrange("p (h d) -> p h d", h=8)
                t24 = tB2.rearrange("p (h b d) -> p h b d", h=8, b=2)
                S3 = SB.rearrange("p (b d) -> p b d", b=2)
                P.op("dve", lambda e, xv=xv, t1v=t1v, CB=CB: e.tensor_tensor(
                    out=t1v, in0=xv, in1=CB.unsqueeze(1).to_broadcast([128, 8, 64]), op=ALU.mult),
                    reads=[("ps", bsrc), ("ropeB", slot)], writes=["tB1"])
                for bb in range(2):
                    P.op("dve", lambda e, bb=bb, xv4=xv4, t24=t24, S3=S3: e.tensor_tensor(
                        out=t24[:, :, bb, :], in0=xv4[:, :, 1 - bb, :],
                        in1=S3[:, bb, :].unsqueeze(1).to_broadcast([128, 8, 32]), op=ALU.mult),
                        reads=[("ps", bsrc), ("ropeB", slot)], writes=[("tB2", bb)])
                dstb = qkB_bf[:, qi * 512:(qi + 1) * 512]
                P.op("dve", lambda e, dstb=dstb: e.tensor_tensor(out=dstb, in0=tB1, in1=tB2, op=ALU.add),
                     reads=["tB1", ("tB2", 0), ("tB2", 1)], writes=[("qkB", qi)])
            bT2 = next_bank()
            for j in range(8):
                P.op("pe", lambda e, j=j, bT2=bT2: e.transpose(psb[bT2][:, j * 128:(j + 1) * 128],
                                                               qkB_bf[:, j * 128:(j + 1) * 128], identB),
                     reads=[("qkB", 0), ("qkB", 1), "identB"], writes=[("ps", bT2)])
            P.op("act", lambda e, bT2=bT2, tc=tc: e.activation(
                out=QKTB[:, :, tc * 128:(tc + 1) * 128],
                in_=psb[bT2][:].rearrange("p (j t) -> p j t", j=8), func=AF.Copy),
                reads=[("ps", bT2)], writes=[("QKTB", tc)])
            P.op("act", lambda e, bV=bV, tc=tc: e.activation(out=VB[:, tc, :], in_=ps[bV][:, 0:512], func=AF.Copy),
                 reads=[("ps", bV)], writes=[("VB", tc)])

        if stage == "p1b":
            def dump(view, off, n, keys):
                P.op("pool", lambda e: e.dma_start(out=dbg_d[:, off:off + n], in_=view), reads=keys, writes=[("dbgout", off)], dma=True)
            dump(bfv(R_OFF, 4096), 0, 8192, [("QTA", t) for t in range(NT)])
            dump(KTA, 8192, 2048, [("KTA", t) for t in range(NT)])
            dump(bfv(R_OFF + 5120, 8192), 10240, 16384, [("QKTB", t) for t in range(NT)])
            dump(bfv(R_OFF + 13312, 3072), 26624, 6144, [("VA", t) for t in range(NT)] + ["VAones0", "VAones1"])
            dump(bfv(HT_OFF, 4096), 32768, 8192, [("VB", t) for t in range(NT)])
            P.wait_tokens("sp", [P.writer[("dbgout", o)] for o in (0, 8192, 10240, 26624, 32768)])
            with nc.Block() as block:
                P.emit(block, nc, esem, dsem)
            return nc


        def finish(final_toks):
            P.wait_tokens("sp", final_toks)
            print("milestones", {e: len(P.milestones[e]) for e in P.ENG}, "ops", P.nops, "dma", P.dma_n)
            with nc.Block() as block:
                P.emit(block, nc, esem, dsem)
            return nc

        bst = f32v(T1, 2048).rearrange("p (a d) -> p a d", a=2)
        P.alias(["bst"], ["sqA_q", "sqA_k", "ssA", "rstdA", ("tA1", 0), ("tA1", 512), ("tA2", 0, 0), ("tA2", 0, 1),
                          ("tA2", 512, 0), ("tA2", 512, 1), "tB1", ("tB2", 0), ("tB2", 1)])
        P.op("sp", lambda e: e.dma_start(out=bst[0:32, 0, :], in_=beg_d), writes=["bst"], dma=True)
        P.op("sp", lambda e: e.dma_start(out=bst[0:32, 1, :], in_=beu_d), reads=["bst"], writes=["bst2"], dma=True)
        for a, (dstT, sc1, sc2) in enumerate(((bgT, -1.0, 7.0), (buT, 1.0, 7.0))):
            b = next_bank()
            for fc in range(8):
                P.op("pe", lambda e, b=b, fc=fc, a=a: e.transpose(ps[b][:, fc * 32:(fc + 1) * 32],
                                                                  bst[0:32, a, fc * 128:(fc + 1) * 128], identF[0:32, 0:32]),
                     reads=["bst", "bst2", "identF"], writes=[("ps", b)])
            P.op("dve", lambda e, b=b, dstT=dstT, sc1=sc1, sc2=sc2: e.tensor_scalar(
                out=dstT, in0=ps[b][:, 0:256].rearrange("p (f e) -> p e f", f=8), scalar1=sc1, scalar2=sc2,
                op0=ALU.mult, op1=ALU.add), reads=[("ps", b)], writes=[("bT", a)])
        wkv = bfv(XT_OFF, 8192).rearrange("p (c n) -> p c n", c=8)
        P.alias(["wkv"], XTk(range(NT)))
        P.op("pool", lambda e: e.dma_start(out=wkv, in_=wkv_d.rearrange("(c p) n -> p c n", p=128)), writes=["wkv"], dma=True)

        lamv = f32v(LNP_OFF + 512, 256).rearrange("p (a b d) -> p a b d", a=2, b=2)
        P.op("sp", lambda e: e.dma_start(out=lamv, in_=lam_d.rearrange("(a b) d -> a b d", a=2).partition_broadcast(128)),
             writes=["lamv"], dma=True)
        P.op("sp", lambda e: e.dma_start(out=gs_col, in_=subln_d), writes=["gs"], dma=True)
        P.op("dve", lambda e: e.tensor_tensor(out=lamv[:, :, 0, :], in0=lamv[:, :, 0, :], in1=lamv[:, :, 1, :], op=ALU.mult),
             reads=["lamv"], writes=["lamv"])
        P.op("dve", lambda e: e.reduce_sum(out=lsum[:, 0:2], in_=lamv[:, :, 0, :], axis=AX.X), reads=["lamv"], writes=["lsum"])
        P.op("act", lambda e: e.activation(out=lsum[:, 2:4], in_=lsum[:, 0:2], func=AF.Exp), reads=["lsum"], writes=["lsum2"])
        P.op("dve", lambda e: e.tensor_tensor(out=lam_col, in0=lsum[:, 2:3], in1=lsum[:, 3:4], op=ALU.subtract),
             reads=["lsum2"], writes=["lam"])
        P.op("dve", lambda e: e.tensor_scalar(out=nlam_col, in0=lam_col, scalar1=LAMBDA_INIT, scalar2=-1.0, op0=ALU.add, op1=ALU.mult),
             reads=["lam"], writes=["nlam"])
        P.op("dve", lambda e: e.tensor_scalar(out=gs_col, in0=gs_col, scalar1=1.0 - LAMBDA_INIT, scalar2=None, op0=ALU.mult),
             reads=["gs"], writes=["gs"])

        wmix = bfv(HT_OFF + 4096, 4096).rearrange("p (c n) -> p c n", c=8)
        P.op("pool", lambda e: e.dma_start(out=wmix, in_=wmix_d.rearrange("(c p) n -> p c n", p=128)), writes=["wmix"], dma=True)

        for g8 in range(NSLOT // 2560):
            P.op("sp", lambda e, g8=g8: e.dma_start(out=yg_d[g8 * 2560:(g8 + 1) * 2560, :], in_=zeros_d[:, :]),
                 writes=[("YGZ", g8)], dma=True)
        catT = bfv(WR_OFF, 8192).rearrange("p (c t) -> p c t", c=8)
        wi_keys = [("wi", i) for i in range(5)]
        t1_keys = ["sqA_q", "sqA_k", "ssA", "rstdA", ("tA1", 0), ("tA1", 512), ("tA2", 0, 0), ("tA2", 0, 1),
                   ("tA2", 512, 0), ("tA2", 512, 1), "tB1", ("tB2", 0), ("tB2", 1)]
        cat_keys = [("catT", c, qt) for c in range(8) for qt in range(4)]
        P.alias(cat_keys, wi_keys)
        denA = [f32v(T1 + i * 512, 512) for i in range(2)]
        sq_bf = bfv(T1 + 1024, 256)
        rstdB = f32v(T1 + 1280, 512)
        P.alias([("denA", 0), ("denA", 1), "sq_bf", "rstdB"], t1_keys)
        Bt = [f32v(TMP_OFF + i * 512, 512) for i in range(4)]
        P.alias([("Bt", i) for i in range(4)], [("xin", 0), ("xin", 1)])
        P.alias([("E", i) for i in range(4)], [("qkB", 0), ("qkB", 1)] + [("qkA", h) for h in range(10)])
        allT = list(range(NT))
        stepsA = [(j, qt, sc) for j in range(4) for qt in range(4) for sc in range(NT)]

        def A_S(i):
            j, qt, sc = stepsA[i]
            qs = slice(qt * 512, (qt + 1) * 512)
            pair = i % 2
            for g in range(2):
                sb = 2 * pair + g
                kp = slice(g * 64, (g + 1) * 64)
                P.op("pe", lambda e, sb=sb, kp=kp: e.matmul(ps[sb][:, :], lhsT=KTA[kp, sc * 128:(sc + 1) * 128], rhs=QTA[kp, j, qs],
                                                            start=True, stop=True),
                     reads=[("KTA", sc)] + [("QTA", t) for t in range(qt * 4, qt * 4 + 4)], writes=[("ps", sb)])
            for g in range(2):
                sb = 2 * pair + g
                P.op("act", lambda e, sb=sb: e.activation(out=Ering[sb], in_=ps[sb][:, :], func=AF.Exp, scale=0.125),
                     reads=[("ps", sb)], writes=[("E", sb)])

        def A_PV(i):
            j, qt, sc = stepsA[i]
            qs = slice(qt * 512, (qt + 1) * 512)
            pair = i % 2
            grp = i // NT
            odd = j % 2
            for g in range(2):
                h = j + 4 * g
                c = h // 2
                es = 2 * pair + g
                ob = 4 + 2 * (grp % 2) + g
                vsl = VA[:, sc, g, 0:128] if odd else VA[:, sc, g, 64:192]
                P.op("pe", lambda e, ob=ob, vsl=vsl, es=es: e.matmul(ps[ob][:, :], lhsT=vsl, rhs=Ering[es], start=(sc == 0), stop=(sc == NT - 1)),
                     reads=[("VA", sc), "VAones0", "VAones1", ("E", es)], writes=[("ps", ob)])
            if sc == NT - 1:
                op_ = slice(odd * 64, odd * 64 + 64)
                dp_ = slice((1 - odd) * 64, (1 - odd) * 64 + 64)
                for g in range(2):
                    h = j + 4 * g
                    c = h // 2
                    ob = 4 + 2 * (grp % 2) + g
                    ds = g
                    P.op("dve", lambda e, ob=ob, ds=ds: e.reciprocal(out=denA[ds][op_, :], in_=ps[ob][dp_, :]),
                         reads=[("ps", ob)], writes=[("denA", ds)])
                    P.op("dve", lambda e, ob=ob, ds=ds, c=c: e.tensor_tensor(out=catT[op_, c, qs], in0=ps[ob][op_, :], in1=denA[ds][op_, :], op=ALU.mult),
                         reads=[("ps", ob), ("denA", ds)], writes=[("catT", c, qt)])

        for i in range(-1, len(stepsA)):
            if i + 1 < len(stepsA):
                A_S(i + 1)
            if i >= 0:
                A_PV(i)

        stepsB = [(h, qt, sc) for h in range(4) for qt in range(4) for sc in range(NT)]

        def B_S(i):
            h, qt, sc = stepsB[i]
            qs = slice(qt * 512, (qt + 1) * 512)
            pair = i % 2
            s1, s2 = 2 * pair, 2 * pair + 1
            rq = [("QKTB", t) for t in range(qt * 4, qt * 4 + 4)] + [("QKTB", sc)]
            P.op("pe", lambda e: e.matmul(ps[s1][:, :], lhsT=QKTB[0:64, 4 + h, sc * 128:(sc + 1) * 128], rhs=QKTB[0:64, h, qs], start=True, stop=True),
                 reads=rq, writes=[("ps", s1)])
            P.op("pe", lambda e: e.matmul(ps[s2][:, :], lhsT=QKTB[64:128, 4 + h, sc * 128:(sc + 1) * 128], rhs=QKTB[64:128, h, qs], start=True, stop=True),
                 reads=rq, writes=[("ps", s2)])
            P.op("act", lambda e: e.activation(out=Ering[s1], in_=ps[s1][:, :], func=AF.Exp, scale=0.125),
                 reads=[("ps", s1)], writes=[("E", s1)])
            P.op("act", lambda e: e.activation(out=Ering[s2], in_=ps[s2][:, :], func=AF.Exp, scale=0.125),
                 reads=[("ps", s2)], writes=[("E", s2)])

        def B_PV(i):
            h, qt, sc = stepsB[i]
            qs = slice(qt * 512, (qt + 1) * 512)
            pair = i % 2
            e1, e2 = 2 * pair, 2 * pair + 1
            vsl = VB[:, sc, h * 128:(h + 1) * 128]
            st_, sp_ = (sc == 0), (sc == NT - 1)
            P.op("pe", lambda e: e.matmul(ps[4][:, :], lhsT=vsl, rhs=Ering[e1], start=st_, stop=sp_),
                 reads=[("VB", sc), ("E", e1)], writes=[("ps", 4)])
            P.op("pe", lambda e: e.matmul(ps[6][:, :], lhsT=onesB, rhs=Ering[e1], start=st_, stop=sp_),
                 reads=["onesB", ("E", e1)], writes=[("ps", 6)])
            P.op("pe", lambda e: e.matmul(ps[5][:, :], lhsT=vsl, rhs=Ering[e2], start=st_, stop=sp_),
                 reads=[("VB", sc), ("E", e2)], writes=[("ps", 5)])
            P.op("pe", lambda e: e.matmul(ps[7][:, :], lhsT=onesB, rhs=Ering[e2], start=st_, stop=sp_),
                 reads=["onesB", ("E", e2)], writes=[("ps", 7)])
            if sc == NT - 1:
                P.op("dve", lambda e: e.reciprocal(out=Bt[0], in_=ps[6][:, :]), reads=[("ps", 6)], writes=[("Bt", 0)])
                P.op("dve", lambda e: e.tensor_tensor(out=Bt[2], in0=ps[4][:, :], in1=Bt[0], op=ALU.mult),
                     reads=[("ps", 4), ("Bt", 0)], writes=[("Bt", 2)])
                P.op("dve", lambda e: e.reciprocal(out=Bt[1], in_=ps[7][:, :]), reads=[("ps", 7)], writes=[("Bt", 1)])
                P.op("dve", lambda e: e.scalar_tensor_tensor(out=Bt[3], in0=ps[5][:, :], scalar=nlam_col, in1=Bt[1], op0=ALU.mult, op1=ALU.mult),
                     reads=[("ps", 5), ("Bt", 1), "nlam"], writes=[("Bt", 3)])
                P.op("dve", lambda e: e.tensor_tensor(out=Bt[2], in0=Bt[2], in1=Bt[3], op=ALU.add),
                     reads=[("Bt", 2), ("Bt", 3)], writes=[("Bt", 2)])
                P.op("dve", lambda e: e.tensor_tensor(out=sq_bf, in0=Bt[2], in1=Bt[2], op=ALU.mult),
                     reads=[("Bt", 2)], writes=["sq_bf"])
                P.op("pe", lambda e: e.matmul(ps[7][:, :], lhsT=onesB, rhs=sq_bf, start=True, stop=True),
                     reads=["onesB", "sq_bf"], writes=[("ps", 7)])
                P.op("act", lambda e: e.activation(out=rstdB, in_=ps[7][:, :], func=AF.Ln, scale=1.0 / 128.0, bias=rmseps_col),
                     reads=[("ps", 7), "eps2"], writes=["rstdB"])
                P.op("act", lambda e: e.activation(out=rstdB, in_=rstdB, func=AF.Exp, scale=-0.5), reads=["rstdB"], writes=["rstdB"])
                P.op("dve", lambda e: e.scalar_tensor_tensor(
                    out=catT[:, 4 + h, qs], in0=Bt[2], scalar=gs_col, in1=rstdB, op0=ALU.mult, op1=ALU.mult),
                    reads=[("Bt", 2), "rstdB", "gs"], writes=[("catT", 4 + h, qt)])

        for i in range(-1, len(stepsB)):
            if i + 1 < len(stepsB):
                B_S(i + 1)
            if i >= 0:
                B_PV(i)

        if stage == "p1d":
            P.op("pool", lambda e: e.dma_start(out=dbg_d[:, 0:16384], in_=bfv(WR_OFF, 8192)), reads=cat_keys, writes=["dbgout"], dma=True)
            return finish([P.writer["dbgout"]])

        mem_st = f32v(TMP_OFF, 2048).rearrange("p (c d) -> p c d", c=2)
        P.alias(["mem_st"], [("Bt", i) for i in range(4)])
        P.op("sp", lambda e: e.dma_start(out=mem_st, in_=mem_d.rearrange("(c p) d -> p c d", p=128)), writes=["mem_st"], dma=True)
        KmT = bfv(HT_OFF, 1024).rearrange("p (h j m) -> p h j m", h=4, j=2)
        Vm = bfv(HT_OFF + 1024, 1024).rearrange("p (c n) -> p c n", c=2)
        memT = bfv(HT_OFF + 2048, 1024).rearrange("p (c m) -> p c m", c=8)
        P.alias(["KmT", "Vm", "memT"], [("VB", t) for t in allT])
        for mc in range(2):
            for half in range(2):
                b = next_bank()
                for jj in range(4):
                    dc = half * 4 + jj
                    P.op("pe", lambda e, b=b, jj=jj, dc=dc, mc=mc: e.transpose(
                        ps[b][:, jj * 128:(jj + 1) * 128], mem_st[:, mc, dc * 128:(dc + 1) * 128], identF),
                        reads=["mem_st", "identF"], writes=[("ps", b)])
                P.op("act", lambda e, b=b, half=half, mc=mc: e.activation(
                    out=memT[:, half * 4:(half + 1) * 4, mc * 128:(mc + 1) * 128],
                    in_=ps[b][:].rearrange("p (j t) -> p j t", j=4), func=AF.Copy),
                    reads=[("ps", b)], writes=[("memT", mc, half)])
        memT_keys = [("memT", mc, half) for mc in range(2) for half in range(2)]
        for h in range(4):
            for j in range(2):
                b = next_bank()
                for dc in range(8):
                    P.op("pe", lambda e, b=b, dc=dc, h=h, j=j: e.matmul(
                        ps[b][:, 0:256], lhsT=wkv[:, dc, h * 256 + j * 128:h * 256 + (j + 1) * 128], rhs=memT[:, dc, :],
                        start=(dc == 0), stop=(dc == 7)), reads=["wkv"] + memT_keys, writes=[("ps", b)])
                P.op("act", lambda e, b=b, h=h, j=j: e.activation(out=KmT[:, h, j, :], in_=ps[b][:, 0:256], func=AF.Copy),
                     reads=[("ps", b)], writes=[("KmT", h, j)])
        for mc in range(2):
            for half in range(2):
                b = next_bank()
                for dc in range(8):
                    P.op("pe", lambda e, b=b, dc=dc, mc=mc, half=half: e.matmul(
                        ps[b][:, :], lhsT=memT[:, dc, mc * 128:(mc + 1) * 128], rhs=wkv[:, dc, 1024 + half * 512:1024 + (half + 1) * 512],
                        start=(dc == 0), stop=(dc == 7)), reads=["wkv"] + memT_keys, writes=[("ps", b)])
                P.op("dve", lambda e, b=b, mc=mc, half=half: e.tensor_copy(out=Vm[:, mc, half * 512:(half + 1) * 512], in_=ps[b][:, :]),
                     reads=[("ps", b)], writes=[("Vm", mc, half)])
        KmT_keys = [("KmT", h, j) for h in range(4) for j in range(2)]
        Vm_keys = [("Vm", mc, half) for mc in range(2) for half in range(2)]
        P.alias(XTk(range(NT)), ["wkv"])

        P.alias(["lnp"], [("ropeA", 0), ("ropeA", 1), ("ropeB", 0), ("ropeB", 1), "lamv"])
        P.op("sp", lambda e: e.dma_start(out=lnp, in_=ln_d[0:2, :].partition_broadcast(128)), writes=["lnp"], dma=True)
        att_keys = ([("QTA", t) for t in allT] + [("KTA", t) for t in allT] + [("QKTB", t) for t in allT] +
                    [("VA", t) for t in allT] + ["VAones0", "VAones1"])
        P.alias([("R", t) for t in allT], att_keys)
        ytile = [f32v(T1 + i * 1024, 1024) for i in range(2)]
        xn = f32v(T1 + 2048, 1024)
        P.alias([("y", 0), ("y", 1), "xn"], [("denA", 0), ("denA", 1), "sq_bf", "rstdB"])
        P.alias([("xin", 0), ("xin", 1)], [("Bt", i) for i in range(4)] + ["mem_st"])
        stats = small[:, 16:28].rearrange("p (c s) -> p c s", c=2)
        mv = small[:, 28:30]
        rstd1 = small[:, 30:31]
        nmr1 = small[:, 31:32]

        small2 = f32v(misc(64), 64)

        def ln_stats(ysrc, ykey, par):
            st_ = small2[:, par * 16:par * 16 + 12].rearrange("p (c s) -> p c s", c=2)
            mv_ = small2[:, par * 16 + 12:par * 16 + 14]
            rs_ = small2[:, par * 16 + 14:par * 16 + 15]
            nm_ = small2[:, par * 16 + 15:par * 16 + 16]
            P.op("dve", lambda e: e.bn_stats(out=st_[:, 0, :], in_=ysrc[:, 0:512]), reads=[ykey], writes=[("stats0", par)])
            P.op("dve", lambda e: e.bn_stats(out=st_[:, 1, :], in_=ysrc[:, 512:1024]), reads=[ykey], writes=[("stats1", par)])
            P.op("dve", lambda e: e.bn_aggr(out=mv_, in_=st_), reads=[("stats0", par), ("stats1", par)], writes=[("mv", par)])
            P.op("act", lambda e: e.activation(out=rs_, in_=mv_[:, 1:2], func=AF.Ln, bias=eps_col), reads=[("mv", par), "eps"], writes=[("rstd1", par)])
            P.op("act", lambda e: e.activation(out=rs_, in_=rs_, func=AF.Exp, scale=-0.5), reads=[("rstd1", par)], writes=[("rstd1", par)])
            P.op("dve", lambda e: e.scalar_tensor_tensor(out=nm_, in0=mv_[:, 0:1], scalar=-1.0, in1=rs_, op0=ALU.mult, op1=ALU.mult),
                 reads=[("mv", par), ("rstd1", par)], writes=[("nmr1", par)])

        def ln_apply(ysrc, ykey, par, dst, dst_keys, lnkey, xnb, xnk):
            rs_ = small2[:, par * 16 + 14:par * 16 + 15]
            nm_ = small2[:, par * 16 + 15:par * 16 + 16]
            P.op("act", lambda e: e.activation(out=xnb, in_=ysrc, func=AF.Identity, scale=rs_, bias=nm_),
                 reads=[ykey, ("rstd1", par), ("nmr1", par)], writes=[xnk])
            P.op("dve", lambda e: e.tensor_tensor(out=xnb, in0=xnb, in1=lnp[:, 0, :], op=ALU.mult), reads=[xnk, lnkey], writes=[xnk])
            P.op("dve", lambda e: e.tensor_tensor(out=dst, in0=xnb, in1=lnp[:, 1, :], op=ALU.add), reads=[xnk, lnkey], writes=dst_keys)

        def skewed(n, stages):
            ns = len(stages)
            for t in range(n + ns - 1):
                for si in range(ns - 1, -1, -1):
                    k = t - si
                    if 0 <= k < n:
                        stages[si](k)

        xnA = [xn, f32v(TMP_OFF + 2048, 1024)]
        P.alias([("xnA", 0), ("xnA", 1)], ["xn"] + [("E", i) for i in range(4)])

        def l1_s0(tc):
            par = tc % 2
            load_x(tc, par)
            for dh in range(2):
                b = par * 2 + dh
                for c in range(8):
                    P.op("pe", lambda e, b=b, c=c, dh=dh: e.matmul(
                        ps[b][:, :], lhsT=catT[:, c, tc * 128:(tc + 1) * 128], rhs=wmix[:, c, dh * 512:(dh + 1) * 512],
                        start=(c == 0), stop=(c == 7)),
                        reads=[("catT", c, tc // 4), "wmix"], writes=[("ps", b)])
                P.op("dve", lambda e, b=b, dh=dh: e.scalar_tensor_tensor(
                    out=ytile[par][:, dh * 512:(dh + 1) * 512], in0=xin[par][:, dh * 512:(dh + 1) * 512], scalar=ALPHA,
                    in1=ps[b][:, :], op0=ALU.mult, op1=ALU.add),
                    reads=[("xin", par), ("ps", b)], writes=[("y", par, dh)])

        def l1_s1(tc):
            par = tc % 2
            P.op("dve", lambda e: e.engine_nop(), reads=[("y", par, 0), ("y", par, 1)], writes=[("y", par)])
            ln_stats(ytile[par], ("y", par), par)

        def l1_s2(tc):
            par = tc % 2
            ln_apply(ytile[par], ("y", par), par, Rt[:, tc, :], [("R", tc)], "lnp", xnA[par], ("xnA", par))
            P.alias([("y", par, 0), ("y", par, 1)], [("y", par)])

        def l1_s3(tc):
            par = tc % 2
            transpose_tile_to_XT(Rt[:, tc, :], ("R", tc), tc, (4 + par * 2, 5 + par * 2))

        skewed(NT, [l1_s0, l1_s1, l1_s2, l1_s3])

        if stage == "p1":
            toks = []
            for tc in range(NT):
                toks.append(P.op("sp", lambda e, tc=tc: e.dma_start(out=out_d[tc * 128:(tc + 1) * 128, :], in_=Rt[:, tc, :]),
                                 reads=[("R", tc)], writes=[("out", tc)], dma=True))
            return finish(toks)


        wq = bfv(WR_OFF, 4096).rearrange("p (c n) -> p c n", c=8)
        wo = bfv(WR_OFF + 4096, 4096).rearrange("p (c n) -> p c n", c=8)
        P.alias(["wq", "wo"], cat_keys)
        P.op("pool", lambda e: e.dma_start(out=wq, in_=wq_d.rearrange("(c p) n -> p c n", p=128)), writes=["wq"], dma=True)
        P.op("pool", lambda e: e.dma_start(out=wo, in_=wo_d.rearrange("(c p) n -> p c n", p=128)), writes=["wo"], dma=True)
        qT_tile = bfv(WR_OFF + 8192, 2048).rearrange("p (c t) -> p c t", c=8)
        oT_tile = bfv(WR_OFF + 10240, 2048).rearrange("p (c t) -> p c t", c=8)
        P.alias([("qT", c) for c in range(8)] + [("oT", c) for c in range(8)], [("y", 0), ("y", 1), ("y", 0, 0), ("y", 0, 1), ("y", 1, 0), ("y", 1, 1), "xn"])
        bd_sb = f32v(HT_OFF + 2048, 1024)
        y2 = f32v(HT_OFF + 3072, 1024)
        xn2 = f32v(HT_OFF + 4096, 1024)
        x2t = f32v(HT_OFF + 5120, 1024)
        x2Tf = f32v(HT_OFF + 6144, 1024).rearrange("p (c t) -> p c t", c=8)
        CTc = f32v(HT_OFF + 7168, 128)
        gsm = f32v(HT_OFF + 7296, 128)
        gsm2 = f32v(HT_OFF + 7424, 256)
        idx8 = gsm2[:, 0:8].bitcast(U32)
        idxf = gsm2[:, 8:12]
        posk = gsm2[:, 12:16]
        destf = gsm2[:, 16:20]
        ovf = gsm2[:, 20:24]
        gk4 = gsm2[:, 24:28]
        msk_bf = gsm2[:, 32:48].bitcast(BF16)
        posf = gsm2[:, 48:80]
        sel4 = gsm2[:, 96:224].rearrange("p (k e) -> p k e", k=4)
        lg = gsm[:, 0:32]
        top8 = gsm[:, 32:40]
        negm = gsm[:, 40:41]
        ssum = gsm[:, 41:42]
        msk = gsm[:, 48:80]
        exg = gsm[:, 80:112]
        P.alias(["bd", "y2", "xn2", "x2t", "x2Tf", "CTc", "gsm", "idx8", "idxf", "posk", "destf", "ovf", "gk4", "msk_bf", "posf", "sel4"], ["memT", "wmix"] + memT_keys)
        P.op("sp", lambda e: e.dma_start(out=wr_sb, in_=wr_d.rearrange("(c p) n -> p c n", p=128)), writes=["wr"], dma=True)
        P.op("sp", lambda e: e.dma_start(out=br_sb, in_=br_d.partition_broadcast(128)), writes=["br"], dma=True)
        P.op("sp", lambda e: e.dma_start(out=lnp, in_=ln_d[2:4, :].partition_broadcast(128)), writes=["lnp"], dma=True)
        dent = [f32v(TMP_OFF + i * 512, 512) for i in range(2)]
        P.alias([("dent", 0), ("dent", 1)], [("xin", 0), ("xin", 1), "mem_st"])
        e_rr2 = [0]

        xb_rr = [0]

        def xbank():
            v = xb_rr[0] % 6
            xb_rr[0] += 1
            return v

        def xattn_tile(tt):
            ts_ = slice(tt * 512, (tt + 1) * 512)
            tcs = list(range(tt * 4, tt * 4 + 4))
            for c in range(8):
                b = xbank()
                for dc in range(8):
                    P.op("pe", lambda e, b=b, dc=dc, c=c, ts_=ts_: e.matmul(
                        ps[b][:, :], lhsT=wq[:, dc, c * 128:(c + 1) * 128], rhs=XT[:, dc, ts_], start=(dc == 0), stop=(dc == 7)),
                        reads=["wq"] + XTk(tcs), writes=[("ps", b)])
                if c % 2 == 0:
                    P.op("act", lambda e, b=b, c=c: e.activation(out=qT_tile[:, c, :], in_=ps[b][:, :], func=AF.Copy),
                         reads=[("ps", b)], writes=[("qT", c)])
                else:
                    P.op("dve", lambda e, b=b, c=c: e.tensor_copy(out=qT_tile[:, c, :], in_=ps[b][:, :]),
                         reads=[("ps", b)], writes=[("qT", c)])
            for h in range(4):
                eslots = []
                for mc in range(2):
                    sb = xbank()
                    for j in range(2):
                        P.op("pe", lambda e, sb=sb, h=h, j=j, mc=mc: e.matmul(
                            ps[sb][:, :], lhsT=KmT[:, h, j, mc * 128:(mc + 1) * 128], rhs=qT_tile[:, h * 2 + j, :],
                            start=(j == 0), stop=(j == 1)), reads=KmT_keys + [("qT", h * 2 + j)], writes=[("ps", sb)])
                    es = e_rr2[0] % 4
                    e_rr2[0] += 1
                    eslots.append(es)
                    P.op("act", lambda e, sb=sb, es=es: e.activation(out=Ering[es], in_=ps[sb][:, :], func=AF.Exp, scale=1.0 / 16.0),
                         reads=[("ps", sb)], writes=[("E", es)])
                obs = []
                for j in range(2):
                    ob = xbank()
                    obs.append(ob)
                    for mc in range(2):
                        P.op("pe", lambda e, ob=ob, mc=mc, h=h, j=j, es=eslots[mc]: e.matmul(
                            ps[ob][:, :], lhsT=Vm[:, mc, h * 256 + j * 128:h * 256 + (j + 1) * 128], rhs=Ering[es],
                            start=(mc == 0), stop=(mc == 1)), reads=Vm_keys + [("E", eslots[mc])], writes=[("ps", ob)])
                db = xbank()
                for mc in range(2):
                    P.op("pe", lambda e, db=db, mc=mc, es=eslots[mc]: e.matmul(
                        ps[db][:, :], lhsT=onesB, rhs=Ering[es], start=(mc == 0), stop=(mc == 1)),
                        reads=["onesB", ("E", eslots[mc])], writes=[("ps", db)])
                ds = h % 2
                P.op("dve", lambda e, db=db, ds=ds: e.reciprocal(out=dent[ds], in_=ps[db][:, :]), reads=[("ps", db)], writes=[("dent", ds)])
                for j in range(2):
                    P.op("dve", lambda e, ob=obs[j], ds=ds, h=h, j=j: e.tensor_tensor(
                        out=oT_tile[:, h * 2 + j, :], in0=ps[ob][:, :], in1=dent[ds], op=ALU.mult),
                        reads=[("ps", obs[j]), ("dent", ds)], writes=[("oT", h * 2 + j)])

        y2b = [f32v(HT_OFF + 3072 + i * 1024, 1024) for i in range(2)]
        x2tb = [f32v(HT_OFF + 5120 + i * 1024, 1024) for i in range(2)]
        x2Tf = f32v(TMP_OFF + 1024, 1024).rearrange("p (c t) -> p c t", c=8)
        P.alias([("y2b", 0), ("y2b", 1), ("x2tb", 0), ("x2tb", 1), ("x2Tf", 0), ("x2Tf", 1)],
                ["y2", "xn2", "x2t", "x2Tf", "memT", "wmix"] + memT_keys + [("xnA", 1)])

        Wring = [bfv(WR_OFF + i * 2048, 2048).rearrange("p (c n) -> p c n", c=8) for i in range(6)]
        wsrc = (weg_d, weu_d, wed_d)

        def unit_src(e_, k):
            if k < 4:
                t = wsrc[k % 2][e_]
                half = k // 2
            else:
                t = wsrc[2][e_]
                half = k - 4
            return t.rearrange("(c p) n -> p c n", p=128)[:, :, half * 512:(half + 1) * 512]

        def load_unit(e_, k):
            P.op("pool", lambda e, e_=e_, k=k: e.dma_start(out=Wring[k], in_=unit_src(e_, k)), writes=[("W", k)], dma=True)


        def p2_s0(tc):
            par = tc % 2
            if tc % 4 == 0:
                xattn_tile(tc // 4)
                if tc == 12:
                    P.alias([("W", 0), ("W", 1)], ["wq"])
                    load_unit(0, 0)
                    load_unit(0, 1)
            tcl = tc % 4
            for dh in range(2):
                b = par * 2 + dh
                for c in range(8):
                    P.op("pe", lambda e, b=b, c=c, dh=dh: e.matmul(
                        ps[b][:, :], lhsT=oT_tile[:, c, tcl * 128:(tcl + 1) * 128], rhs=wo[:, c, dh * 512:(dh + 1) * 512],
                        start=(c == 0), stop=(c == 7)), reads=[("oT", c), "wo"], writes=[("ps", b)])
                P.op("dve", lambda e, b=b, dh=dh: e.scalar_tensor_tensor(
                    out=y2b[par][:, dh * 512:(dh + 1) * 512], in0=Rt[:, tc, dh * 512:(dh + 1) * 512], scalar=ALPHA,
                    in1=ps[b][:, :], op0=ALU.mult, op1=ALU.add), reads=[("R", tc), ("ps", b)], writes=[("y2b", par, dh)])

        def p2_s1(tc):
            par = tc % 2
            P.op("dve", lambda e: e.engine_nop(), reads=[("y2b", par, 0), ("y2b", par, 1)], writes=[("y2b", par)])
            ln_stats(y2b[par], ("y2b", par), par)

        def p2_s2(tc):
            par = tc % 2
            ln_apply(y2b[par], ("y2b", par), par, x2tb[par], [("x2tb", par)], "lnp", y2b[par], ("y2b", par))
            P.alias([("y2b", par, 0), ("y2b", par, 1)], [("y2b", par)])

        def p2_s2b(tc):
            par = tc % 2
            P.op("act", lambda e: e.activation(out=Rt[:, tc, :], in_=x2tb[par], func=AF.Copy, scale=ALPHA),
                 reads=[("x2tb", par)], writes=[("R", tc)])
            P.op("pool", lambda e: e.dma_start(out=xrows_d[tc * 128:(tc + 1) * 128, :], in_=x2tb[par]),
                 reads=[("x2tb", par)], writes=[("XROWS", tc)], dma=True)
            for half in range(2):
                b = 4 + half
                for jj in range(4):
                    dc = half * 4 + jj
                    P.op("pe", lambda e, b=b, jj=jj, dc=dc: e.transpose(ps[b][:, jj * 128:(jj + 1) * 128],
                                                                        x2tb[par][:, dc * 128:(dc + 1) * 128], identF),
                         reads=[("x2tb", par), "identF"], writes=[("ps", b)])
                srcp = ps[b][:].rearrange("p (j t) -> p j t", j=4)
                P.op("act", lambda e, half=half, srcp=srcp: e.activation(
                    out=x2Tf[:, half * 4:(half + 1) * 4, :], in_=srcp, func=AF.Copy),
                    reads=[("ps", b)], writes=[("x2Tf", half)])

        def p2_s2c(tc):
            par = tc % 2
            lb = 6 + par
            for dc in range(8):
                P.op("pe", lambda e, dc=dc: e.matmul(ps[lb][:, 0:32], lhsT=x2Tf[:, dc, :], rhs=wr_sb[:, dc, :],
                                                     start=(dc == 0), stop=(dc == 7)),
                     reads=[("x2Tf", 0), ("x2Tf", 1), "wr"], writes=[("ps", lb)])

        def p2_s3a(tc):
            par = tc % 2
            lb = 6 + par
            P.op("dve", lambda e: e.tensor_tensor(out=lg, in0=ps[lb][:, 0:32], in1=br_sb, op=ALU.add),
                 reads=[("ps", lb), "br"], writes=["lg"])
            P.op("dve", lambda e: e.max(out=top8, in_=lg), reads=["lg"], writes=["top8"])
            P.op("dve", lambda e: e.tensor_scalar(out=msk, in0=lg, scalar1=top8[:, 3:4], scalar2=None, op0=ALU.is_ge),
                 reads=["lg", "top8"], writes=["msk"])
            P.op("dve", lambda e: e.tensor_scalar(out=negm, in0=top8[:, 0:1], scalar1=-1.0, scalar2=None, op0=ALU.mult),
                 reads=["top8"], writes=["negm"])
            P.op("act", lambda e: e.activation(out=exg, in_=lg, func=AF.Exp, bias=negm), reads=["lg", "negm"], writes=["exg"])
            P.op("dve", lambda e: e.tensor_copy(out=msk_bf, in_=msk), reads=["msk"], writes=["msk_bf"])
            P.op("dve", lambda e: e.max_index(out=idx8, in_max=top8, in_values=lg), reads=["top8", "lg"], writes=["idx8"])
            P.op("dve", lambda e: e.tensor_copy(out=idxf, in_=idx8[:, 0:4]), reads=["idx8"], writes=["idxf"])
            P.op("dve", lambda e: e.tensor_tensor(out=exg, in0=exg, in1=msk, op=ALU.mult), reads=["exg", "msk"], writes=["exg"])
            P.op("dve", lambda e: e.reduce_sum(out=ssum, in_=exg, axis=AX.X), reads=["exg"], writes=["ssum"])
            P.op("dve", lambda e: e.reciprocal(out=ssum, in_=ssum), reads=["ssum"], writes=["ssum"])
            P.op("act", lambda e: e.activation(out=gk4, in_=top8[:, 0:4], func=AF.Exp, bias=negm), reads=["top8", "negm"], writes=["gk4"])
            P.op("dve", lambda e: e.tensor_scalar(out=gk4, in0=gk4, scalar1=ssum, scalar2=None, op0=ALU.mult),
                 reads=["gk4", "ssum"], writes=["gk4"])
            P.op("pe", lambda e: e.matmul(ps[lb][:, 64:96], lhsT=Ltri, rhs=msk_bf, start=True, stop=True),
                 reads=["Ltri", "msk_bf", "lg"], writes=[("ps", lb)])
            P.op("pe", lambda e: e.matmul(ps[lb][:, 96:128], lhsT=onesB, rhs=msk_bf, start=True, stop=True),
                 reads=["onesB", "msk_bf"], writes=[("ps", lb)])

        def p2_s3b(tc):
            par = tc % 2
            pb = 6 + par
            P.op("dve", lambda e: e.tensor_tensor(out=posf, in0=ps[pb][:, 64:96], in1=cnt, op=ALU.add),
                 reads=[("ps", pb), "cnt"], writes=["posf"])
            P.op("dve", lambda e: e.tensor_tensor(out=cnt, in0=ps[pb][:, 96:128], in1=cnt, op=ALU.add),
                 reads=[("ps", pb), "cnt", "posf"], writes=["cnt"])
            P.op("dve", lambda e: e.tensor_tensor(out=sel4, in0=iota32.unsqueeze(1).to_broadcast([128, 4, 32]),
                                                  in1=idxf.unsqueeze(2).to_broadcast([128, 4, 32]), op=ALU.is_equal),
                 reads=["iota32", "idxf"], writes=["sel4"])
            P.op("dve", lambda e: e.tensor_tensor(out=sel4, in0=sel4, in1=posf.unsqueeze(1).to_broadcast([128, 4, 32]), op=ALU.mult),
                 reads=["sel4", "posf"], writes=["sel4"])
            P.op("dve", lambda e: e.reduce_sum(out=posk, in_=sel4, axis=AX.X), reads=["sel4"], writes=["posk"])
            P.op("dve", lambda e: e.scalar_tensor_tensor(out=destf, in0=idxf, scalar=float(CAP), in1=posk, op0=ALU.mult, op1=ALU.add),
                 reads=["idxf", "posk"], writes=["destf"])
            P.op("dve", lambda e: e.tensor_scalar(out=ovf, in0=posk, scalar1=float(CAP), scalar2=None, op0=ALU.is_ge),
                 reads=["posk"], writes=["ovf"])
            P.op("dve", lambda e: e.scalar_tensor_tensor(out=destf, in0=ovf, scalar=4.0e6, in1=destf, op0=ALU.mult, op1=ALU.add),
                 reads=["ovf", "destf"], writes=["destf"])
            P.op("dve", lambda e: e.tensor_copy(out=DEST[:, tc, :], in_=destf), reads=["destf"], writes=[("DEST", tc)])
            P.op("dve", lambda e: e.tensor_scalar(out=destf, in0=destf, scalar1=float(NSLOT - 1), scalar2=None, op0=ALU.min),
                 reads=["destf", ("DEST", tc)], writes=["destf"])
            P.op("dve", lambda e: e.tensor_copy(out=DESTG[:, tc, :], in_=destf), reads=["destf"], writes=[("DESTG", tc)])
            P.op("dve", lambda e: e.tensor_scalar(out=ovf, in0=ovf, scalar1=-1.0, scalar2=1.0, op0=ALU.mult, op1=ALU.add),
                 reads=["ovf", "destf"], writes=["ovf"])
            P.op("dve", lambda e: e.tensor_tensor(out=Gk[:, tc, :], in0=gk4, in1=ovf, op=ALU.mult),
                 reads=["gk4", "ovf"], writes=[("Gk", tc)])
            for k in range(4):
                P.op("pool", lambda e, k=k: e.indirect_dma_start(
                    out=tok_d[:, :], out_offset=bass.IndirectOffsetOnAxis(ap=DEST_t[:, tc * 4 + k:tc * 4 + k + 1], axis=0),
                    in_=TOKID_t[:, tc:tc + 1], in_offset=None, bounds_check=pool_regs["bc"], oob_is_err=False),
                    reads=[("DEST", tc), "TOKID", "TOKZ"], writes=[("TOK", tc, k)], dma=True)

        def p2_s3c(tc):
            for dh in range(2):
                b = 4 + dh
                P.op("pe", lambda e, b=b, dh=dh: e.matmul(ps[b][:, :], lhsT=CTc[0:32, :], rhs=bd_sb[0:32, dh * 512:(dh + 1) * 512],
                                                           start=True, stop=True), reads=["CTc", "bd"], writes=[("ps", b)])
                P.op("dve", lambda e, b=b, dh=dh: e.tensor_tensor(
                    out=Rt[:, tc, dh * 512:(dh + 1) * 512], in0=ps[b][:, :], in1=Rt[:, tc, dh * 512:(dh + 1) * 512], op=ALU.add),
                    reads=[("ps", b), ("R", tc)], writes=[("R", tc)])

        skewed(NT, [p2_s0, p2_s1, p2_s2, p2_s2b, p2_s2c, p2_s3a, p2_s3b])

        if stage == "p2":
            toks = []
            for tc in range(NT):
                toks.append(P.op("sp", lambda e, tc=tc: e.dma_start(out=out_d[tc * 128:(tc + 1) * 128, :], in_=Rt[:, tc, :]),
                                 reads=[("R", tc), ("R", tc, 0), ("R", tc, 1)], writes=[("out", tc)], dma=True))
            return finish(toks)

        P.alias([("W", i) for i in range(2, 6)], ["wq", "wo"] + [("qT", c) for c in range(8)] + [("oT", c) for c in range(8)])
        xg_tok = [bfv(XT_OFF, 2560).rearrange("p (a d) -> p a d", a=NCH)] * 2
        xgTb = [bfv(XT_OFF + 2560 + i * 2560, 2560).rearrange("p (c j) -> p c j", c=8) for i in range(2)]
        P.alias([("xg", 0, a) for a in range(NCH)] + [("xgT", i, a) for i in range(2) for a in range(NCH)], XTk(range(NT)))
        hTs = bfv(HT_OFF, 2560).rearrange("p (f j) -> p f j", f=8)
        ystage = [f32v(HT_OFF + 2560 + i * 1024, 1024) for i in range(3)]
        bdb = [f32v(HT_OFF + 5632 + i * 1024, 1024) for i in range(2)]
        ph2_keys = (["bd", "y2", ("y2", 0), ("y2", 1), "xn2", "x2t", ("x2Tf", 0), ("x2Tf", 1), "CTc", "lg", "top8", "msk", "negm",
                     "exg", "ssum", "idx8", "idxf", "posk", "destf", "ovf", "gk4", "msk_bf", "posf", "sel4"] + KmT_keys + Vm_keys)
        P.alias([("hTs", f) for f in range(8)] + [("ys", i) for i in range(3)] + [("bdb", 0), ("bdb", 1)],
                ph2_keys + [("y2b", 0), ("y2b", 1), ("x2tb", 0), ("x2tb", 1), ("y2b", 0, 0), ("y2b", 0, 1), ("y2b", 1, 0), ("y2b", 1, 1)])
        HW_ = CAP // 2
        rg = [f32v(TMP_OFF + i * 512, HW_) for i in range(2)]
        sg = [f32v(TMP_OFF + 1024 + i * 512, HW_) for i in range(2)]
        pp = [f32v(TMP_OFF + 2048 + i * 512, HW_) for i in range(2)]
        P.alias([("rg", 0), ("rg", 1), ("sg", 0), ("sg", 1), ("pp", 0), ("pp", 1)],
                [("dent", 0), ("dent", 1)] + [("E", i) for i in range(4)])
        for k in range(2, 6):
            load_unit(0, k)
        yv = f32v(TMP_OFF, NE * NCH).rearrange("p (e a) -> p e a", e=NE)
        yb = f32v(TMP_OFF + 256, NE * NCH).rearrange("p (e a) -> p e a", e=NE)
        ysp = f32v(TMP_OFF + 512, NCH)
        yi_i = f32v(TMP_OFF + 768, NE * NCH).bitcast(I32)
        P.alias(["yv", "yb", "ysp", "yi_i"], [("dent", 0), ("dent", 1), ("x2Tf", 0), ("x2Tf", 1)] + [("E", i) for i in range(4)])
        P.op("pool", lambda e: e.iota(yi_i, pattern=[[CAP, NE], [1, NCH]], base=0, channel_multiplier=NCH), writes=["yi_i"])
        P.op("pool", lambda e: e.tensor_copy(out=yb, in_=yi_i.rearrange("p (e a) -> p e a", e=NE)), reads=["yi_i"], writes=["yb"])
        P.op("pool", lambda e: e.tensor_copy(out=ysp, in_=yi_i[:, 0:NCH]), reads=["yi_i"], writes=["ysp"])
        P.op("dve", lambda e: e.tensor_tensor(out=yv, in0=ysp.unsqueeze(1).to_broadcast([128, NE, NCH]),
                                              in1=cnt.unsqueeze(2).to_broadcast([128, NE, NCH]), op=ALU.is_lt),
             reads=["ysp", "cnt"], writes=["yv"])
        P.op("dve", lambda e: e.tensor_scalar(out=yv, in0=yv, scalar1=-30000.0, scalar2=30000.0, op0=ALU.mult, op1=ALU.add),
             reads=["yv"], writes=["yv"])
        P.op("dve", lambda e: e.tensor_tensor(out=yv, in0=yv, in1=yb, op=ALU.add), reads=["yv", "yb"], writes=["yv"])
        P.op("dve", lambda e: e.tensor_copy(out=YIDX_t[:, :].rearrange("p (e a) -> p e a", e=NE), in_=yv), reads=["yv"], writes=["YIDX"])
        P.alias([("rg", 0), ("rg", 1), ("sg", 0), ("sg", 1), ("pp", 0), ("pp", 1)], ["yv", "yb", "ysp", "yi_i"])
        tok_keys = ["TOKZ"] + [("TOK", tc, k) for tc in range(NT) for k in range(4)]
        P.op("pool", lambda e: e.dma_start(out=gidx_t[:, :].rearrange("p (e a) -> p e a", e=NE),
                                           in_=tok_d.rearrange("(e p a) o -> p e (a o)", e=NE, p=128)),
             reads=tok_keys + ["gidx"], writes=["gidx"], dma=True)
        xrow_keys = [("XROWS", tc) for tc in range(NT)]

        def gather_expert(ex):
            for a in range(NCH):
                P.op("pool", lambda e, ex=ex, a=a: e.indirect_dma_start(
                    out=xg_tok[0][:, a, :], out_offset=None, in_=xrows_d[:, :],
                    in_offset=bass.IndirectOffsetOnAxis(ap=gidx_t[:, ex * NCH + a:ex * NCH + a + 1], axis=0),
                    bounds_check=pool_regs["bx"], oob_is_err=False),
                    reads=["gidx", "xgzero"] + xrow_keys, writes=[("xg", 0, a)], dma=True)

        evf = [0]

        def transpose_expert(ex):
            tb = ex % 2
            for a in range(NCH):
                b = next_bank()
                for dc in range(8):
                    P.op("pe", lambda e, b=b, dc=dc, a=a: e.transpose(
                        psb[b][:, dc * 128:(dc + 1) * 128], xg_tok[0][:, a, dc * 128:(dc + 1) * 128], identB),
                        reads=[("xg", 0, a), "identB"], writes=[("ps", b)])
                src3 = psb[b][:].rearrange("p (c j) -> p c j", c=8)
                dst3 = xgTb[tb][:, :, a * 128:(a + 1) * 128]
                if evf[0] % 2 == 0:
                    P.op("act", lambda e, src3=src3, dst3=dst3: e.activation(out=dst3, in_=src3, func=AF.Copy),
                         reads=[("ps", b)], writes=[("xgT", tb, a)])
                else:
                    P.op("dve", lambda e, src3=src3, dst3=dst3: e.tensor_copy(out=dst3, in_=src3),
                         reads=[("ps", b)], writes=[("xgT", tb, a)])
                evf[0] += 1

        P.alias(["xgzero"], XTk(range(NT)))
        P.op("pool", lambda e: e.memset(xg_tok[0], 0.0), reads=[("xg", 0, a) for a in range(NCH)], writes=["xgzero"] + [("xg", 0, a) for a in range(NCH)])
        gather_expert(0)
        transpose_expert(0)
        if NE > 1:
            gather_expert(1)
        sl = [0]
        ysl = [0]
        for ex in range(NE):
            xgT = xgTb[ex % 2]
            xgT_keys = [("xgT", ex % 2, a) for a in range(NCH)]
            for fc in range(8):
                half = fc // 4
                gslot, uslot = 2 * half, 2 * half + 1
                cl = (fc % 4) * 128
                for hh in range(2):
                    js = slice(hh * HW_, (hh + 1) * HW_)
                    bG, bU = next_bank(), next_bank()
                    for (b, ws) in ((bG, gslot), (bU, uslot)):
                        for dc in range(8):
                            P.op("pe", lambda e, b=b, ws=ws, dc=dc, cl=cl, js=js, xgT=xgT: e.matmul(
                                ps[b][:, 0:HW_], lhsT=Wring[ws][:, dc, cl:cl + 128], rhs=xgT[:, dc, js], start=(dc == 0), stop=(dc == 7)),
                                reads=[("W", ws)] + xgT_keys, writes=[("ps", b)])
                    s_ = sl[0] % 2
                    sl[0] += 1
                    P.op("act", lambda e, bG=bG, s_=s_, ex=ex, fc=fc: e.activation(
                        out=rg[s_], in_=ps[bG][:, 0:HW_], func=AF.Relu, scale=-1.0, bias=bgT[:, ex, fc:fc + 1]),
                        reads=[("ps", bG), ("bT", 0)], writes=[("rg", s_)])
                    P.op("act", lambda e, s_=s_: e.activation(out=sg[s_], in_=rg[s_], func=AF.Sigmoid, scale=-1.702, bias=sig_bias_col),
                         reads=[("rg", s_), "sigb"], writes=[("sg", s_)])
                    P.op("act", lambda e, bU=bU, s_=s_, ex=ex, fc=fc: e.activation(
                        out=pp[s_], in_=ps[bU][:, 0:HW_], func=AF.Relu, bias=buT[:, ex, fc:fc + 1]),
                        reads=[("ps", bU), ("bT", 1)], writes=[("pp", s_)])
                    P.op("dve", lambda e, s_=s_: e.scalar_tensor_tensor(out=rg[s_], in0=rg[s_], scalar=-7.0, in1=sg[s_], op0=ALU.add, op1=ALU.mult),
                         reads=[("rg", s_), ("sg", s_)], writes=[("rg", s_)])
                    P.op("dve", lambda e, s_=s_: e.scalar_tensor_tensor(out=pp[s_], in0=pp[s_], scalar=14.0, in1=rg[s_], op0=ALU.min, op1=ALU.mult),
                         reads=[("pp", s_), ("rg", s_)], writes=[("pp", s_)])
                    P.op("dve", lambda e, s_=s_, fc=fc, js=js: e.scalar_tensor_tensor(
                        out=hTs[:, fc, js], in0=rg[s_], scalar=6.0, in1=pp[s_], op0=ALU.mult, op1=ALU.subtract),
                        reads=[("rg", s_), ("pp", s_)], writes=[("hTs", fc, hh)])
                if fc == 3 and ex + 1 < NE:
                    load_unit(ex + 1, 0)
                    load_unit(ex + 1, 1)
            if ex + 1 < NE:
                load_unit(ex + 1, 2)
                load_unit(ex + 1, 3)
            hT_keys = [("hTs", f, hh) for f in range(8) for hh in range(2)]
            if ex + 1 < NE:
                transpose_expert(ex + 1)
                if ex + 2 < NE:
                    gather_expert(ex + 2)
            bpar = ex % 2
            P.op("sp", lambda e, ex=ex, bpar=bpar: e.dma_start(out=bdb[bpar], in_=bed_d[ex:ex + 1, :].partition_broadcast(128)),
                 writes=[("bdb", bpar)], dma=True)
            ygv = yg_d[ex * CAP:(ex + 1) * CAP, :].rearrange("(p a) d -> p a d", a=NCH)
            for a in range(NCH):
                ys_ = ysl[0] % 3
                ysl[0] += 1
                for dh in range(2):
                    b = next_bank()
                    for fc in range(8):
                        P.op("pe", lambda e, b=b, fc=fc, a=a, dh=dh: e.matmul(
                            ps[b][:, :], lhsT=hTs[:, fc, a * 128:(a + 1) * 128], rhs=Wring[4 + dh][:, fc, :],
                            start=(fc == 0), stop=(fc == 7)), reads=hT_keys + [("W", 4 + dh)], writes=[("ps", b)])
                    P.op("dve", lambda e, b=b, ys_=ys_, dh=dh, bpar=bpar: e.tensor_tensor(
                        out=ystage[ys_][:, dh * 512:(dh + 1) * 512], in0=ps[b][:, :], in1=bdb[bpar][:, dh * 512:(dh + 1) * 512], op=ALU.add),
                        reads=[("ps", b), ("bdb", bpar)], writes=[("ys", ys_, dh)])
                P.op("pool", lambda e, ys_=ys_, a=a, ex=ex: e.indirect_dma_start(
                    out=yg_d[:, :], out_offset=bass.IndirectOffsetOnAxis(ap=YIDX_t[:, ex * NCH + a:ex * NCH + a + 1], axis=0),
                    in_=ystage[ys_], in_offset=None, bounds_check=pool_regs["bc"], oob_is_err=False),
                    reads=[("ys", ys_, 0), ("ys", ys_, 1), "YIDX"] + [("YGZ", g8) for g8 in range(NSLOT // 2560)],
                    writes=[("YG", ex, a)], dma=True)
            if ex + 1 < NE:
                load_unit(ex + 1, 4)
                load_unit(ex + 1, 5)

        P.op("sp", lambda e: e.dma_start(out=lnp, in_=ln_d[4:6, :].partition_broadcast(128)), writes=["lnp"], dma=True)
        yg_keys = [("YG", ex, a) for ex in range(NE) for a in range(NCH)]
        yk = [f32v(WR_OFF + i * 1024, 1024) for i in range(8)]
        xn3 = f32v(WR_OFF + 8192, 1024)
        otile = [f32v(WR_OFF + 9216 + i * 1024, 1024) for i in range(2)]
        P.alias([("yk", i) for i in range(8)] + ["xn3", ("ot", 0), ("ot", 1)], [("W", i) for i in range(6)])
        toks = []
        xn3b = [xn3, f32v(WR_OFF + 11264, 1024)]
        P.alias([("xn3b", 0), ("xn3b", 1)], ["xn3"] + [("W", i) for i in range(6)])
        for tc in range(NT):
            P.alias([("R", tc)], [("R", tc, 0), ("R", tc, 1)])

        def t_s0(tc):
            for k in range(4):
                yi = (tc % 2) * 4 + k
                P.op("pool", lambda e, k=k, yi=yi: e.indirect_dma_start(
                    out=yk[yi], out_offset=None, in_=yg_d[:, :],
                    in_offset=bass.IndirectOffsetOnAxis(ap=DESTG_t[:, tc * 4 + k:tc * 4 + k + 1], axis=0)),
                    reads=yg_keys + [("DESTG", tc)], writes=[("yk", yi)], dma=True)

        def t_s0b(tc):
            for k in range(4):
                yi = (tc % 2) * 4 + k
                P.op("dve", lambda e, k=k, yi=yi: e.scalar_tensor_tensor(
                    out=Rt[:, tc, :], in0=yk[yi], scalar=Gk[:, tc, k:k + 1], in1=Rt[:, tc, :], op0=ALU.mult, op1=ALU.add),
                    reads=[("yk", yi), ("Gk", tc), ("R", tc)], writes=[("R", tc)])

        def t_s1(tc):
            ln_stats(Rt[:, tc, :], ("R", tc), tc % 2)

        def t_s2(tc):
            par = tc % 2
            ln_apply(Rt[:, tc, :], ("R", tc), par, otile[par], [("ot", par)], "lnp", xn3b[par], ("xn3b", par))
            toks.append(P.op("sp", lambda e: e.dma_start(out=out_d[tc * 128:(tc + 1) * 128, :], in_=otile[par]),
                             reads=[("ot", par)], writes=[("out", tc)], dma=True))

        skewed(NT, [t_s0, t_s0b, t_s1, t_s2])
        return finish(toks)

    return nc


def _rope_tables():
    t = np.arange(S)
    def cs(pos, dim):
        inv = (10000.0 ** (-np.arange(0, dim, 2, dtype=np.float32) / dim)).astype(np.float32)
        ang = pos.astype(np.float32)[:, None] * inv[None, :]
        return np.cos(ang).astype(np.float32), np.sin(ang).astype(np.float32)
    cr, sr = cs(t // 64, 32)
    cc, sc = cs(t % 64, 32)
    cq, sq = cs(t, 64)
    A = np.zeros((S, 2, 64), np.float32)
    A[:, 0] = np.concatenate([cr, cr, cc, cc], -1)
    A[:, 1] = np.concatenate([-sr, sr, -sc, sc], -1)
    B = np.zeros((S, 2, 64), np.float32)
    B[:, 0] = np.concatenate([cq, cq], -1)
    B[:, 1] = np.concatenate([-sq, sq], -1)
    return A, B


def make_in_maps(inputs, cores):
    f = lambda k: np.ascontiguousarray(np.asarray(inputs[k], dtype=np.float32))
    A, B = _rope_tables()
    shared = {
        "w_in": f("w_in")[0],
        "a_q_norm": f("a_q_norm"),
        "a_k_norm": f("a_k_norm"),
        "b_lambda": np.ascontiguousarray(np.concatenate(
            [f("b_lambda_q1"), f("b_lambda_k1"), f("b_lambda_q2"), f("b_lambda_k2")], 0)),
        "b_subln": np.ascontiguousarray(f("b_subln").reshape(128, 1)),
        "w_mix_out": f("w_mix_out")[0],
        "ln_gb": np.ascontiguousarray(np.concatenate(
            [f("ln1_g"), f("ln1_b"), f("ln2_g"), f("ln2_b"), f("ln3_g"), f("ln3_b")], 0)),
        "w_mem_q": f("w_mem_q")[0],
        "w_mem_kv": f("w_mem_kv")[0],
        "w_mem_out": f("w_mem_out")[0],
        "w_router": f("w_router")[0],
        "b_router": f("b_router"),
        "w_e_gate": f("w_e_gate")[0],
        "b_e_gate": f("b_e_gate")[0],
        "w_e_up": f("w_e_up")[0],
        "b_e_up": f("b_e_up")[0],
        "w_e_down": f("w_e_down")[0],
        "b_e_down": f("b_e_down")[0],
        "ropeA": A,
        "ropeB": B,
        "zeros_rows": np.zeros((2560, D), np.float32),
    }
    x = f("x")
    mem = f("mem")
    maps = []
    for c in cores:
        m = dict(shared)
        m["x"] = x[c]
        m["mem"] = mem[c]
        maps.append(m)
    return maps


def kernel(**inputs):
    nc = build_program("full")
    cores = list(range(8))
    in_maps = make_in_maps(inputs, cores)
    res = run_bass_kernel_spmd(nc, in_maps, core_ids=cores)
    out = np.stack([np.asarray(r["out"], dtype=np.float32) for r in res.results], 0)
    return out
```
